# Optimizing a Trainium2 kernel written in Bass

```python
import math
import jax
import jax.numpy as jnp
from jax import lax
import numpy as np

D_MODEL = 1024
BATCH = 2
SEQ = 8192
DEPTH = 2

HEAD_DIM = 64
N_HEADS_SB = 8
N_HEADS_NSA = 8
NSA_KV_GROUPS = 2
NSA_HPG = N_HEADS_NSA // NSA_KV_GROUPS
W_SB = N_HEADS_SB * HEAD_DIM
W_NSA = N_HEADS_NSA * HEAD_DIM
CMP_BLOCK = 32
CMP_STRIDE = 16
CMP_HIDDEN = 256
SLC_BLOCK = 64
N_SELECT = 16
WINDOW = 512
Q_BLOCK = 128
REL_BUCKETS = 32
REL_MAX_DIST = 128
D_FF_DENSE = 2816
N_EXPERTS = 8
TOP_K = 2
D_FF_EXPERT = 3584
MOE_ROW_BLOCK = 256
NORM_EPS = 1e-6
FORCED_BLOCK_SCORE = 1e4
N_DENSE_LAYERS = (DEPTH + 1) // 2
N_MOE_LAYERS = DEPTH // 2

OFF_SB_Q = 0
OFF_SB_K = OFF_SB_Q + W_SB
OFF_SB_V = OFF_SB_K + W_SB
OFF_NSA_Q = OFF_SB_V + W_SB
OFF_NSA_KV = OFF_NSA_Q + W_NSA
OFF_NSA_GATE = OFF_NSA_KV + 3 * 2 * NSA_KV_GROUPS * HEAD_DIM
IN_COLS = OFF_NSA_GATE + 3 * N_HEADS_NSA

kernel_name = "hybrid_sb_nsa_moe_block"


def rms_norm(x, gain):
    xf = x.astype(jnp.float32)
    y = xf * lax.rsqrt(jnp.mean(xf * xf, axis=-1, keepdims=True) + NORM_EPS)
    return (y * gain.astype(jnp.float32)).astype(x.dtype)


def t5_bucket(dist):
    n = jnp.maximum(dist, 0)
    max_exact = REL_BUCKETS // 2
    large = max_exact + (jnp.log(jnp.maximum(n, 1).astype(jnp.float32) / max_exact)
                         / math.log(REL_MAX_DIST / max_exact)
                         * (REL_BUCKETS - max_exact)).astype(jnp.int32)
    large = jnp.minimum(large, REL_BUCKETS - 1)
    return jnp.where(n < max_exact, n, large)


def masked_softmax(logits, mask):
    logits = jnp.where(mask, logits, -jnp.inf)
    m = jnp.max(logits, axis=-1, keepdims=True)
    m = jnp.where(jnp.isfinite(m), m, 0.0)
    p = jnp.where(mask, jnp.exp(logits - m), 0.0)
    return p / jnp.maximum(jnp.sum(p, axis=-1, keepdims=True), 1e-30)


def stick_breaking_attention(q, k, v):
    B, H, S, dh = q.shape
    nq = S // Q_BLOCK
    scale = dh ** -0.5
    q_blocks = jnp.moveaxis(q.reshape(B, H, nq, Q_BLOCK, dh), 2, 0)
    key_pos = jnp.arange(S, dtype=jnp.int32)

    def one_block(args):
        qb, blk = args
        t = blk * Q_BLOCK + jnp.arange(Q_BLOCK, dtype=jnp.int32)
        mask = key_pos[None, :] < t[:, None]
        z = jnp.einsum('bhqd,bhkd->bhqk', qb, k) * scale
        log_keep = jnp.where(mask, jax.nn.log_sigmoid(-z), 0.0)
        later = lax.cumsum(log_keep, axis=3, reverse=True) - log_keep
        w = jnp.where(mask, jnp.exp(jax.nn.log_sigmoid(z) + later), 0.0)
        return jnp.einsum('bhqk,bhkd->bhqd', w, v)

    out = lax.map(one_block, (q_blocks, jnp.arange(nq, dtype=jnp.int32)))
    return jnp.moveaxis(out, 0, 2).reshape(B, H, S, dh)


def compress_tokens(k, pos_emb, w1, w2):
    B, G, S, dh = k.shape
    n_cmp = (S - CMP_BLOCK) // CMP_STRIDE + 1
    idx = jnp.arange(n_cmp)[:, None] * CMP_STRIDE + jnp.arange(CMP_BLOCK)[None, :]
    blocks = k[:, :, idx] + pos_emb
    flat = blocks.reshape(B, G, n_cmp, CMP_BLOCK * dh)
    return (jax.nn.gelu(flat @ w1) @ w2).astype(jnp.float32)


def native_sparse_attention(q, k_cmp_raw, v_cmp_raw, k_slc, v_slc, k_win, v_win, gates,
                            rel_table, cmp_pos_k, cmp_w1_k, cmp_w2_k,
                            cmp_pos_v, cmp_w1_v, cmp_w2_v):
    B, G, Z, S, dh = q.shape
    scale = dh ** -0.5
    nq = S // Q_BLOCK
    n_slc = S // SLC_BLOCK
    n_sel = min(N_SELECT, n_slc)

    k_c = compress_tokens(k_cmp_raw, cmp_pos_k, cmp_w1_k, cmp_w2_k)
    v_c = compress_tokens(v_cmp_raw, cmp_pos_v, cmp_w1_v, cmp_w2_v)
    n_cmp = k_c.shape[2]
    cmp_start = jnp.arange(n_cmp, dtype=jnp.int32) * CMP_STRIDE
    cmp_end = cmp_start + CMP_BLOCK - 1
    slc_start = jnp.arange(n_slc, dtype=jnp.int32) * SLC_BLOCK
    overlap = ((cmp_start[:, None] < slc_start[None, :] + SLC_BLOCK)
               & (cmp_start[:, None] + CMP_BLOCK > slc_start[None, :])).astype(jnp.float32)

    k_blocks = k_slc.reshape(B, G, n_slc, SLC_BLOCK, dh)
    v_blocks = v_slc.reshape(B, G, n_slc, SLC_BLOCK, dh)
    k_win_pad = jnp.pad(k_win, ((0, 0), (0, 0), (WINDOW, 0), (0, 0)))
    v_win_pad = jnp.pad(v_win, ((0, 0), (0, 0), (WINDOW, 0), (0, 0)))

    table_hd = rel_table.reshape(REL_BUCKETS, G, Z)
    table_g = jnp.transpose(table_hd, (1, 0, 2))
    bi = jnp.arange(B)[:, None, None, None]
    gi = jnp.arange(G)[None, :, None, None]
    in_blk = jnp.arange(SLC_BLOCK, dtype=jnp.int32)
    win_off = jnp.arange(Q_BLOCK + WINDOW, dtype=jnp.int32) - WINDOW
    blk_ids = jnp.arange(n_slc, dtype=jnp.int32)[None, :]

    q_blocks = jnp.moveaxis(q.reshape(B, G, Z, nq, Q_BLOCK, dh), 3, 0)
    g_blocks = jnp.moveaxis(gates.reshape(B, G, Z, nq, Q_BLOCK, 3), 3, 0)

    def head_bias(dist):
        return jnp.transpose(table_hd[t5_bucket(dist)], (2, 3, 0, 1))

    def one_block(args):
        qb, gb, blk = args
        c0 = blk * Q_BLOCK
        t = c0 + jnp.arange(Q_BLOCK, dtype=jnp.int32)

        dist_c = t[:, None] - cmp_end[None, :]
        logit_c = jnp.einsum('bgzqd,bgnd->bgzqn', qb, k_c) * scale + head_bias(dist_c)
        p_c = masked_softmax(logit_c, dist_c >= 0)
        o_c = jnp.einsum('bgzqn,bgnd->bgzqd', p_c, v_c)

        imp = jnp.einsum('bgzqn,nj->bgqj', p_c, overlap)
        valid = slc_start[None, :] <= t[:, None]
        cur = (t // SLC_BLOCK)[:, None]
        forced = valid & ((blk_ids == 0) | (blk_ids == cur) | (blk_ids == cur - 1))
        score = jnp.where(valid, imp + jnp.where(forced, FORCED_BLOCK_SCORE, 0.0), -1.0)
        top_score, top_idx = lax.top_k(score, n_sel)
        k_sel = k_blocks[bi, gi, top_idx]
        v_sel = v_blocks[bi, gi, top_idx]
        tok = top_idx[..., None] * SLC_BLOCK + in_blk
        dist_s = t[None, None, :, None, None] - tok
        mask_s = (top_score >= 0.0)[..., None] & (dist_s >= 0)
        bias_s = jnp.moveaxis(table_g[gi[..., None], t5_bucket(dist_s)], -1, 2)
        logit_s = jnp.einsum('bgzqd,bgqnld->bgzqnl', qb, k_sel) * scale + bias_s
        n_tok = n_sel * SLC_BLOCK
        p_s = masked_softmax(logit_s.reshape(B, G, Z, Q_BLOCK, n_tok),
                             mask_s.reshape(B, G, 1, Q_BLOCK, n_tok))
        o_s = jnp.einsum('bgzqm,bgqmd->bgzqd', p_s, v_sel.reshape(B, G, Q_BLOCK, n_tok, dh))

        k_w = lax.dynamic_slice_in_dim(k_win_pad, c0, Q_BLOCK + WINDOW, axis=2)
        v_w = lax.dynamic_slice_in_dim(v_win_pad, c0, Q_BLOCK + WINDOW, axis=2)
        pos_w = c0 + win_off
        dist_w = t[:, None] - pos_w[None, :]
        mask_w = (dist_w >= 0) & (dist_w < WINDOW) & (pos_w[None, :] >= 0)
        logit_w = jnp.einsum('bgzqd,bgkd->bgzqk', qb, k_w) * scale + head_bias(dist_w)
        p_w = masked_softmax(logit_w, mask_w)
        o_w = jnp.einsum('bgzqk,bgkd->bgzqd', p_w, v_w)

        return gb[..., 0:1] * o_c + gb[..., 1:2] * o_s + gb[..., 2:3] * o_w

    out = lax.map(one_block, (q_blocks, g_blocks, jnp.arange(nq, dtype=jnp.int32)))
    return jnp.transpose(out, (1, 0, 4, 2, 3, 5)).reshape(B, S, G * Z * dh)


def hybrid_mixer(h, w_in, w_out, g_sb, g_nsa, rel_table, cmp_pos_k, cmp_w1_k, cmp_w2_k,
                 cmp_pos_v, cmp_w1_v, cmp_w2_v):
    B, S, _ = h.shape
    G, Z, dh = NSA_KV_GROUPS, NSA_HPG, HEAD_DIM
    proj = (h @ w_in).astype(jnp.float32)

    def heads(lo, n):
        return proj[..., lo:lo + n * dh].reshape(B, S, n, dh).transpose(0, 2, 1, 3)

    o_sb = stick_breaking_attention(heads(OFF_SB_Q, N_HEADS_SB), heads(OFF_SB_K, N_HEADS_SB),
                                    heads(OFF_SB_V, N_HEADS_SB))
    o_sb = o_sb.transpose(0, 2, 1, 3).reshape(B, S, W_SB)

    q_nsa = proj[..., OFF_NSA_Q:OFF_NSA_KV].reshape(B, S, G, Z, dh).transpose(0, 2, 3, 1, 4)
    kv = proj[..., OFF_NSA_KV:OFF_NSA_GATE].reshape(B, S, 3, 2, G, dh).transpose(2, 3, 0, 4, 1, 5)
    gates = jax.nn.sigmoid(proj[..., OFF_NSA_GATE:IN_COLS]).reshape(B, S, G, Z, 3)
    gates = gates.transpose(0, 2, 3, 1, 4)
    o_nsa = native_sparse_attention(q_nsa, kv[0, 0], kv[0, 1], kv[1, 0], kv[1, 1],
                                    kv[2, 0], kv[2, 1], gates, rel_table,
                                    cmp_pos_k, cmp_w1_k, cmp_w2_k, cmp_pos_v, cmp_w1_v, cmp_w2_v)

    merged = jnp.concatenate([rms_norm(o_sb, g_sb), rms_norm(o_nsa, g_nsa)], axis=-1)
    return merged.astype(h.dtype) @ w_out


def swiglu(h, w_gate, w_up, w_down):
    return (jax.nn.silu(h @ w_gate) * (h @ w_up)) @ w_down


def moe_swiglu(h, w_router, b_router, w_gate, w_up, w_down):
    T, D = h.shape
    logits = h.astype(jnp.float32) @ w_router.astype(jnp.float32) + b_router.astype(jnp.float32)
    probs = jax.nn.softmax(logits, axis=-1)
    top_p, top_e = lax.top_k(probs, TOP_K)
    top_p = top_p / jnp.sum(top_p, axis=-1, keepdims=True)

    n_assign = T * TOP_K
    flat_e = top_e.reshape(-1)
    flat_tok = jnp.arange(n_assign, dtype=jnp.int32) // TOP_K
    order = jnp.argsort(flat_e)
    sorted_e = flat_e[order]
    sorted_tok = flat_tok[order]
    sorted_w = top_p.reshape(-1)[order]

    counts = jnp.bincount(flat_e, length=N_EXPERTS)
    padded = (counts + MOE_ROW_BLOCK - 1) // MOE_ROW_BLOCK * MOE_ROW_BLOCK
    start = jnp.cumsum(counts) - counts
    pad_end = jnp.cumsum(padded)
    pad_start = pad_end - padded
    dest = pad_start[sorted_e] + jnp.arange(n_assign, dtype=jnp.int32) - start[sorted_e]
    n_blocks = -(-n_assign // MOE_ROW_BLOCK) + N_EXPERTS
    row_tok = jnp.zeros((n_blocks * MOE_ROW_BLOCK,), jnp.int32).at[dest].set(sorted_tok)
    block_expert = jnp.minimum(
        jnp.searchsorted(pad_end, jnp.arange(n_blocks, dtype=jnp.int32) * MOE_ROW_BLOCK, side='right'),
        N_EXPERTS - 1)
    rows = h[row_tok].reshape(n_blocks, MOE_ROW_BLOCK, D)

    def expert_block(args):
        xb, e = args
        return swiglu(xb, w_gate[e], w_up[e], w_down[e])

    y = lax.map(expert_block, (rows, block_expert)).reshape(-1, D)
    contrib = y[dest].astype(jnp.float32) * sorted_w[:, None]
    return jax.ops.segment_sum(contrib, sorted_tok, num_segments=T).astype(h.dtype)


def setup_inputs(seed: int = 0) -> dict:
    key = jax.random.key(seed)
    ks = iter(jax.random.split(key, 32))
    D, dh = D_MODEL, HEAD_DIM

    def nrm(shape, scale):
        return jax.random.normal(next(ks), shape, jnp.float32) * scale

    def gain(shape):
        return 1.0 + nrm(shape, 0.02)

    return {
        "x": nrm((BATCH, SEQ, D), 1.0),
        "c": nrm((BATCH, D), 1.0),
        "rel_table": nrm((REL_BUCKETS, N_HEADS_NSA), 0.2),
        "w_ada": nrm((DEPTH, D, 6 * D), 0.5 * D ** -0.5),
        "b_ada": nrm((DEPTH, 6 * D), 0.01),
        "g_pre_mix": gain((DEPTH, D)),
        "g_post_mix": gain((DEPTH, D)),
        "g_pre_ffn": gain((DEPTH, D)),
        "g_post_ffn": gain((DEPTH, D)),
        "w_in": nrm((DEPTH, D, IN_COLS), D ** -0.5),
        "w_out": nrm((DEPTH, W_SB + W_NSA, D), (W_SB + W_NSA) ** -0.5),
        "g_sb": gain((DEPTH, W_SB)),
        "g_nsa": gain((DEPTH, W_NSA)),
        "cmp_pos_k": nrm((DEPTH, CMP_BLOCK, dh), 0.5),
        "cmp_w1_k": nrm((DEPTH, CMP_BLOCK * dh, CMP_HIDDEN), (CMP_BLOCK * dh) ** -0.5),
        "cmp_w2_k": nrm((DEPTH, CMP_HIDDEN, dh), CMP_HIDDEN ** -0.5),
        "cmp_pos_v": nrm((DEPTH, CMP_BLOCK, dh), 0.5),
        "cmp_w1_v": nrm((DEPTH, CMP_BLOCK * dh, CMP_HIDDEN), (CMP_BLOCK * dh) ** -0.5),
        "cmp_w2_v": nrm((DEPTH, CMP_HIDDEN, dh), CMP_HIDDEN ** -0.5),
        "ffn_w_gate": nrm((N_DENSE_LAYERS, D, D_FF_DENSE), D ** -0.5),
        "ffn_w_up": nrm((N_DENSE_LAYERS, D, D_FF_DENSE), D ** -0.5),
        "ffn_w_down": nrm((N_DENSE_LAYERS, D_FF_DENSE, D), D_FF_DENSE ** -0.5),
        "moe_w_router": nrm((N_MOE_LAYERS, D, N_EXPERTS), D ** -0.5),
        "moe_b_router": nrm((N_MOE_LAYERS, N_EXPERTS), 0.01),
        "moe_w_gate": nrm((N_MOE_LAYERS, N_EXPERTS, D, D_FF_EXPERT), D ** -0.5),
        "moe_w_up": nrm((N_MOE_LAYERS, N_EXPERTS, D, D_FF_EXPERT), D ** -0.5),
        "moe_w_down": nrm((N_MOE_LAYERS, N_EXPERTS, D_FF_EXPERT, D), D_FF_EXPERT ** -0.5),
    }


def reference(x, c, rel_table, w_ada, b_ada, g_pre_mix, g_post_mix, g_pre_ffn, g_post_ffn,
              w_in, w_out, g_sb, g_nsa, cmp_pos_k, cmp_w1_k, cmp_w2_k, cmp_pos_v, cmp_w1_v,
              cmp_w2_v, ffn_w_gate, ffn_w_up, ffn_w_down, moe_w_router, moe_b_router,
              moe_w_gate, moe_w_up, moe_w_down):
    B, S, D = x.shape
    c_act = jax.nn.silu(c)
    for layer in range(DEPTH):
        mod = (c_act @ w_ada[layer] + b_ada[layer])[:, None, :]
        shift_m, scale_m, gate_m, shift_f, scale_f, gate_f = jnp.split(mod, 6, axis=-1)

        h = rms_norm(x, g_pre_mix[layer]) * (1.0 + scale_m) + shift_m
        m = hybrid_mixer(h, w_in[layer], w_out[layer], g_sb[layer], g_nsa[layer], rel_table,
                         cmp_pos_k[layer], cmp_w1_k[layer], cmp_w2_k[layer],
                         cmp_pos_v[layer], cmp_w1_v[layer], cmp_w2_v[layer])
        x = x + gate_m * rms_norm(m, g_post_mix[layer])

        h = rms_norm(x, g_pre_ffn[layer]) * (1.0 + scale_f) + shift_f
        i = layer // 2
        if layer % 2 == 0:
            f = swiglu(h, ffn_w_gate[i], ffn_w_up[i], ffn_w_down[i])
        else:
            f = moe_swiglu(h.reshape(B * S, D), moe_w_router[i], moe_b_router[i],
                           moe_w_gate[i], moe_w_up[i], moe_w_down[i]).reshape(B, S, D)
        x = x + gate_f * rms_norm(f, g_post_ffn[layer])
    return x
```

```python
import numpy as np
import ml_dtypes
import concourse.bass as bass
import concourse.mybir as mybir
from concourse.bass_utils import run_bass_kernel_spmd

F32 = mybir.dt.float32
BF16 = mybir.dt.bfloat16
AF = mybir.ActivationFunctionType
OP = mybir.AluOpType
AX = mybir.AxisListType

ENGS = ("pe", "act", "dve", "pool", "sp")
NEG = -30000.0
DEBUG = False


_NM = [0]


def SBT(nc, name, shape, dt):
    _NM[0] += 1
    return nc.sbuf_tensor("t%d_%s" % (_NM[0], name), shape, dt)


import types


def freeze(fn):
    if fn.__closure__ is None:
        return fn
    cells = []
    for c in fn.__closure__:
        try:
            cells.append(types.CellType(c.cell_contents))
        except ValueError:
            cells.append(c)
    return types.FunctionType(fn.__code__, fn.__globals__, fn.__name__, fn.__defaults__, tuple(cells))


class Dep:
    __slots__ = ("lw", "rd")

    def __init__(self):
        self.lw = []
        self.rd = []


def deps(n):
    return [Dep() for _ in range(n)]


class Prog:
    def __init__(self, nc, ring=6):
        self.nc = nc
        self.q = {e: [] for e in ENGS}
        self.cnt = {e: 0 for e in ENGS}
        self.sems = {}
        self.waited = {e: {} for e in ENGS}
        self.ring = ring
        self.dma_n = {"sp": 0, "pool": 0}
        self.fence = []
        self.colls = []
        for e in ("pe", "act", "dve", "pool"):
            self.sems[e] = nc.alloc_semaphore("s_" + e)
        for qn in ("sp", "pool"):
            for i in range(ring):
                self.sems[(qn, i)] = nc.alloc_semaphore("d_%s%d" % (qn, i))

    def barrier(self):
        ev = []
        for e in ("pe", "act", "dve", "pool"):
            if self.cnt[e] > 0:
                ev.append((e, self.cnt[e]))
        for qn in ("sp", "pool"):
            n = self.dma_n[qn]
            for slot in range(min(n, self.ring)):
                uses = (n - slot + self.ring - 1) // self.ring
                ev.append(((qn, slot), 16 * uses))
        ev.extend((c, 1) for c in self.colls)
        self.fence = ev

    def _deps(self, eng, r, w):
        evs = list(self.fence)
        for d in r:
            evs.extend(d.lw)
        for d in w:
            evs.extend(d.lw)
            evs.extend(d.rd)
        wd = self.waited[eng]
        best = {}
        for (k, v) in evs:
            if k == eng and eng == "pe":
                continue
            if wd.get(k, 0) >= v:
                continue
            if best.get(k, 0) < v:
                best[k] = v
        waits = []
        for k, v in best.items():
            wd[k] = v
            waits.append((k, v))
        return waits

    def _commit(self, ev, r, w):
        for d in r:
            d.rd.append(ev)
            if len(d.rd) > 64:
                d.rd = d.rd[-32:] if False else d.rd
        comp = ("pe", "act", "dve", "pool")
        isdma = ev[0] not in comp
        for d in w:
            if isdma and d.lw and d.lw[0][0] not in comp:
                d.lw = d.lw[-11:] + [ev]
            else:
                d.lw = [ev]
            d.rd = []

    def op(self, eng, fn, r=(), w=()):
        waits = self._deps(eng, r, w)
        self.cnt[eng] += 1
        ev = (eng, self.cnt[eng])
        self.q[eng].append((waits, freeze(fn), (eng, 1)))
        self._commit(ev, r, w)

    def dma(self, qn, out, in_, r=(), w=(), **kw):
        n = self.dma_n[qn]
        slot = n % self.ring
        key = (qn, slot)
        val = 16 * (n // self.ring + 1)
        waits = self._deps(qn, r, w)
        if n >= self.ring:
            pv = val - 16
            if self.waited[qn].get(key, 0) < pv:
                self.waited[qn][key] = pv
                waits.append((key, pv))
        self.dma_n[qn] = n + 1
        fn = (lambda e, out=out, in_=in_, kw=kw: e.dma_start(out=out, in_=in_, **kw))
        self.q[qn].append((waits, fn, (key, 16)))
        self._commit((key, val), r, w)

    def coll(self, kind, src, dst, groups, r=(), w=()):
        name = "coll%d" % len(self.colls)
        self.colls.append(name)
        self.sems[name] = self.nc.alloc_semaphore(name)
        waits = self._deps("pool", r, w)
        fn = freeze(lambda e: e.collective_compute(kind, OP.bypass, replica_groups=groups, ins=[src], outs=[dst]))
        self.q["pool"].append((waits, fn, (name, 1)))
        self._commit((name, 1), r, w)

    def finish(self, eng="sp"):
        waits = [(c, 1) for c in self.colls]
        for e in ("pe", "act", "dve", "pool"):
            if self.cnt[e] > 0 and e != eng:
                waits.append((e, self.cnt[e]))
        for qn in ("sp", "pool"):
            n = self.dma_n[qn]
            for slot in range(min(n, self.ring)):
                uses = (n - slot + self.ring - 1) // self.ring
                waits.append(((qn, slot), 16 * uses))
        self.q[eng].append((waits, None, None))

    def emit(self):
        nc = self.nc
        names = {"pe": "tensor", "act": "scalar", "dve": "vector", "pool": "gpsimd", "sp": "sync"}
        with nc.Block() as block:
            for e in ENGS:
                items = self.q[e]
                if not items:
                    continue

                def body(engobj, items=items):
                    for waits, fn, inc in items:
                        for (k, v) in waits:
                            engobj.wait_ge(self.sems[k], v)
                        if isinstance(fn, tuple):
                            fn[1](engobj)
                        elif fn is not None:
                            ins = fn(engobj)
                            ins.then_inc(self.sems[inc[0]], inc[1])

                getattr(block, names[e])(body)


def run_pipeline(steps, nst):
    n = len(steps)
    for it in range(n + nst - 1):
        for k in range(nst):
            i = it - k
            if 0 <= i < n and steps[i][k] is not None:
                steps[i][k]()


class Ctx:
    pass


def mk_ctx(nc):
    C = Ctx()
    C.nc = nc
    C.P = Prog(nc)
    C.psall = nc.alloc_psum_tensor("psall", [128, 8, 512], F32)
    C.bank = [C.psall[:, i, :] for i in range(8)]
    C.dbank = deps(8)
    P = C.P
    C.onesf = nc.alloc_sbuf_tensor("onesf", [128, 128], F32)
    C.ident = nc.alloc_sbuf_tensor("ident", [128, 128], BF16)
    C.onesb = nc.alloc_sbuf_tensor("onesb", [128, 128], BF16)
    C.identf = nc.alloc_sbuf_tensor("identf", [128, 128], F32)
    C.dconst = Dep()
    P.op("pool", lambda e: e.memset(C.onesf[:], 1.0), w=[C.dconst])
    P.op("pool", lambda e: e.affine_select(out=C.identf[:], in_=C.onesf[:], pattern=[[-1, 128]],
                                           compare_op=OP.is_equal, fill=0.0, base=0,
                                           channel_multiplier=1), r=[C.dconst], w=[C.dconst])
    P.op("pool", lambda e: e.memset(C.onesb[:], 1.0), w=[C.dconst])
    P.op("pool", lambda e: e.affine_select(out=C.ident[:], in_=C.onesf[:], pattern=[[-1, 128]],
                                           compare_op=OP.is_equal, fill=0.0, base=0,
                                           channel_multiplier=1), r=[C.dconst], w=[C.dconst])
    return C


def ada_rows(C, stk, cT, wada, bada, ncols, out_tile, dout):
    nc, P = C.nc, C.P
    csb = stk.enter_context(SBT(nc, "ada_c", [128, 8], F32))
    ca = stk.enter_context(SBT(nc, "ada_ca", [128, 8], F32))
    crep = stk.enter_context(SBT(nc, "ada_crep", [128, 8, 128], F32))
    wbuf = [stk.enter_context(SBT(nc, "ada_w%d" % i, [128, 8, 512], F32)) for i in range(2)]
    brow = stk.enter_context(SBT(nc, "ada_b", [1, ncols], F32))
    dc, dca, dcr, db = Dep(), Dep(), Dep(), Dep()
    dw = deps(2)
    P.dma("sp", csb[:], cT[:, :], w=[dc])
    P.dma("sp", brow[:], bada[0:1, 0:ncols], w=[db])
    P.op("act", lambda e: e.activation(out=ca[:], in_=csb[:], func=AF.Silu), r=[dc], w=[dca])
    for kc in range(8):
        P.op("dve", lambda e, kc=kc: e.tensor_scalar(out=crep[:, kc, :], in0=C.onesf[:], scalar1=ca[:, kc:kc + 1],
                                                     scalar2=None, op0=OP.mult), r=[dca, C.dconst], w=[dcr])
    wv = wada.rearrange("(kc p) n -> p kc n", p=128)
    for j in range(ncols // 512):
        b = j % 2
        P.dma("sp", wbuf[b][:], wv[:, :, j * 512:(j + 1) * 512], w=[dw[b]])
        bk = C.bank[j % 2]
        dbk = C.dbank[j % 2]
        for kc in range(8):
            P.op("pe", lambda e, kc=kc, b=b, bk=bk: e.matmul(bk[:], lhsT=crep[:, kc, :], rhs=wbuf[b][:, kc, :],
                                                            start=(kc == 0), stop=False), r=[dcr, dw[b]], w=[dbk])
        P.op("pe", lambda e, j=j, bk=bk: e.matmul(bk[:], lhsT=C.onesf[0:1, :], rhs=brow[0:1, j * 512:(j + 1) * 512],
                                                  start=False, stop=True), r=[db, C.dconst], w=[dbk])
        P.op("dve", lambda e, j=j, bk=bk: e.tensor_copy(out=out_tile[:, j * 512:(j + 1) * 512], in_=bk[:]),
             r=[dbk], w=[dout])


def bcast_row(C, stk, name, src, n, out_tile, dout, col0=0):
    C.P.dma("sp", out_tile[:, col0:col0 + n], src[0:1, 0:n].to_broadcast([128, n]), w=[dout])


def prenorm_proj(C, stk0, S, x, srow, shrow, dmod, wfm_d, nfm, wtm_d, ntm, fm_evac, tm_evac):
    nc, P = C.nc, C.P
    from contextlib import ExitStack
    with ExitStack() as stk:
        wfm = stk.enter_context(SBT(nc, "pp_wfm", [128, 8, nfm], BF16))
        wtm = stk.enter_context(SBT(nc, "pp_wtm", [128, 8, ntm], BF16))
        xt = [stk.enter_context(SBT(nc, "pp_x%d" % i, [128, 1024], F32)) for i in range(2)]
        junk = stk.enter_context(SBT(nc, "pp_junk", [128, 1024], BF16))
        tmp = stk.enter_context(SBT(nc, "pp_tmp", [128, 1024], F32))
        hb = [stk.enter_context(SBT(nc, "pp_hb%d" % i, [128, 1024], BF16)) for i in range(2)]
        hT = [stk.enter_context(SBT(nc, "pp_hT%d" % i, [128, 8, 512], BF16)) for i in range(2)]
        st = stk.enter_context(SBT(nc, "pp_st", [128, 8], F32))
        dwf, dwt, djunk, dtmp, dst_ = Dep(), Dep(), Dep(), Dep(), Dep()
        dx, dhb, dhT = deps(2), deps(2), deps(2)
        P.dma("pool", wfm[:], wfm_d.rearrange("(kc p) n -> p kc n", p=128), w=[dwf])
        P.dma("pool", wtm[:], wtm_d.rearrange("(kc p) n -> p kc n", p=128), w=[dwt])
        xt.append(stk.enter_context(SBT(nc, "pp_x2", [128, 1024], F32)))
        hb.append(stk.enter_context(SBT(nc, "pp_hb2", [128, 1024], BF16)))
        st3 = [stk.enter_context(SBT(nc, "pp_st%d" % i, [128, 8], F32)) for i in range(3)]
        dx.append(Dep()); dhb.append(Dep())
        dst3 = deps(3)
        steps = []
        for tt in range(S // 128):
            ch, i = tt // 4, tt % 4
            hb_ = ch % 2
            b = tt % 3
            bk = 6 + (tt % 2)

            def stA(tt=tt, b=b):
                st_, dstx = st3[b], dst3[b]
                P.dma("sp", xt[b][:], C.xtile(tt), r=[C.dxsrc(tt)], w=[dx[b]])
                P.op("act", lambda e: e.activation(out=junk[:], in_=xt[b][:], func=AF.Square, accum_out=st_[:, 0:1]), r=[dx[b]], w=[djunk, dstx])
                P.op("act", lambda e: e.activation(out=st_[:, 1:2], in_=st_[:, 0:1], func=AF.Sqrt, scale=1.0 / 1024, bias=C.epsb[:, 0:1]), r=[dstx, C.dconst], w=[dstx])
                P.op("dve", lambda e: e.reciprocal(out=st_[:, 2:3], in_=st_[:, 1:2]), r=[dstx], w=[dstx])
                P.op("dve", lambda e: e.scalar_tensor_tensor(out=tmp[:], in0=xt[b][:], scalar=st_[:, 2:3], in1=srow[:], op0=OP.mult, op1=OP.mult),
                     r=[dx[b], dstx, dmod], w=[dtmp])
                P.op("dve", lambda e: e.tensor_tensor(out=hb[b][:], in0=tmp[:], in1=shrow[:], op=OP.add), r=[dtmp, dmod], w=[dhb[b]])

            def stB(tt=tt, b=b, bk=bk, i=i, hb_=hb_):
                pT = C.bank[bk][:].bitcast(BF16)
                for kc in range(8):
                    P.op("pe", lambda e, kc=kc: e.transpose(out=pT[:, kc * 128:(kc + 1) * 128], in_=hb[b][:, kc * 128:(kc + 1) * 128], identity=C.ident[:]),
                         r=[dhb[b], C.dconst], w=[C.dbank[bk]])
                P.op("act", lambda e: e.activation(out=hT[hb_][:, :, i * 128:(i + 1) * 128], in_=pT.rearrange("p (k t) -> p k t", k=8), func=AF.Copy),
                     r=[C.dbank[bk]], w=[dhT[hb_]])

            def stC(ch=ch, hb_=hb_):
                for g in range(nfm // 128):
                    bk2 = g % 3
                    for kc in range(8):
                        P.op("pe", lambda e, g=g, kc=kc, bk2=bk2: e.matmul(C.bank[bk2][:], lhsT=wfm[:, kc, g * 128:(g + 1) * 128], rhs=hT[hb_][:, kc, :],
                                                                         start=(kc == 0), stop=(kc == 7)), r=[dwf, dhT[hb_]], w=[C.dbank[bk2]])
                    fm_evac(g, ch, C.bank[bk2], C.dbank[bk2])
                for i2 in range(4):
                    bk2 = 3 + (i2 % 3)
                    for kc in range(8):
                        P.op("pe", lambda e, i2=i2, kc=kc, bk2=bk2: e.matmul(C.bank[bk2][:, 0:ntm], lhsT=hT[hb_][:, kc, i2 * 128:(i2 + 1) * 128], rhs=wtm[:, kc, :],
                                                                           start=(kc == 0), stop=(kc == 7)), r=[dwt, dhT[hb_]], w=[C.dbank[bk2]])
                    tm_evac(ch * 4 + i2, C.bank[bk2], C.dbank[bk2])

            steps.append((stA, stB, stC if i == 3 else None))
        run_pipeline(steps, 3)
    P.barrier()


def build_A(C, L, S, x, o, din):
    from contextlib import ExitStack
    NT, NQC = S // 128, S // 512
    NC_ = S // 16 - 1
    NCH = (NC_ + 127) // 128
    nc = C.nc
    cT = din("cT", [128, 8]); wada = din("wada", [1024, 2048]); bada = din("bada", [1, 2048])
    gpre = din("gpre", [1, 1024])
    wfm_sb = din("wfm_sb", [1024, 256]); wtm_sb = din("wtm_sb", [1024, 128])
    wfm_n = din("wfm_n", [1024, 640]); wtm_n = din("wtm_n", [1024, 134])
    w1_d = din("w1", [128, 32, 256]); posT_d = din("posT", [128, 32]); w2_d = din("w2", [128, 2, 2, 64])
    rconst_d = din("rconst", [128, 4, 193]); wsel_d = din("wsel", [128, 255])
    tbg_d = din("tbg", [128, 2, 2, 128]); tcg_d = din("tcg", [128, 4, 5, 512]); chv_d = din("chv", [128, 4])
    negs_d = din("negs", [128, 3, 128]); negc_d = din("negc", [128, 5, 512])
    P = C.P
    C.do = Dep()
    bank, dbank = C.bank, C.dbank
    top = ExitStack()
    C.epsb = top.enter_context(SBT(nc, "epsb", [128, 1], F32))
    P.op("pool", lambda e: e.memset(C.epsb[:], 1e-6), w=[C.dconst])
    mod = top.enter_context(SBT(nc, "mod", [128, 2048], F32))
    srow = top.enter_context(SBT(nc, "srow", [128, 1024], F32))
    dmod = Dep()
    with ExitStack() as stk:
        ada_rows(C, stk, cT, wada, bada, 2048, mod, dmod)
    P.barrier()
    bcast_row(C, None, "gpre", gpre, 1024, srow, dmod)
    P.op("dve", lambda e: e.scalar_tensor_tensor(out=srow[:], in0=mod[:, 1024:2048], scalar=1.0, in1=srow[:], op0=OP.add, op1=OP.mult),
         r=[dmod], w=[dmod])
    shrow = mod[:, 0:1024]
    negs = top.enter_context(SBT(nc, "negs", [128, 3, 128], BF16))
    NIU = top.enter_context(SBT(nc, "NIU", [128, 128], BF16))
    P.dma("pool", negs[:], negs_d[:, :, :], w=[C.dconst])
    P.op("pool", lambda e: e.memset(NIU[:], -1.0), w=[C.dconst])
    P.op("pool", lambda e: e.affine_select(out=NIU[:], in_=NIU[:], pattern=[[-1, 128]], compare_op=OP.is_ge, fill=0.0,
                                           base=0, channel_multiplier=1), r=[C.dconst], w=[C.dconst])
    ot = [top.enter_context(SBT(nc, "ot%d" % i, [128, 4, 64], F32)) for i in range(2)]
    dot = deps(2)
    otn = 0

    with ExitStack() as stk:
        qk = stk.enter_context(SBT(nc, "qk", [128, 2, S], BF16))
        vsb = stk.enter_context(SBT(nc, "vsb", [128, NT, 128], BF16))
        dqk, dv = Dep(), Dep()

        def fm_evac(g, ch, bk, dbk):
            sc = 0.125 if g == 0 else 1.0
            P.op("act", lambda e: e.activation(out=qk[:, g, ch * 512:(ch + 1) * 512], in_=bk[:], func=AF.Copy, scale=sc),
                 r=[dbk], w=[dqk])

        def tm_evac(tt, bk, dbk):
            P.op("dve", lambda e: e.tensor_copy(out=vsb[:, tt, :], in_=bk[:, 0:128]), r=[dbk], w=[dv])

        prenorm_proj(C, stk, S, x, srow, shrow, dmod, wfm_sb, 256, wtm_sb, 128, fm_evac, tm_evac)
        if DEBUG:
            dq = nc.dram_tensor("dbg_qk", [128, 2, S], BF16, kind="ExternalOutput").ap()
            dvv = nc.dram_tensor("dbg_v", [128, NT, 128], BF16, kind="ExternalOutput").ap()
            dmo = nc.dram_tensor("dbg_mod", [128, 2048], F32, kind="ExternalOutput").ap()
            P.dma("sp", dq[:, :, :], qk[:], r=[dqk])
            P.dma("sp", dvv[:, :, :], vsb[:], r=[dv])
            P.dma("sp", dmo[:, :], mod[:], r=[dmod])

        NB = 3
        nones = stk.enter_context(SBT(nc, "sb_nones", [128, 128], BF16))
        P.op("pool", lambda e: e.memset(nones[:], -1.0), w=[C.dconst])
        esb = [stk.enter_context(SBT(nc, "sb_e%d" % i, [128, 2, 512], F32)) for i in range(2)]
        spb = [stk.enter_context(SBT(nc, "sb_sp%d" % i, [128, 2, 512], BF16)) for i in range(NB)]
        wb = [stk.enter_context(SBT(nc, "sb_w%d" % i, [128, 2, 512], BF16)) for i in range(NB)]
        ssum = stk.enter_context(SBT(nc, "sb_ss", [128, 2, 512], F32))
        sbf = [stk.enter_context(SBT(nc, "sb_sb%d" % i, [128, 2, 512], BF16)) for i in range(3)]
        de, dsp, dwb, dsbf = deps(2), deps(NB), deps(NB), deps(3)
        dss = Dep()
        dpair = deps(3)
        psall = C.psall
        otc = [otn]
        steps = []
        cnt = 0
        for qc in range(NQC):
            for si, kb in enumerate(range(4 * qc + 3, -1, -1)):
                k3 = cnt % NB
                k2 = cnt % 2
                pk = (cnt - 1) % 3
                ck = cnt % 3
                cnt += 1
                first = si == 0
                last = kb == 0
                i0 = max(0, kb - 4 * qc)
                c0 = 128 * i0
                cs = slice(c0, 512)
                qs = slice(qc * 512 + c0, (qc + 1) * 512)
                diag = kb >= 4 * qc

                def stA(k3=k3, k2=k2, first=first, last=last, c0=c0, cs=cs, qs=qs, diag=diag, kb=kb, ck=ck):
                    pair = psall[:, 2 * k3:2 * k3 + 2, :]
                    if first:
                        P.op("pool", lambda e: e.memset(ssum[:].rearrange("p h c -> p (h c)"), 0.0), w=[dss])
                    for h in range(2):
                        hp = slice(h * 64, (h + 1) * 64)
                        P.op("pe", lambda e, h=h, hp=hp: e.matmul(pair[:, h, cs], lhsT=qk[hp, 1, kb * 128:(kb + 1) * 128], rhs=qk[hp, 0, qs], start=True, stop=not diag),
                             r=[dqk], w=[dpair[k3]])
                        if diag:
                            P.op("pe", lambda e, h=h: e.matmul(pair[:, h, c0:c0 + 128], lhsT=C.ident[:], rhs=negs[:, 0, :], start=False, stop=True, skip_group_check=True),
                                 r=[C.dconst], w=[dpair[k3]])
                    P.op("act", lambda e: e.activation(out=esb[k2][:, :, cs], in_=pair[:, :, cs], func=AF.Exp), r=[dpair[k3]], w=[de[k2]])
                    P.op("act", lambda e: e.activation(out=spb[k3][:, :, cs], in_=esb[k2][:, :, cs], func=AF.Ln, bias=1.0), r=[de[k2]], w=[dsp[k3]])
                    if not last:
                        P.op("pool", lambda e: e.tensor_tensor(out=ssum[:, :, cs], in0=spb[k3][:, :, cs], in1=ssum[:, :, cs], op=OP.add), r=[dsp[k3]], w=[dss])
                        P.op("dve", lambda e: e.tensor_copy(out=sbf[ck][:].rearrange("p h c -> p (h c)"), in_=ssum[:].rearrange("p h c -> p (h c)")), r=[dss], w=[dsbf[ck]])

                def stB(k3=k3, first=first, cs=cs, pk=pk):
                    pair = psall[:, 2 * k3:2 * k3 + 2, :]
                    for h in range(2):
                        P.op("pe", lambda e, h=h: e.matmul(pair[:, h, cs], lhsT=NIU[:], rhs=spb[k3][:, h, cs], start=False, stop=True, skip_group_check=True),
                             r=[dsp[k3], C.dconst], w=[dpair[k3]])
                        if not first:
                            P.op("pe", lambda e, h=h: e.matmul(pair[:, h, cs], lhsT=nones[:], rhs=sbf[pk][:, h, cs], start=False, stop=True, skip_group_check=True),
                                 r=[dsbf[pk], C.dconst], w=[dpair[k3]])
                    P.op("act", lambda e: e.activation(out=wb[k3][:, :, cs], in_=pair[:, :, cs], func=AF.Exp), r=[dpair[k3]], w=[dwb[k3]])

                def stC(k3=k3, first=first, last=last, i0=i0, kb=kb, qc=qc):
                    for h in range(2):
                        hp = slice(h * 64, (h + 1) * 64)
                        OB = 6 + h
                        for i in range(i0, 4):
                            P.op("pe", lambda e, i=i, h=h, hp=hp, OB=OB: e.matmul(bank[OB][:, i * 64:(i + 1) * 64], lhsT=wb[k3][:, h, i * 128:(i + 1) * 128], rhs=vsb[:, kb, hp],
                                                                               start=(first and i == i0), stop=(last and i == 3), skip_group_check=True),
                                 r=[dwb[k3], dv], w=[dbank[OB]])
                    if last:
                        for h in range(2):
                            OB = 6 + h
                            ob = otc[0] % 2
                            otc[0] += 1
                            P.op("dve", lambda e, ob=ob, OB=OB: e.tensor_copy(out=ot[ob][:].rearrange("p i c -> p (i c)"), in_=bank[OB][:, 0:256]), r=[dbank[OB]], w=[dot[ob]])
                            P.dma("sp", o[qc * 512:(qc + 1) * 512, h * 64:(h + 1) * 64].rearrange("(i p) c -> p i c", p=128), ot[ob][:], r=[dot[ob]], w=[C.do])

                steps.append((stA, stB, stC))
        run_pipeline(steps, 3)
        otn = otc[0]
    P.barrier()
    C.top = top
    C.ot, C.dot, C.otn = ot, dot, otn
    C.io = dict(x=x, o=o, srow=srow, shrow=shrow, dmod=dmod, negs=negs, wfm_n=wfm_n, wtm_n=wtm_n, w1_d=w1_d, posT_d=posT_d, w2_d=w2_d,
                rconst_d=rconst_d, wsel_d=wsel_d, negs_d=negs_d, tbg_d=tbg_d, tcg_d=tcg_d, chv_d=chv_d, negc_d=negc_d)
    C.S = S
    C.zown = [0, 1]
    return C


def build_A_nsa(C):
    from contextlib import ExitStack
    nc, P, S = C.nc, C.P, C.S
    bank, dbank = C.bank, C.dbank
    io = C.io
    NT, NQC = S // 128, S // 512
    NC_ = S // 16 - 1
    NCH = (NC_ + 127) // 128
    x, o, srow, shrow, dmod, negs = io["x"], io["o"], io["srow"], io["shrow"], io["dmod"], io["negs"]
    ot, dot = C.ot, C.dot
    stk = ExitStack()
    qn = stk.enter_context(SBT(nc, "qn", [128, 2, S], BF16))
    kk = stk.enter_context(SBT(nc, "kk", [128, 2, S], BF16))
    vs1 = stk.enter_context(SBT(nc, "vs1", [128, NT, 2, 65], BF16))
    gat = stk.enter_context(SBT(nc, "gat", [128, NT, 6], F32))
    kcT = stk.enter_context(SBT(nc, "kcT", [128, 512], BF16))
    Rc = stk.enter_context(SBT(nc, "Rc", [128, 4, 193], BF16))
    dqn, dkk, dvs, dgat, dkc, dRc = Dep(), Dep(), Dep(), Dep(), Dep(), Dep()
    P.op("pool", lambda e: e.memset(vs1[:].rearrange("p t b c -> p (t b c)"), 1.0), w=[dvs])
    P.op("pool", lambda e: e.memset(kcT[:], 0.0), w=[dkc])
    P.dma("pool", Rc[:], io["rconst_d"][:, :, :], w=[dRc])
    with ExitStack() as stk_raw:
        raw = stk_raw.enter_context(SBT(nc, "raw", [128, S], BF16))
        draw = Dep()

        def fm_evac(g, ch, bk, dbk):
            if g < 2:
                P.op("act", lambda e: e.activation(out=qn[:, g, ch * 512:(ch + 1) * 512], in_=bk[:], func=AF.Copy, scale=0.125), r=[dbk], w=[dqn])
            elif g == 2:
                P.op("act", lambda e: e.activation(out=raw[:, ch * 512:(ch + 1) * 512], in_=bk[:], func=AF.Copy), r=[dbk], w=[draw])
            else:
                P.op("act", lambda e: e.activation(out=kk[:, g - 3, ch * 512:(ch + 1) * 512], in_=bk[:], func=AF.Copy), r=[dbk], w=[dkk])

        def tm_evac(tt, bk, dbk):
            P.op("dve", lambda e: e.tensor_copy(out=vs1[:, tt, :, 0:64], in_=bk[:, 0:128].rearrange("p (b c) -> p b c", b=2)), r=[dbk], w=[dvs])
            P.op("act", lambda e: e.activation(out=gat[:, tt, :], in_=bk[:, 128:134], func=AF.Sigmoid), r=[dbk], w=[dgat])

        prenorm_proj(C, stk, S, x, srow, shrow, dmod, io["wfm_n"], 640, io["wtm_n"], 134, fm_evac, tm_evac)

        with ExitStack() as s2:
            w1 = s2.enter_context(SBT(nc, "w1", [128, 32, 256], BF16))
            posT = s2.enter_context(SBT(nc, "posT", [128, 32], BF16))
            w2 = s2.enter_context(SBT(nc, "w2", [128, 2, 2, 64], BF16))
            gT = s2.enter_context(SBT(nc, "gT", [128, 2, 2, 512], BF16))
            bv = s2.enter_context(SBT(nc, "bv", [128, 4], F32))
            t0 = s2.enter_context(SBT(nc, "cm_t0", [128, 512], F32))
            t1 = s2.enter_context(SBT(nc, "cm_t1", [128, 512], F32))
            dw1, dgT, dbv, dt0, dt1 = Dep(), Dep(), Dep(), Dep(), Dep()
            P.dma("pool", w1[:], io["w1_d"][:, :, :], w=[dw1])
            P.dma("pool", posT[:], io["posT_d"][:, :], w=[dw1])
            P.dma("pool", w2[:], io["w2_d"][:, :, :, :], w=[dw1])
            for kv in range(2):
                rp = slice(kv * 64, (kv + 1) * 64)
                for half in range(2):
                    hs = slice(half * 128, (half + 1) * 128)
                    for l in range(32):
                        P.op("pe", lambda e, l=l: e.matmul(bank[1][:, 0:1], lhsT=w1[rp, l, hs], rhs=posT[rp, l:l + 1], start=(l == 0), stop=(l == 31)),
                             r=[dw1], w=[dbank[1]])
                    ci = kv * 2 + half
                    P.op("dve", lambda e, ci=ci: e.tensor_copy(out=bv[:, ci:ci + 1], in_=bank[1][:, 0:1]), r=[dbank[1]], w=[dbv])
                    for l in range(32):
                        P.op("pe", lambda e, l=l: e.matmul(bank[0][:, 0:NC_], lhsT=w1[rp, l, hs], rhs=raw[rp, l:l + 16 * (NC_ - 1) + 1:16],
                                                           start=(l == 0), stop=(l == 31)), r=[dw1, draw], w=[dbank[0]])
                    P.op("act", lambda e, ci=ci: e.activation(out=t0[:, 0:NC_], in_=bank[0][:, 0:NC_], func=AF.Identity, bias=bv[:, ci:ci + 1]),
                         r=[dbank[0], dbv], w=[dt0])
                    P.op("dve", lambda e: e.tensor_tensor(out=t1[:, 0:NC_], in0=t0[:, 0:NC_], in1=t0[:, 0:NC_], op=OP.mult), r=[dt0], w=[dt1])
                    P.op("dve", lambda e: e.tensor_scalar(out=t1[:, 0:NC_], in0=t1[:, 0:NC_], scalar1=0.044715, scalar2=1.0, op0=OP.mult, op1=OP.add),
                         r=[dt1], w=[dt1])
                    P.op("dve", lambda e: e.tensor_tensor(out=t1[:, 0:NC_], in0=t1[:, 0:NC_], in1=t0[:, 0:NC_], op=OP.mult), r=[dt1, dt0], w=[dt1])
                    P.op("act", lambda e: e.activation(out=t1[:, 0:NC_], in_=t1[:, 0:NC_], func=AF.Sigmoid, scale=1.5957691216057308), r=[dt1], w=[dt1])
                    P.op("dve", lambda e, kv=kv, half=half: e.tensor_tensor(out=gT[:, kv, half, 0:NC_], in0=t1[:, 0:NC_], in1=t0[:, 0:NC_], op=OP.mult),
                         r=[dt1, dt0], w=[dgT])
            w2kd = s2.enter_context(SBT(nc, "w2kd", [128, 2, 128], BF16))
            dw2 = Dep()
            for half in range(2):
                for dup in range(2):
                    P.op("dve", lambda e, half=half, dup=dup: e.tensor_copy(out=w2kd[:, half, dup * 64:(dup + 1) * 64], in_=w2[:, 0, half, :]), r=[dw1], w=[dw2])
            for half in range(2):
                P.op("pe", lambda e, half=half: e.matmul(bank[2][:, 0:NC_], lhsT=w2kd[:, half, :], rhs=gT[:, 0, half, 0:NC_],
                                                         start=(half == 0), stop=(half == 1)), r=[dw2, dgT], w=[dbank[2]])
            P.op("act", lambda e: e.activation(out=kcT[:, 0:NC_], in_=bank[2][:, 0:NC_], func=AF.Copy), r=[dbank[2]], w=[dkc])
            for c in range(NCH):
                nv = min(128, NC_ - c * 128)
                for half in range(2):
                    P.op("pe", lambda e, half=half, c=c, nv=nv: e.matmul(bank[3][0:nv, 0:64], lhsT=gT[:, 1, half, c * 128:c * 128 + nv], rhs=w2[:, 1, half, :],
                                                                       start=(half == 0), stop=(half == 1)), r=[dw1, dgT], w=[dbank[3]])
                P.op("dve", lambda e, c=c, nv=nv: e.tensor_copy(out=Rc[0:nv, c, 0:64], in_=bank[3][0:nv, 0:64]), r=[dbank[3]], w=[dRc])
    P.barrier()

    EW = stk.enter_context(SBT(nc, "EW", [128, S], BF16))
    Tc = stk.enter_context(SBT(nc, "Tc", [128, 4, 5, 512], BF16))
    TB = stk.enter_context(SBT(nc, "TB", [128, 2, 2, 128], BF16))
    chv = stk.enter_context(SBT(nc, "chv", [128, 4], F32))
    wsel = stk.enter_context(SBT(nc, "wsel", [128, 255], F32))
    dK = Dep()
    P.dma("sp", chv[:], io["chv_d"][:, :], w=[dK])
    P.dma("sp", wsel[:], io["wsel_d"][:, :], w=[dK])
    P.op("pool", lambda e: e.memset(EW[:], -NEG), w=[dK])
    P.op("pool", lambda e: e.affine_select(out=EW[:], in_=EW[:], pattern=[[1, S]], compare_op=OP.is_ge, fill=0.0, base=0, channel_multiplier=-64),
         r=[dK], w=[dK])
    P.op("pool", lambda e: e.affine_select(out=EW[:], in_=EW[:], pattern=[[-1, S]], compare_op=OP.is_ge, fill=0.0, base=63, channel_multiplier=64),
         r=[dK], w=[dK])
    with ExitStack() as s3:
        tcf = s3.enter_context(SBT(nc, "tcf", [128, 5, 512], F32))
        ncf = s3.enter_context(SBT(nc, "ncf", [128, 5, 512], F32))
        tbf = s3.enter_context(SBT(nc, "tbf", [128, 2, 2, 128], F32))
        ngf = s3.enter_context(SBT(nc, "ngf", [128, 3, 128], F32))
        dtc, dnf, dtb = Dep(), Dep(), Dep()
        P.dma("sp", ncf[:], io["negc_d"][:, :, :], w=[dnf])
        P.dma("sp", ngf[:], io["negs_d"][:, :, :], w=[dnf])
        P.dma("sp", tbf[:], io["tbg_d"][:, :, :, :], w=[dtb])
        for z in range(4):
            P.dma("sp", tcf[:], io["tcg_d"][:, z, :, :], w=[dtc])
            P.op("dve", lambda e, z=z: e.scalar_tensor_tensor(out=Tc[:, z, :, :], in0=tcf[:], scalar=chv[:, z:z + 1], in1=ncf[:],
                                                              op0=OP.subtract, op1=OP.add), r=[dtc, dnf, dK], w=[dK])
        for zz in range(2):
            zc = C.zown[zz]
            P.op("dve", lambda e, zz=zz, zc=zc: e.scalar_tensor_tensor(out=TB[:, zz, 0, :], in0=tbf[:, zz, 0, :], scalar=chv[:, zc:zc + 1], in1=ngf[:, 1, :],
                                                                       op0=OP.subtract, op1=OP.add), r=[dtb, dnf, dK], w=[dK])
            P.op("dve", lambda e, zz=zz, zc=zc: e.tensor_scalar(out=TB[:, zz, 1, :], in0=tbf[:, zz, 1, :], scalar1=chv[:, zc:zc + 1], scalar2=None,
                                                                op0=OP.subtract), r=[dtb, dK], w=[dK])
    P.barrier()

    pb = [stk.enter_context(SBT(nc, "n_p%d" % i, [128, 512], BF16)) for i in range(2)]
    dpb = deps(2)
    imp = stk.enter_context(SBT(nc, "imp", [128, 4, 128], F32))
    onsa = stk.enter_context(SBT(nc, "onsa", [128, 2, 4, 64], F32))
    sm = stk.enter_context(SBT(nc, "n_sm", [128, 16], F32))
    sc = stk.enter_context(SBT(nc, "n_sc", [128, 128], F32))
    sc2 = stk.enter_context(SBT(nc, "n_sc2", [128, 128], F32))
    m8 = stk.enter_context(SBT(nc, "n_m8", [128, 16], F32))
    selb = [stk.enter_context(SBT(nc, "n_sel%d" % i, [128, 128], BF16)) for i in range(4)]
    dselb = deps(4)
    selT = stk.enter_context(SBT(nc, "n_selT", [128, 512], BF16))
    dimp, donsa, dsm, dsc, dsel, dselT = Dep(), Dep(), Dep(), Dep(), Dep(), Dep()
    pit = 0
    zown = C.zown
    otn = C.otn

    def finalize(OBv, zz, br, qc, first_branch):
        P.op("dve", lambda e: e.tensor_scalar(out=sm[:, 0:4], in0=OBv[:, :, 64], scalar1=1e-30, scalar2=None, op0=OP.max), r=[dbank_of[0]], w=[dsm])
        P.op("dve", lambda e: e.reciprocal(out=sm[:, 4:8], in_=sm[:, 0:4]), r=[dsm], w=[dsm])
        P.op("dve", lambda e: e.tensor_tensor(out=sm[:, 8:12], in0=sm[:, 4:8], in1=gat[:, 4 * qc:4 * qc + 4, zz * 3 + br], op=OP.mult), r=[dsm, dgat], w=[dsm])
        for i in range(4):
            if first_branch:
                P.op("dve", lambda e, i=i: e.tensor_scalar(out=onsa[:, zz, i, :], in0=OBv[:, i, 0:64], scalar1=sm[:, 8 + i:9 + i], scalar2=None, op0=OP.mult),
                     r=[dbank_of[0], dsm], w=[donsa])
            else:
                P.op("dve", lambda e, i=i: e.scalar_tensor_tensor(out=onsa[:, zz, i, :], in0=OBv[:, i, 0:64], scalar=sm[:, 8 + i:9 + i], in1=onsa[:, zz, i, :],
                                                                  op0=OP.mult, op1=OP.add), r=[dbank_of[0], dsm], w=[donsa])

    dbank_of = [None]
    otT = stk.enter_context(SBT(nc, "n_otT", [128, 512], F32))
    dotT = Dep()
    pb3 = stk.enter_context(SBT(nc, "n_p2", [128, 512], BF16))
    pb = [pb[0], pb[1], pb3]
    dpb = dpb + [Dep()]
    otc = [otn]
    for qc in range(NQC):
        qsl = slice(qc * 512, (qc + 1) * 512)
        cmax = min(NCH - 1, qc // 4)
        steps = []
        for z in range(4):
            zp = slice((z % 2) * 64, (z % 2) * 64 + 64)
            O0 = 4 + 2 * (z % 2)
            for c in range(cmax + 1):
                b = pit % 3
                A = pit % 4
                pit += 1
                dl = qc - 4 * c
                near = 0 <= dl <= 4

                def stA(z=z, zp=zp, c=c, b=b, A=A, dl=dl, near=near, qsl=qsl):
                    P.op("pe", lambda e: e.matmul(bank[A][:], lhsT=kcT[zp, c * 128:(c + 1) * 128], rhs=qn[zp, z // 2, qsl], start=True, stop=not near),
                         r=[dkc, dqn], w=[dbank[A]])
                    if near:
                        P.op("pe", lambda e: e.matmul(bank[A][:], lhsT=C.ident[:], rhs=Tc[:, z, dl, :], start=False, stop=True), r=[dK, C.dconst], w=[dbank[A]])
                    P.op("act", lambda e: e.activation(out=pb[b][:], in_=bank[A][:], func=AF.Exp, bias=chv[:, z:z + 1]), r=[dbank[A], dK], w=[dpb[b]])

                def stB(z=z, c=c, b=b, O0=O0, cmax=cmax, qc=qc):
                    for i in range(4):
                        ob = O0 + i // 2
                        P.op("pe", lambda e, i=i, ob=ob: e.matmul(bank[ob][:, (i % 2) * 193:(i % 2) * 193 + 193], lhsT=pb[b][:, i * 128:(i + 1) * 128], rhs=Rc[:, c, :],
                                                                  start=(c == 0 and i % 2 == 0), stop=(c == cmax and i % 2 == 1), skip_group_check=True),
                             r=[dpb[b], dRc], w=[dbank[ob]])
                    if c != cmax:
                        return
                    own = z in zown
                    for i in range(4):
                        ob = O0 + i // 2
                        v = bank[ob][:, (i % 2) * 193:(i % 2) * 193 + 193]
                        P.op("dve", lambda e, v=v, i=i: e.tensor_scalar(out=sm[:, i:i + 1], in0=v[:, 64:65], scalar1=1e-30, scalar2=None, op0=OP.max), r=[dbank[ob]], w=[dsm])
                        P.op("dve", lambda e, i=i: e.reciprocal(out=sm[:, 4 + i:5 + i], in_=sm[:, i:i + 1]), r=[dsm], w=[dsm])
                        if z == 0:
                            P.op("dve", lambda e, v=v, i=i: e.tensor_scalar(out=imp[:, i, :], in0=v[:, 65:193], scalar1=sm[:, 4 + i:5 + i], scalar2=None, op0=OP.mult),
                                 r=[dbank[ob], dsm], w=[dimp])
                        else:
                            P.op("dve", lambda e, v=v, i=i: e.scalar_tensor_tensor(out=imp[:, i, :], in0=v[:, 65:193], scalar=sm[:, 4 + i:5 + i], in1=imp[:, i, :],
                                                                                   op0=OP.mult, op1=OP.add), r=[dbank[ob], dsm], w=[dimp])
                        if own:
                            zz = zown.index(z)
                            P.op("dve", lambda e, i=i, zz=zz: e.tensor_tensor(out=sm[:, 8 + i:9 + i], in0=sm[:, 4 + i:5 + i], in1=gat[:, 4 * qc + i, zz * 3:zz * 3 + 1], op=OP.mult),
                                 r=[dsm, dgat], w=[dsm])
                            P.op("dve", lambda e, v=v, i=i, zz=zz: e.tensor_scalar(out=onsa[:, zz, i, :], in0=v[:, 0:64], scalar1=sm[:, 8 + i:9 + i], scalar2=None, op0=OP.mult),
                                 r=[dbank[ob], dsm], w=[donsa])

                steps.append((stA, stB))
        run_pipeline(steps, 2)
        for i in range(4):
            qb = 4 * qc + i
            P.op("dve", lambda e, i=i, qb=qb: e.tensor_tensor(out=sc[:], in0=imp[:, i, :], in1=wsel[:, 127 - 2 * qb:255 - 2 * qb], op=OP.add), r=[dimp, dK], w=[dsc])
            if qb >= 1:
                P.op("dve", lambda e: e.tensor_scalar(out=sc[:, 0:1], in0=sc[:, 0:1], scalar1=1e4, scalar2=None, op0=OP.add), r=[dsc], w=[dsc])
            P.op("dve", lambda e: e.max(out=m8[:, 0:8], in_=sc[:]), r=[dsc], w=[dsm])
            P.op("dve", lambda e: e.match_replace(out=sc2[:], in_to_replace=m8[:, 0:8], in_values=sc[:], imm_value=-1e9), r=[dsc, dsm], w=[dsc])
            P.op("dve", lambda e: e.max(out=m8[:, 8:16], in_=sc2[:]), r=[dsc], w=[dsm])
            P.op("dve", lambda e: e.tensor_scalar(out=m8[:, 15:16], in0=m8[:, 15:16], scalar1=0.0, scalar2=None, op0=OP.max), r=[dsm], w=[dsm])
            P.op("dve", lambda e, i=i: e.tensor_scalar(out=selb[i][:], in0=sc[:], scalar1=m8[:, 15:16], scalar2=None, op0=OP.is_ge), r=[dsc, dsm], w=[dselb[i]])

        def sel_transposes():
            for i in range(4):
                pT = bank[0][:].bitcast(BF16)
                P.op("pe", lambda e, pT=pT, i=i: e.transpose(out=pT[:, 0:128], in_=selb[i][:], identity=C.ident[:]), r=[dselb[i], C.dconst], w=[dbank[0]])
                P.op("dve", lambda e, pT=pT, i=i: e.tensor_scalar(out=selT[:, i * 128:(i + 1) * 128], in0=pT[:, 0:128], scalar1=-1.0, scalar2=None, op0=OP.add),
                     r=[dbank[0]], w=[dselT])

        for br in (2, 1):
            steps = []
            if br == 1:
                sel_transposes()
            klo = 0 if br == 1 else max(0, 4 * qc - 4)
            for kb in range(klo, 4 * qc + 4):
                for zz in range(2):
                    z = zown[zz]
                    zp = slice((z % 2) * 64, (z % 2) * 64 + 64)
                    OB = (6 if br == 1 else 4) + zz
                    i0 = max(0, kb - 4 * qc)
                    i1 = 3 if br == 1 else min(3, kb - 4 * qc + 4)
                    c0, c1 = 128 * i0, 128 * (i1 + 1)
                    cs = slice(c0, c1)
                    b = pit % 3
                    A = pit % 4
                    pit += 1
                    extra = []
                    if br == 1:
                        extra.append((cs, EW[:, kb * 128:(kb + 1) * 128], selT[:, cs], [dK, dselT]))
                    for i in range(i0, i1 + 1):
                        d = kb - (4 * qc + i)
                        isl = slice(i * 128, (i + 1) * 128)
                        if d == 0:
                            extra.append((isl, C.ident[:], TB[:, zz, 0, :], [dK, C.dconst]))
                        elif d == -1:
                            extra.append((isl, C.ident[:], TB[:, zz, 1, :], [dK, C.dconst]))
                        elif d == -4 and br == 2:
                            extra.append((isl, C.ident[:], negs[:, 2, :], [C.dconst]))
                    firstk = kb == klo
                    lastk = kb == 4 * qc + 3

                    def stA(z=z, zp=zp, br=br, kb=kb, cs=cs, c0=c0, c1=c1, A=A, b=b, extra=extra, qc=qc):
                        P.op("pe", lambda e: e.matmul(bank[A][:, cs], lhsT=kk[zp, br - 1, kb * 128:(kb + 1) * 128], rhs=qn[zp, z // 2, qc * 512 + c0:qc * 512 + c1],
                                                      start=True, stop=(len(extra) == 0)), r=[dkk, dqn], w=[dbank[A]])
                        for xi, (sl, lt, rh, dd) in enumerate(extra):
                            P.op("pe", lambda e, sl=sl, lt=lt, rh=rh, lastx=(xi == len(extra) - 1): e.matmul(bank[A][:, sl], lhsT=lt, rhs=rh, start=False, stop=lastx,
                                                                                                           skip_group_check=True), r=dd, w=[dbank[A]])
                        P.op("act", lambda e: e.activation(out=pb[b][:, cs], in_=bank[A][:, cs], func=AF.Exp, bias=chv[:, z:z + 1]), r=[dbank[A], dK], w=[dpb[b]])

                    tbk = None
                    if lastk:
                        tbk = pit % 4
                        pit += 1

                    def stB(zz=zz, br=br, kb=kb, cs=cs, b=b, OB=OB, firstk=firstk, lastk=lastk, qc=qc, tbk=tbk):
                        P.op("pe", lambda e: e.matmul(bank[OB][0:65, cs], lhsT=vs1[:, kb, br - 1, :], rhs=pb[b][:, cs], start=firstk, stop=lastk, skip_group_check=True),
                             r=[dpb[b], dvs], w=[dbank[OB]])
                        if not lastk:
                            return
                        P.op("act", lambda e: e.activation(out=otT[0:65, :], in_=bank[OB][0:65, :], func=AF.Copy), r=[dbank[OB]], w=[dotT])
                        for i in range(4):
                            P.op("pe", lambda e, i=i: e.transpose(out=bank[tbk][:, i * 65:(i + 1) * 65], in_=otT[0:65, i * 128:(i + 1) * 128], identity=C.identf[0:65, 0:65]),
                                 r=[dotT, C.dconst], w=[dbank[tbk]])
                        dbank_of[0] = dbank[tbk]
                        finalize(bank[tbk][:, 0:260].rearrange("p (i c) -> p i c", c=65), zz, br, qc, False)
                        if br == 1:
                            ob = otc[0] % 2
                            otc[0] += 1
                            P.op("act", lambda e: e.activation(out=ot[ob][:], in_=onsa[:, zz, :, :], func=AF.Copy), r=[donsa], w=[dot[ob]])
                            P.dma("sp", o[qc * 512:(qc + 1) * 512, 128 + zz * 64:128 + (zz + 1) * 64].rearrange("(i p) c -> p i c", p=128), ot[ob][:],
                                  r=[dot[ob]], w=[C.do])
                            if zz == 1 and qc % 2 == 1 and C.og is not None:
                                c_ = qc // 2
                                P.coll("AllGather", o[c_ * 1024:(c_ + 1) * 1024, :], C.og[c_ * 4096:(c_ + 1) * 4096, :], GROUPS, r=[C.do], w=[C.dog])

                    steps.append((stA, stB))
            run_pipeline(steps, 2)
    stk.close()
    C.top.close()
    P.barrier()


import math

D_MODEL = 1024
W_SB = 512
OFF_SB_Q, OFF_SB_K, OFF_SB_V, OFF_NSA_Q = 0, 512, 1024, 1536
OFF_NSA_KV = 2048
OFF_NSA_GATE = OFF_NSA_KV + 3 * 2 * 2 * 64
IN_COLS = OFF_NSA_GATE + 24


def t5_bucket_np(dist):
    n = np.maximum(dist, 0)
    large = 16 + (np.log(np.maximum(n, 1).astype(np.float32) / np.float32(16)) / np.float32(math.log(128 / 16))
                  * np.float32(16)).astype(np.int32)
    large = np.minimum(large, 31)
    return np.where(n < 16, n, large)


_CONST_CACHE = {}


def consts_A(S):
    if S in _CONST_CACHE:
        return _CONST_CACHE[S]
    NC_ = S // 16 - 1
    k = np.arange(128)[:, None]
    q = np.arange(128)[None, :]
    negs = np.zeros((128, 3, 128), np.float32)
    negs[:, 0, :] = np.where(q > k, 0.0, NEG)
    negs[:, 1, :] = np.where(q >= k, 0.0, NEG)
    negs[:, 2, :] = np.where(q < k, 0.0, NEG)
    idx_tb = np.zeros((128, 2, 128), np.int64)
    idx_tb[:, 0, :] = t5_bucket_np(np.maximum(q - k, 0))
    idx_tb[:, 1, :] = t5_bucket_np(128 + q - k)
    q5 = np.arange(512)[None, None, :]
    dl = np.arange(5)[None, :, None]
    n_ = np.arange(128)[:, None, None]
    dist_c = 512 * dl + q5 - 16 * n_ - 31
    idx_tc = t5_bucket_np(np.maximum(dist_c, 0))
    negc = np.where(dist_c >= 0, 0.0, NEG).astype(np.float32)
    t = np.arange(128)[:, None]
    jp = np.arange(255)[None, :] - 127
    cur = (t >= 64).astype(np.int64)
    wsel = np.zeros((128, 255), np.float32)
    wsel[(jp == cur) | (jp == cur - 1)] = 1e4
    wsel[jp > cur] = -1.0
    rconst = np.zeros((128, 4, 193), np.float32)
    for c in range(4):
        n = c * 128 + np.arange(128)
        valid = n < NC_
        rconst[:, c, 64] = valid
        j = np.arange(128)[None, :]
        ov = (n[:, None] >= 4 * j - 1) & (n[:, None] <= 4 * j + 3) & valid[:, None]
        rconst[:, c, 65:193] = ov
    out = dict(negs=negs, idx_tb=idx_tb, idx_tc=idx_tc, negc=negc, wsel=wsel, rconst=rconst)
    _CONST_CACHE[S] = out
    return out


def prep_A(inp, layer, b, hg, S):
    cs = consts_A(S)
    g = hg // 2
    zo = [2 * (hg % 2), 2 * (hg % 2) + 1]
    L = zo + [z for z in range(4) if z not in zo]
    w_in = inp["w_in"][layer]
    hs = [2 * hg, 2 * hg + 1]

    def cols(off, n=64):
        return list(range(off, off + n))

    c_sb_fm = sum([cols(OFF_SB_Q + h * 64) for h in hs] + [cols(OFF_SB_K + h * 64) for h in hs], [])
    c_sb_tm = sum([cols(OFF_SB_V + h * 64) for h in hs], [])

    def kvcol(br, kvi):
        return cols(OFF_NSA_KV + ((br * 2 + kvi) * 2 + g) * 64)

    c_n_fm = sum([cols(OFF_NSA_Q + (g * 4 + z) * 64) for z in L], []) + kvcol(0, 0) + kvcol(0, 1) + kvcol(1, 0) + kvcol(1, 0) + kvcol(2, 0) + kvcol(2, 0)
    c_n_tm = kvcol(1, 1) + kvcol(2, 1) + sum([cols(OFF_NSA_GATE + (g * 4 + z) * 3, 3) for z in zo], [])
    heads = [g * 4 + z for z in L]
    tab = inp["rel_table"]
    tbg = np.stack([tab[cs["idx_tb"], heads[zz]] for zz in range(2)], axis=1)
    tcg = np.stack([tab[cs["idx_tc"], heads[z]] for z in range(4)], axis=1)
    chv = np.broadcast_to(tab[31, heads][None, :], (128, 4))
    w1 = np.concatenate([inp["cmp_w1_k"][layer].reshape(32, 64, 256).transpose(1, 0, 2),
                         inp["cmp_w1_v"][layer].reshape(32, 64, 256).transpose(1, 0, 2)], axis=0)
    posT = np.concatenate([inp["cmp_pos_k"][layer].T, inp["cmp_pos_v"][layer].T], axis=0)
    w2 = np.stack([inp["cmp_w2_k"][layer].reshape(2, 128, 64).transpose(1, 0, 2),
                   inp["cmp_w2_v"][layer].reshape(2, 128, 64).transpose(1, 0, 2)], axis=1)
    f = lambda a: np.ascontiguousarray(a, dtype=np.float32)
    return {
        "cT": f(inp["c"][b].reshape(8, 128).T), "wada": f(inp["w_ada"][layer][:, 0:2048]),
        "bada": f(inp["b_ada"][layer][None, 0:2048]), "gpre": f(inp["g_pre_mix"][layer][None]),
        "wfm_sb": f(w_in[:, c_sb_fm]), "wtm_sb": f(w_in[:, c_sb_tm]), "wfm_n": f(w_in[:, c_n_fm]), "wtm_n": f(w_in[:, c_n_tm]),
        "w1": f(w1), "posT": f(posT), "w2": f(w2), "rconst": cs["rconst"], "wsel": cs["wsel"],
        "tbg": f(tbg), "tcg": f(tcg), "chv": f(chv), "negs": cs["negs"], "negc": cs["negc"],
    }


def build_B(C, L, NTOK, n_exp, dff, x, dxres, og, dog, idx_d, y, dy, din):
    from contextlib import ExitStack
    NT = NTOK // 128
    NFC = dff // 128
    G = 4
    moe = n_exp > 1
    nc = C.nc
    cT = din("cTb", [128, 8]); wada = din("wadab", [1024, 4096]); bada = din("badab", [1, 4096])
    grow_d = din("grow", [1, 1024]); gpm_d = din("gpm", [1, 1024]); gpf_d = din("gpf", [1, 1024]); gqf_d = din("gqf", [1, 1024])
    wout_d = din("wout", [1024, 1024])
    wg_d = din("wg", [n_exp, 1024, dff]); wu_d = din("wu", [n_exp, 1024, dff]); wd_d = din("wd", [n_exp, dff, 1024])
    if moe:
        wr_d = din("wr", [8, 1024]); br_d = din("br", [1, 8])
    x1s = nc.dram_tensor("x1s_%d" % L, [NTOK, 1024], F32).ap()
    P = C.P
    bank, dbank = C.bank, C.dbank
    top = ExitStack()
    epsb = top.enter_context(SBT(nc, "epsb", [128, 1], F32))
    P.op("pool", lambda e: e.memset(epsb[:], 1e-6), w=[C.dconst])
    mod = top.enter_context(SBT(nc, "mod", [128, 4096], F32))
    dmod = Dep()
    with ExitStack() as stk:
        ada_rows(C, stk, cT, wada, bada, 4096, mod, dmod)
    P.barrier()
    h2T = top.enter_context(SBT(nc, "h2T", [128, 8, NTOK], BF16))
    gw = top.enter_context(SBT(nc, "gw", [128, NT, 8], F32))
    dh2T, dgw, dx1s = Dep(), Dep(), Dep()
    gm = mod[:, 0:1024]; shf = mod[:, 1024:2048]; srf = mod[:, 2048:3072]; gf = mod[:, 3072:4096]
    with ExitStack() as stk:
        rows = stk.enter_context(SBT(nc, "rows", [128, 4, 1024], F32))
        drows = Dep()
        for i, d in enumerate((grow_d, gpm_d, gpf_d, gqf_d)):
            P.dma("sp", rows[:, i, :], d[0:1, 0:1024].to_broadcast([128, 1024]), w=[drows])
        P.op("dve", lambda e: e.tensor_tensor(out=gm, in0=gm, in1=rows[:, 1, :], op=OP.mult), r=[drows, dmod], w=[dmod])
        P.op("dve", lambda e: e.scalar_tensor_tensor(out=srf, in0=srf, scalar=1.0, in1=rows[:, 2, :], op0=OP.add, op1=OP.mult), r=[drows, dmod], w=[dmod])
        P.op("dve", lambda e: e.tensor_tensor(out=gf, in0=gf, in1=rows[:, 3, :], op=OP.mult), r=[drows, dmod], w=[dmod])
        wout = stk.enter_context(SBT(nc, "wout", [128, 8, 1024], BF16))
        dwo = Dep()
        P.dma("pool", wout[:], wout_d.rearrange("(kc p) n -> p kc n", p=128), w=[dwo])
        if moe:
            wrr = stk.enter_context(SBT(nc, "wrr", [128, 8, 1024], F32))
            brr = stk.enter_context(SBT(nc, "brr", [128, 8], F32))
            dwr = Dep()
            for e_ in range(8):
                P.dma("sp", wrr[:, e_, :], wr_d[e_:e_ + 1, 0:1024].to_broadcast([128, 1024]), w=[dwr])
            P.dma("sp", brr[:], br_d[0:1, 0:8].to_broadcast([128, 8]), w=[dwr])
        o2 = [stk.enter_context(SBT(nc, "o2_%d" % i, [128, 4, 256], F32)) for i in range(2)]
        idxs = stk.enter_context(SBT(nc, "idxs", [128, NT * 4], mybir.dt.int32))
        didx = Dep()
        P.dma("sp", idxs[:], idx_d[:, :], w=[didx])
        xt = [stk.enter_context(SBT(nc, "xt_%d" % i, [128, 1024], F32)) for i in range(2)]
        x1t = [stk.enter_context(SBT(nc, "x1t_%d" % i, [128, 1024], F32)) for i in range(2)]
        junk = stk.enter_context(SBT(nc, "junk", [128, 1024], BF16))
        junkf = stk.enter_context(SBT(nc, "junkf", [128, 1024], F32))
        tmp = stk.enter_context(SBT(nc, "tmp", [128, 1024], F32))
        h2f = stk.enter_context(SBT(nc, "h2f", [128, 1024], F32))
        mg = stk.enter_context(SBT(nc, "mg", [128, 1024], BF16))
        h2b = stk.enter_context(SBT(nc, "h2b", [128, 1024], BF16))
        mT = stk.enter_context(SBT(nc, "mT", [128, 8, 128], BF16))
        st = stk.enter_context(SBT(nc, "st", [128, 16], F32))
        lg = stk.enter_context(SBT(nc, "lg", [128, 32], F32))
        do2, dxt, dx1t = deps(2), deps(2), deps(2)
        djunk, djf, dtmp, dh2f, dmg, dh2b, dmT, dst_, dlg = (Dep() for _ in range(9))

        def rms(src_aps, n, col):
            nn = len(src_aps)
            for k_, (ap_, dd) in enumerate(src_aps):
                jo = junk[:, 0:n] if len(ap_.shape) == 2 else junk[:, 0:n].rearrange("p (r c) -> p r c", r=ap_.shape[1])
                P.op("act", lambda e, ap_=ap_, k_=k_, jo=jo: e.activation(out=jo, in_=ap_, func=AF.Square, accum_out=st[:, col + k_:col + k_ + 1]),
                     r=[dd], w=[djunk, dst_])
            P.op("act", lambda e: e.activation(out=st[:, col:col + nn], in_=st[:, col:col + nn], func=AF.Sqrt, scale=1.0 / n, bias=epsb[:, 0:1]),
                 r=[dst_, C.dconst], w=[dst_])
            P.op("dve", lambda e: e.reciprocal(out=st[:, col:col + nn], in_=st[:, col:col + nn]), r=[dst_], w=[dst_])

        steps = []
        for tt in range(NT):
            b = tt % 2
            tsl = slice(tt * 128, (tt + 1) * 128)
            wo = 2 * (tt % 2)

            def stA(tt=tt, b=b, tsl=tsl, wo=wo):
                for r_ in range(4):
                    P.dma("pool", o2[b][:, r_, :], None, r=[dog, didx], w=[do2[b]])
                    waits_, fn_, inc_ = P.q["pool"][-1]
                    P.q["pool"][-1] = (waits_, freeze(lambda e, r_=r_, b=b, tt=tt: e.indirect_dma_start(
                        out=o2[b][:, r_, :], out_offset=None, in_=og[:, :], in_offset=bass.IndirectOffsetOnAxis(ap=idxs[:, tt * 4 + r_:tt * 4 + r_ + 1], axis=0))), inc_)
                P.dma("sp", xt[b][:], x[tsl, :], r=[dxres], w=[dxt[b]])
                rms([(o2[b][:, :, 0:128], do2[b]), (o2[b][:, :, 128:256], do2[b])], 512, 0)
                for hf in range(2):
                    hs = slice(hf * 512, (hf + 1) * 512)
                    P.op("dve", lambda e, hf=hf, hs=hs, b=b: e.scalar_tensor_tensor(out=mg[:, hs].rearrange("p (r c) -> p r c", r=4), in0=o2[b][:, :, hf * 128:(hf + 1) * 128], scalar=st[:, hf:hf + 1],
                                                                                   in1=rows[:, 0, hs].rearrange("p (r c) -> p r c", r=4),
                                                                                   op0=OP.mult, op1=OP.mult), r=[do2[b], dst_, drows], w=[dmg])
                pT = bank[6][:].bitcast(BF16)
                for kc in range(8):
                    P.op("pe", lambda e, kc=kc, pT=pT: e.transpose(out=pT[:, kc * 128:(kc + 1) * 128], in_=mg[:, kc * 128:(kc + 1) * 128], identity=C.ident[:]),
                         r=[dmg, C.dconst], w=[dbank[6]])
                P.op("act", lambda e, pT=pT: e.activation(out=mT[:], in_=pT.rearrange("p (k t) -> p k t", k=8), func=AF.Copy), r=[dbank[6]], w=[dmT])
                for hf in range(2):
                    for kc in range(8):
                        P.op("pe", lambda e, kc=kc, hf=hf: e.matmul(bank[hf + wo][:], lhsT=mT[:, kc, :], rhs=wout[:, kc, hf * 512:(hf + 1) * 512], start=(kc == 0), stop=(kc == 7)),
                             r=[dmT, dwo], w=[dbank[hf + wo]])

            def stB(tt=tt, b=b, tsl=tsl, wo=wo):
                for hf in range(2):
                    P.op("act", lambda e, hf=hf: e.activation(out=junk[:, 0:512], in_=bank[hf + wo][:], func=AF.Square, accum_out=st[:, 4 + hf:5 + hf]), r=[dbank[hf + wo]], w=[djunk, dst_])
                P.op("dve", lambda e: e.tensor_tensor(out=st[:, 6:7], in0=st[:, 4:5], in1=st[:, 5:6], op=OP.add), r=[dst_], w=[dst_])
                P.op("act", lambda e: e.activation(out=st[:, 6:7], in_=st[:, 6:7], func=AF.Sqrt, scale=1.0 / 1024, bias=epsb[:, 0:1]), r=[dst_, C.dconst], w=[dst_])
                P.op("dve", lambda e: e.reciprocal(out=st[:, 6:7], in_=st[:, 6:7]), r=[dst_], w=[dst_])
                for hf in range(2):
                    hs = slice(hf * 512, (hf + 1) * 512)
                    P.op("dve", lambda e, hf=hf, hs=hs: e.scalar_tensor_tensor(out=tmp[:, hs], in0=bank[hf + wo][:], scalar=st[:, 6:7], in1=gm[:, hs], op0=OP.mult, op1=OP.mult),
                         r=[dbank[hf + wo], dst_, dmod], w=[dtmp])
                P.op("dve", lambda e, b=b: e.tensor_tensor(out=x1t[b][:], in0=tmp[:], in1=xt[b][:], op=OP.add), r=[dtmp, dxt[b]], w=[dx1t[b]])
                P.dma("sp", x1s[tsl, :], x1t[b][:], r=[dx1t[b]], w=[dx1s])
                rms([(x1t[b][:], dx1t[b])], 1024, 8)
                P.op("dve", lambda e, b=b: e.scalar_tensor_tensor(out=tmp[:], in0=x1t[b][:], scalar=st[:, 8:9], in1=srf, op0=OP.mult, op1=OP.mult),
                     r=[dx1t[b], dst_, dmod], w=[dtmp])
                P.op("dve", lambda e: e.tensor_tensor(out=h2f[:], in0=tmp[:], in1=shf, op=OP.add), r=[dtmp, dmod], w=[dh2f])
                P.op("act", lambda e: e.activation(out=h2b[:], in_=h2f[:], func=AF.Copy), r=[dh2f], w=[dh2b])
                pT2 = bank[7][:].bitcast(BF16)
                for kc in range(8):
                    P.op("pe", lambda e, kc=kc, pT2=pT2: e.transpose(out=pT2[:, kc * 128:(kc + 1) * 128], in_=h2b[:, kc * 128:(kc + 1) * 128], identity=C.ident[:]),
                         r=[dh2b, C.dconst], w=[dbank[7]])
                P.op("act", lambda e, pT2=pT2, tsl=tsl: e.activation(out=h2T[:, :, tsl], in_=pT2.rearrange("p (k t) -> p k t", k=8), func=AF.Copy), r=[dbank[7]], w=[dh2T])
                if moe:
                    for e_ in range(8):
                        P.op("dve", lambda e, e_=e_: e.scalar_tensor_tensor(out=junkf[:], in0=h2f[:], scalar=1.0, in1=wrr[:, e_, :], op0=OP.mult, op1=OP.mult,
                                                                            accum_out=lg[:, e_:e_ + 1]), r=[dh2f, dwr], w=[djf, dlg])
                    P.op("dve", lambda e: e.tensor_tensor(out=lg[:, 0:8], in0=lg[:, 0:8], in1=brr[:], op=OP.add), r=[dlg, dwr], w=[dlg])
                    P.op("dve", lambda e: e.max(out=lg[:, 8:16], in_=lg[:, 0:8]), r=[dlg], w=[dlg])
                    P.op("dve", lambda e: e.tensor_scalar(out=lg[:, 16:24], in0=lg[:, 0:8], scalar1=lg[:, 8:9], scalar2=None, op0=OP.subtract), r=[dlg], w=[dlg])
                    P.op("act", lambda e: e.activation(out=lg[:, 16:24], in_=lg[:, 16:24], func=AF.Exp), r=[dlg], w=[dlg])
                    P.op("dve", lambda e: e.scalar_tensor_tensor(out=lg[:, 16:24], in0=lg[:, 0:8], scalar=lg[:, 9:10], in1=lg[:, 16:24], op0=OP.is_ge, op1=OP.mult,
                                                                 accum_out=lg[:, 24:25]), r=[dlg], w=[dlg])
                    P.op("dve", lambda e: e.reciprocal(out=lg[:, 25:26], in_=lg[:, 24:25]), r=[dlg], w=[dlg])
                    P.op("dve", lambda e, tt=tt: e.tensor_scalar(out=gw[:, tt, :], in0=lg[:, 16:24], scalar1=lg[:, 25:26], scalar2=None, op0=OP.mult), r=[dlg], w=[dgw])

            steps.append((stA, stB))
        run_pipeline(steps, 2)
    P.barrier()

    acc = top.enter_context(SBT(nc, "acc", [128, NT, 1024], F32))
    dacc = Dep()
    with ExitStack() as stk:
        wgb = [stk.enter_context(SBT(nc, "wg%d" % i, [128, 8, G * 128], BF16)) for i in range(2)]
        wub = [stk.enter_context(SBT(nc, "wu%d" % i, [128, 8, G * 128], BF16)) for i in range(2)]
        wdb = [stk.enter_context(SBT(nc, "wd%d" % i, [128, G, 1024], BF16)) for i in range(2)]
        sg = [stk.enter_context(SBT(nc, "sg%d" % i, [128, 256], F32)) for i in range(2)]
        aT = [stk.enter_context(SBT(nc, "aT%d" % i, [128, 256], BF16)) for i in range(2)]
        dwb, dsg, daT = deps(2), deps(2), deps(2)
        groups_ = [(ex, g0, min(G, NFC - g0)) for ex in range(n_exp) for g0 in range(0, NFC, G)]

        def load_group(gi):
            ex, g0, ng = groups_[gi]
            wb_ = gi % 2
            cs = slice(g0 * 128, (g0 + ng) * 128)
            P.dma("pool", wgb[wb_][:, :, 0:ng * 128], wg_d[ex, :, cs].rearrange("(kc p) n -> p kc n", p=128), w=[dwb[wb_]])
            P.dma("pool", wub[wb_][:, :, 0:ng * 128], wu_d[ex, :, cs].rearrange("(kc p) n -> p kc n", p=128), w=[dwb[wb_]])
            P.dma("pool", wdb[wb_][:, 0:ng, :], wd_d[ex, cs, :].rearrange("(f p) n -> p f n", p=128), w=[dwb[wb_]])

        load_group(0)
        steps = []
        it = 0
        for gi, (ex, g0, ng) in enumerate(groups_):
            wb_ = gi % 2
            firstgrp = gi == 0
            for tc_ in range(NTOK // 256):
                tks = slice(tc_ * 256, (tc_ + 1) * 256)
                for f in range(ng):
                    b = it % 2
                    it += 1
                    GB, UB = b, 2 + b
                    pre = gi + 1 if (tc_ == 0 and f == 0 and gi + 1 < len(groups_)) else None

                    def stA(f=f, b=b, GB=GB, UB=UB, wb_=wb_, tks=tks):
                        for kc in range(8):
                            P.op("pe", lambda e, kc=kc: e.matmul(bank[GB][:, 0:256], lhsT=wgb[wb_][:, kc, f * 128:(f + 1) * 128], rhs=h2T[:, kc, tks],
                                                                 start=(kc == 0), stop=(kc == 7)), r=[dwb[wb_], dh2T], w=[dbank[GB]])
                        for kc in range(8):
                            P.op("pe", lambda e, kc=kc: e.matmul(bank[UB][:, 0:256], lhsT=wub[wb_][:, kc, f * 128:(f + 1) * 128], rhs=h2T[:, kc, tks],
                                                                 start=(kc == 0), stop=(kc == 7)), r=[dwb[wb_], dh2T], w=[dbank[UB]])
                        P.op("act", lambda e: e.activation(out=sg[b][:], in_=bank[GB][:, 0:256], func=AF.Silu), r=[dbank[GB]], w=[dsg[b]])
                        P.op("dve", lambda e: e.tensor_tensor(out=aT[b][:], in0=bank[UB][:, 0:256], in1=sg[b][:], op=OP.mult), r=[dbank[UB], dsg[b]], w=[daT[b]])

                    def stB(f=f, b=b, wb_=wb_, ng=ng, tc_=tc_, ex=ex, firstgrp=firstgrp, pre=pre):
                        if pre is not None:
                            load_group(pre)
                        for sub in range(2):
                            for hf in range(2):
                                ob = 4 + sub * 2 + hf
                                P.op("pe", lambda e, sub=sub, hf=hf, ob=ob: e.matmul(
                                    bank[ob][:], lhsT=aT[b][:, sub * 128:(sub + 1) * 128], rhs=wdb[wb_][:, f, hf * 512:(hf + 1) * 512],
                                    start=(f == 0), stop=(f == ng - 1)), r=[daT[b], dwb[wb_]], w=[dbank[ob]])
                        if f != ng - 1:
                            return
                        for sub in range(2):
                            tt = tc_ * 2 + sub
                            for hf in range(2):
                                ob = 4 + sub * 2 + hf
                                hs = slice(hf * 512, (hf + 1) * 512)
                                if moe:
                                    if firstgrp:
                                        P.op("dve", lambda e, ob=ob, tt=tt, hs=hs: e.tensor_scalar(out=acc[:, tt, hs], in0=bank[ob][:], scalar1=gw[:, tt, ex:ex + 1], scalar2=None, op0=OP.mult),
                                             r=[dbank[ob], dgw], w=[dacc])
                                    else:
                                        P.op("dve", lambda e, ob=ob, tt=tt, hs=hs: e.scalar_tensor_tensor(out=acc[:, tt, hs], in0=bank[ob][:], scalar=gw[:, tt, ex:ex + 1], in1=acc[:, tt, hs],
                                                                                                        op0=OP.mult, op1=OP.add), r=[dbank[ob], dgw], w=[dacc])
                                else:
                                    if firstgrp:
                                        P.op("dve", lambda e, ob=ob, tt=tt, hs=hs: e.tensor_copy(out=acc[:, tt, hs], in_=bank[ob][:]), r=[dbank[ob]], w=[dacc])
                                    else:
                                        P.op("dve", lambda e, ob=ob, tt=tt, hs=hs: e.tensor_tensor(out=acc[:, tt, hs], in0=bank[ob][:], in1=acc[:, tt, hs], op=OP.add), r=[dbank[ob]], w=[dacc])

                    steps.append((stA, stB))
        run_pipeline(steps, 2)
    P.barrier()
    with ExitStack() as stk:
        xt = [stk.enter_context(SBT(nc, "fx%d" % i, [128, 1024], F32)) for i in range(2)]
        yt = [stk.enter_context(SBT(nc, "fy%d" % i, [128, 1024], F32)) for i in range(2)]
        junk = stk.enter_context(SBT(nc, "fjunk", [128, 1024], BF16))
        st = stk.enter_context(SBT(nc, "fst", [128, 4], F32))
        dxt, dyt = deps(2), deps(2)
        djunk, dst_ = Dep(), Dep()
        for tt in range(NT):
            b = tt % 2
            tsl = slice(tt * 128, (tt + 1) * 128)
            P.dma("sp", xt[b][:], x1s[tsl, :], r=[dx1s], w=[dxt[b]])
            P.op("act", lambda e, tt=tt: e.activation(out=junk[:], in_=acc[:, tt, :], func=AF.Square, accum_out=st[:, 0:1]), r=[dacc], w=[djunk, dst_])
            P.op("act", lambda e: e.activation(out=st[:, 1:2], in_=st[:, 0:1], func=AF.Sqrt, scale=1.0 / 1024, bias=epsb[:, 0:1]), r=[dst_, C.dconst], w=[dst_])
            P.op("dve", lambda e: e.reciprocal(out=st[:, 2:3], in_=st[:, 1:2]), r=[dst_], w=[dst_])
            P.op("dve", lambda e, tt=tt, b=b: e.scalar_tensor_tensor(out=yt[b][:], in0=acc[:, tt, :], scalar=st[:, 2:3], in1=gf, op0=OP.mult, op1=OP.mult),
                 r=[dacc, dst_, dmod], w=[dyt[b]])
            P.op("dve", lambda e, b=b: e.tensor_tensor(out=yt[b][:], in0=yt[b][:], in1=xt[b][:], op=OP.add), r=[dxt[b], dyt[b]], w=[dyt[b]])
            P.dma("sp", y[tsl, :], yt[b][:], r=[dyt[b]], w=[dy])
    top.close()
    P.barrier()


def prep_B(inp, layer, b):
    f = lambda a: np.ascontiguousarray(a, dtype=np.float32)
    m = {
        "cTb": f(inp["c"][b].reshape(8, 128).T), "wadab": f(inp["w_ada"][layer][:, 2048:6144]), "badab": f(inp["b_ada"][layer][None, 2048:6144]),
        "grow": f(np.concatenate([inp["g_sb"][layer], inp["g_nsa"][layer]])[None]), "gpm": f(inp["g_post_mix"][layer][None]),
        "gpf": f(inp["g_pre_ffn"][layer][None]), "gqf": f(inp["g_post_ffn"][layer][None]), "wout": f(inp["w_out"][layer]),
    }
    i = layer // 2
    if layer % 2 == 0:
        m["wg"] = f(inp["ffn_w_gate"][i][None]); m["wu"] = f(inp["ffn_w_up"][i][None]); m["wd"] = f(inp["ffn_w_down"][i][None])
    else:
        m["wg"] = f(inp["moe_w_gate"][i]); m["wu"] = f(inp["moe_w_up"][i]); m["wd"] = f(inp["moe_w_down"][i])
        m["wr"] = f(inp["moe_w_router"][i].T); m["br"] = f(inp["moe_b_router"][i][None])
    return m


_PROG_CACHE = {}
GROUPS = [[0, 1, 2, 3], [4, 5, 6, 7]]


def build_fused(S):
    NTOK = 2 * S // 8
    NT = NTOK // 128
    nc = bass.Bass("TRN2", target_bir_lowering=False)
    C = mk_ctx(nc)
    P = C.P

    def mk_din(L):
        def din(name, shape):
            return nc.dram_tensor("%s_%d" % (name, L), shape, F32, kind="ExternalInput").ap()
        return din

    xb = nc.dram_tensor("xb", [S, 1024], F32, kind="ExternalInput").ap()
    xtok = nc.dram_tensor("xtok", [NTOK, 1024], F32, kind="ExternalInput").ap()
    idx_d = nc.dram_tensor("idxg", [128, NT * 4], mybir.dt.int32, kind="ExternalInput").ap()
    y = nc.dram_tensor("y", [NTOK, 1024], F32, kind="ExternalOutput").ap()
    o0 = nc.dram_tensor("o0", [S, 256], F32).ap(); og0 = nc.dram_tensor("og0", [4 * S, 256], F32).ap()
    o1 = nc.dram_tensor("o1", [S, 256], F32).ap(); og1 = nc.dram_tensor("og1", [4 * S, 256], F32).ap()
    xo0 = nc.dram_tensor("xo0", [NTOK, 1024], F32).ap(); xg1 = nc.dram_tensor("xg1", [4 * NTOK, 1024], F32).ap()
    def gather_o(o, og, dsrc, ddst):
        for c in range(S // 1024):
            P.coll("AllGather", o[c * 1024:(c + 1) * 1024, :], og[c * 4096:(c + 1) * 4096, :], GROUPS, r=[dsrc], w=[ddst])

    dx0 = Dep()
    C.dxsrc = lambda tt: dx0
    C.xtile = lambda tt: xb[tt * 128:(tt + 1) * 128, :]
    dog0 = Dep()
    C.og, C.dog = og0, dog0
    build_A(C, 0, S, xb, o0, mk_din(0)); build_A_nsa(C)
    dxo0 = Dep()
    build_B(C, 0, NTOK, 1, 2816, xtok, Dep(), og0, dog0, idx_d, xo0, dxo0, mk_din(0))
    dxg1 = deps(NTOK // 256)
    for c in range(NTOK // 256):
        P.coll("AllGather", xo0[c * 256:(c + 1) * 256, :], xg1[c * 1024:(c + 1) * 1024, :], GROUPS, r=[dxo0], w=[dxg1[c]])
    C.dxsrc = lambda tt: dxg1[((tt * 128) % NTOK) // 256]

    def xtile1(tt):
        t = tt * 128
        rank, row = t // NTOK, t % NTOK
        c, rr = row // 256, row % 256
        r0 = c * 1024 + rank * 256 + rr
        return xg1[r0:r0 + 128, :]

    C.xtile = xtile1
    dog1 = Dep()
    C.og, C.dog = og1, dog1
    build_A(C, 1, S, xg1, o1, mk_din(1)); build_A_nsa(C)
    dy = Dep()
    build_B(C, 1, NTOK, 8, 3584, xo0, dxo0, og1, dog1, idx_d, y, dy, mk_din(1))
    P.finish("sp")
    P.emit()
    return nc


def kernel(**inp):
    inp = {k: np.asarray(v) for k, v in inp.items()}
    x = np.ascontiguousarray(inp["x"], dtype=np.float32)
    B, S, _ = x.shape
    NTOK = B * S // 8
    NT = NTOK // 128
    if S not in _PROG_CACHE:
        _PROG_CACHE[S] = build_fused(S)
    nc = _PROG_CACHE[S]
    maps = []
    for cid in range(8):
        b, part = cid // 4, cid % 4
        m = {"xb": x[b], "xtok": np.ascontiguousarray(x[b, part * NTOK:(part + 1) * NTOK])}
        p = np.arange(128)[:, None, None]
        tt = np.arange(NT)[None, :, None]
        r = np.arange(4)[None, None, :]
        t0 = part * NTOK + tt * 128
        m["idxg"] = ((t0 // 1024) * 4096 + r * 1024 + (t0 % 1024) + p).reshape(128, NT * 4).astype(np.int32)
        for L in range(2):
            for k, v in prep_A(inp, L, b, part, S).items():
                m["%s_%d" % (k, L)] = v
            for k, v in prep_B(inp, L, b).items():
                m["%s_%d" % (k, L)] = v
        maps.append(m)
    res = run_bass_kernel_spmd(nc, maps, core_ids=list(range(8)))
    out = np.zeros((B, S, 1024), np.float32)
    for cid in range(8):
        b, part = cid // 4, cid % 4
        out[b, part * NTOK:(part + 1) * NTOK] = res.results[cid]["y"]
    return out
```

```python
import numpy as np
import ml_dtypes
import concourse.bass as bass
import concourse.mybir as mybir
from concourse.bass_utils import run_bass_kernel_spmd

F32 = mybir.dt.float32
BF16 = mybir.dt.bfloat16
AF = mybir.ActivationFunctionType
OP = mybir.AluOpType
AX = mybir.AxisListType

ENGS = ("pe", "act", "dve", "pool", "sp")
NEG = -30000.0
DEBUG = False


_NM = [0]


def SBT(nc, name, shape, dt):
    _NM[0] += 1
    return nc.sbuf_tensor("t%d_%s" % (_NM[0], name), shape, dt)


import types


def freeze(fn):
    if fn.__closure__ is None:
        return fn
    cells = []
    for c in fn.__closure__:
        try:
            cells.append(types.CellType(c.cell_contents))
        except ValueError:
            cells.append(c)
    return types.FunctionType(fn.__code__, fn.__globals__, fn.__name__, fn.__defaults__, tuple(cells))


class Dep:
    __slots__ = ("lw", "rd")

    def __init__(self):
        self.lw = []
        self.rd = []


def deps(n):
    return [Dep() for _ in range(n)]


class Prog:
    def __init__(self, nc, ring=6):
        self.nc = nc
        self.q = {e: [] for e in ENGS}
        self.cnt = {e: 0 for e in ENGS}
        self.sems = {}
        self.waited = {e: {} for e in ENGS}
        self.ring = ring
        self.dma_n = {"sp": 0, "pool": 0}
        self.fence = []
        self.colls = []
        for e in ("pe", "act", "dve", "pool"):
            self.sems[e] = nc.alloc_semaphore("s_" + e)
        for qn in ("sp", "pool"):
            for i in range(ring):
                self.sems[(qn, i)] = nc.alloc_semaphore("d_%s%d" % (qn, i))

    def barrier(self):
        ev = []
        for e in ("pe", "act", "dve", "pool"):
            if self.cnt[e] > 0:
                ev.append((e, self.cnt[e]))
        for qn in ("sp", "pool"):
            n = self.dma_n[qn]
            for slot in range(min(n, self.ring)):
                uses = (n - slot + self.ring - 1) // self.ring
                ev.append(((qn, slot), 16 * uses))
        ev.extend((c, 1) for c in self.colls)
        self.fence = ev

    def _deps(self, eng, r, w):
        evs = list(self.fence)
        for d in r:
            evs.extend(d.lw)
        for d in w:
            evs.extend(d.lw)
            evs.extend(d.rd)
        wd = self.waited[eng]
        best = {}
        for (k, v) in evs:
            if k == eng and eng == "pe":
                continue
            if wd.get(k, 0) >= v:
                continue
            if best.get(k, 0) < v:
                best[k] = v
        waits = []
        for k, v in best.items():
            wd[k] = v
            waits.append((k, v))
        return waits

    def _commit(self, ev, r, w):
        for d in r:
            d.rd.append(ev)
            if len(d.rd) > 64:
                d.rd = d.rd[-32:] if False else d.rd
        comp = ("pe", "act", "dve", "pool")
        isdma = ev[0] not in comp
        for d in w:
            if isdma and d.lw and d.lw[0][0] not in comp:
                d.lw = d.lw[-11:] + [ev]
            else:
                d.lw = [ev]
            d.rd = []

    def op(self, eng, fn, r=(), w=()):
        waits = self._deps(eng, r, w)
        self.cnt[eng] += 1
        ev = (eng, self.cnt[eng])
        self.q[eng].append((waits, freeze(fn), (eng, 1)))
        self._commit(ev, r, w)

    def dma(self, qn, out, in_, r=(), w=(), **kw):
        n = self.dma_n[qn]
        slot = n % self.ring
        key = (qn, slot)
        val = 16 * (n // self.ring + 1)
        waits = self._deps(qn, r, w)
        if n >= self.ring:
            pv = val - 16
            if self.waited[qn].get(key, 0) < pv:
                self.waited[qn][key] = pv
                waits.append((key, pv))
        self.dma_n[qn] = n + 1
        fn = (lambda e, out=out, in_=in_, kw=kw: e.dma_start(out=out, in_=in_, **kw))
        self.q[qn].append((waits, fn, (key, 16)))
        self._commit((key, val), r, w)

    def coll(self, kind, src, dst, groups, r=(), w=()):
        name = "coll%d" % len(self.colls)
        self.colls.append(name)
        self.sems[name] = self.nc.alloc_semaphore(name)
        waits = self._deps("pool", r, w)
        fn = freeze(lambda e: e.collective_compute(kind, OP.bypass, replica_groups=groups, ins=[src], outs=[dst]))
        self.q["pool"].append((waits, fn, (name, 1)))
        self._commit((name, 1), r, w)

    def finish(self, eng="sp"):
        waits = [(c, 1) for c in self.colls]
        for e in ("pe", "act", "dve", "pool"):
            if self.cnt[e] > 0 and e != eng:
                waits.append((e, self.cnt[e]))
        for qn in ("sp", "pool"):
            n = self.dma_n[qn]
            for slot in range(min(n, self.ring)):
                uses = (n - slot + self.ring - 1) // self.ring
                waits.append(((qn, slot), 16 * uses))
        self.q[eng].append((waits, None, None))

    def emit(self):
        nc = self.nc
        names = {"pe": "tensor", "act": "scalar", "dve": "vector", "pool": "gpsimd", "sp": "sync"}
        with nc.Block() as block:
            for e in ENGS:
                items = self.q[e]
                if not items:
                    continue

                def body(engobj, items=items):
                    for waits, fn, inc in items:
                        for (k, v) in waits:
                            engobj.wait_ge(self.sems[k], v)
                        if isinstance(fn, tuple):
                            fn[1](engobj)
                        elif fn is not None:
                            ins = fn(engobj)
                            ins.then_inc(self.sems[inc[0]], inc[1])

                getattr(block, names[e])(body)


def run_pipeline(steps, nst):
    n = len(steps)
    for it in range(n + nst - 1):
        for k in range(nst):
            i = it - k
            if 0 <= i < n and steps[i][k] is not None:
                steps[i][k]()


class Ctx:
    pass


def mk_ctx(nc):
    C = Ctx()
    C.nc = nc
    C.P = Prog(nc)
    C.psall = nc.alloc_psum_tensor("psall", [128, 8, 512], F32)
    C.bank = [C.psall[:, i, :] for i in range(8)]
    C.dbank = deps(8)
    P = C.P
    C.onesf = nc.alloc_sbuf_tensor("onesf", [128, 128], F32)
    C.ident = nc.alloc_sbuf_tensor("ident", [128, 128], BF16)
    C.onesb = nc.alloc_sbuf_tensor("onesb", [128, 128], BF16)
    C.dconst = Dep()
    P.op("pool", lambda e: e.memset(C.onesf[:], 1.0), w=[C.dconst])
    P.op("pool", lambda e: e.memset(C.onesb[:], 1.0), w=[C.dconst])
    P.op("pool", lambda e: e.affine_select(out=C.ident[:], in_=C.onesf[:], pattern=[[-1, 128]],
                                           compare_op=OP.is_equal, fill=0.0, base=0,
                                           channel_multiplier=1), r=[C.dconst], w=[C.dconst])
    return C


def ada_rows(C, stk, cT, wada, bada, ncols, out_tile, dout):
    nc, P = C.nc, C.P
    csb = stk.enter_context(SBT(nc, "ada_c", [128, 8], F32))
    ca = stk.enter_context(SBT(nc, "ada_ca", [128, 8], F32))
    crep = stk.enter_context(SBT(nc, "ada_crep", [128, 8, 128], F32))
    wbuf = [stk.enter_context(SBT(nc, "ada_w%d" % i, [128, 8, 512], F32)) for i in range(2)]
    brow = stk.enter_context(SBT(nc, "ada_b", [1, ncols], F32))
    dc, dca, dcr, db = Dep(), Dep(), Dep(), Dep()
    dw = deps(2)
    P.dma("sp", csb[:], cT[:, :], w=[dc])
    P.dma("sp", brow[:], bada[0:1, 0:ncols], w=[db])
    P.op("act", lambda e: e.activation(out=ca[:], in_=csb[:], func=AF.Silu), r=[dc], w=[dca])
    for kc in range(8):
        P.op("dve", lambda e, kc=kc: e.tensor_scalar(out=crep[:, kc, :], in0=C.onesf[:], scalar1=ca[:, kc:kc + 1],
                                                     scalar2=None, op0=OP.mult), r=[dca, C.dconst], w=[dcr])
    wv = wada.rearrange("(kc p) n -> p kc n", p=128)
    for j in range(ncols // 512):
        b = j % 2
        P.dma("sp", wbuf[b][:], wv[:, :, j * 512:(j + 1) * 512], w=[dw[b]])
        bk = C.bank[j % 2]
        dbk = C.dbank[j % 2]
        for kc in range(8):
            P.op("pe", lambda e, kc=kc, b=b, bk=bk: e.matmul(bk[:], lhsT=crep[:, kc, :], rhs=wbuf[b][:, kc, :],
                                                            start=(kc == 0), stop=False), r=[dcr, dw[b]], w=[dbk])
        P.op("pe", lambda e, j=j, bk=bk: e.matmul(bk[:], lhsT=C.onesf[0:1, :], rhs=brow[0:1, j * 512:(j + 1) * 512],
                                                  start=False, stop=True), r=[db, C.dconst], w=[dbk])
        P.op("dve", lambda e, j=j, bk=bk: e.tensor_copy(out=out_tile[:, j * 512:(j + 1) * 512], in_=bk[:]),
             r=[dbk], w=[dout])


def bcast_row(C, stk, name, src, n, out_tile, dout, col0=0):
    C.P.dma("sp", out_tile[:, col0:col0 + n], src[0:1, 0:n].to_broadcast([128, n]), w=[dout])


def prenorm_proj(C, stk0, S, x, srow, shrow, dmod, wfm_d, nfm, wtm_d, ntm, fm_evac, tm_evac):
    nc, P = C.nc, C.P
    from contextlib import ExitStack
    with ExitStack() as stk:
        wfm = stk.enter_context(SBT(nc, "pp_wfm", [128, 8, nfm], BF16))
        wtm = stk.enter_context(SBT(nc, "pp_wtm", [128, 8, ntm], BF16))
        xt = [stk.enter_context(SBT(nc, "pp_x%d" % i, [128, 1024], F32)) for i in range(2)]
        junk = stk.enter_context(SBT(nc, "pp_junk", [128, 1024], BF16))
        tmp = stk.enter_context(SBT(nc, "pp_tmp", [128, 1024], F32))
        hb = [stk.enter_context(SBT(nc, "pp_hb%d" % i, [128, 1024], BF16)) for i in range(2)]
        hT = [stk.enter_context(SBT(nc, "pp_hT%d" % i, [128, 8, 512], BF16)) for i in range(2)]
        st = stk.enter_context(SBT(nc, "pp_st", [128, 8], F32))
        dwf, dwt, djunk, dtmp, dst_ = Dep(), Dep(), Dep(), Dep(), Dep()
        dx, dhb, dhT = deps(2), deps(2), deps(2)
        P.dma("pool", wfm[:], wfm_d.rearrange("(kc p) n -> p kc n", p=128), w=[dwf])
        P.dma("pool", wtm[:], wtm_d.rearrange("(kc p) n -> p kc n", p=128), w=[dwt])
        xt.append(stk.enter_context(SBT(nc, "pp_x2", [128, 1024], F32)))
        hb.append(stk.enter_context(SBT(nc, "pp_hb2", [128, 1024], BF16)))
        st3 = [stk.enter_context(SBT(nc, "pp_st%d" % i, [128, 8], F32)) for i in range(3)]
        dx.append(Dep()); dhb.append(Dep())
        dst3 = deps(3)
        steps = []
        for tt in range(S // 128):
            ch, i = tt // 4, tt % 4
            hb_ = ch % 2
            b = tt % 3
            bk = 6 + (tt % 2)

            def stA(tt=tt, b=b):
                st_, dstx = st3[b], dst3[b]
                P.dma("sp", xt[b][:], C.xtile(tt), r=[C.dxsrc(tt)], w=[dx[b]])
                P.op("act", lambda e: e.activation(out=junk[:], in_=xt[b][:], func=AF.Square, accum_out=st_[:, 0:1]), r=[dx[b]], w=[djunk, dstx])
                P.op("act", lambda e: e.activation(out=st_[:, 1:2], in_=st_[:, 0:1], func=AF.Sqrt, scale=1.0 / 1024, bias=C.epsb[:, 0:1]), r=[dstx, C.dconst], w=[dstx])
                P.op("dve", lambda e: e.reciprocal(out=st_[:, 2:3], in_=st_[:, 1:2]), r=[dstx], w=[dstx])
                P.op("dve", lambda e: e.scalar_tensor_tensor(out=tmp[:], in0=xt[b][:], scalar=st_[:, 2:3], in1=srow[:], op0=OP.mult, op1=OP.mult),
                     r=[dx[b], dstx, dmod], w=[dtmp])
                P.op("dve", lambda e: e.tensor_tensor(out=hb[b][:], in0=tmp[:], in1=shrow[:], op=OP.add), r=[dtmp, dmod], w=[dhb[b]])

            def stB(tt=tt, b=b, bk=bk, i=i, hb_=hb_):
                pT = C.bank[bk][:].bitcast(BF16)
                for kc in range(8):
                    P.op("pe", lambda e, kc=kc: e.transpose(out=pT[:, kc * 128:(kc + 1) * 128], in_=hb[b][:, kc * 128:(kc + 1) * 128], identity=C.ident[:]),
                         r=[dhb[b], C.dconst], w=[C.dbank[bk]])
                P.op("act", lambda e: e.activation(out=hT[hb_][:, :, i * 128:(i + 1) * 128], in_=pT.rearrange("p (k t) -> p k t", k=8), func=AF.Copy),
                     r=[C.dbank[bk]], w=[dhT[hb_]])

            def stC(ch=ch, hb_=hb_):
                for g in range(nfm // 128):
                    bk2 = g % 3
                    for kc in range(8):
                        P.op("pe", lambda e, g=g, kc=kc, bk2=bk2: e.matmul(C.bank[bk2][:], lhsT=wfm[:, kc, g * 128:(g + 1) * 128], rhs=hT[hb_][:, kc, :],
                                                                         start=(kc == 0), stop=(kc == 7)), r=[dwf, dhT[hb_]], w=[C.dbank[bk2]])
                    fm_evac(g, ch, C.bank[bk2], C.dbank[bk2])
                for i2 in range(4):
                    bk2 = 3 + (i2 % 3)
                    for kc in range(8):
                        P.op("pe", lambda e, i2=i2, kc=kc, bk2=bk2: e.matmul(C.bank[bk2][:, 0:ntm], lhsT=hT[hb_][:, kc, i2 * 128:(i2 + 1) * 128], rhs=wtm[:, kc, :],
                                                                           start=(kc == 0), stop=(kc == 7)), r=[dwt, dhT[hb_]], w=[C.dbank[bk2]])
                    tm_evac(ch * 4 + i2, C.bank[bk2], C.dbank[bk2])

            steps.append((stA, stB, stC if i == 3 else None))
        run_pipeline(steps, 3)
    P.barrier()


def build_A(C, L, S, x, o, din):
    from contextlib import ExitStack
    NT, NQC = S // 128, S // 512
    NC_ = S // 16 - 1
    NCH = (NC_ + 127) // 128
    nc = C.nc
    cT = din("cT", [128, 8]); wada = din("wada", [1024, 2048]); bada = din("bada", [1, 2048])
    gpre = din("gpre", [1, 1024])
    wfm_sb = din("wfm_sb", [1024, 256]); wtm_sb = din("wtm_sb", [1024, 128])
    wfm_n = din("wfm_n", [1024, 640]); wtm_n = din("wtm_n", [1024, 134])
    w1_d = din("w1", [128, 32, 256]); posT_d = din("posT", [128, 32]); w2_d = din("w2", [128, 2, 2, 64])
    rconst_d = din("rconst", [128, 4, 193]); wsel_d = din("wsel", [128, 255])
    tbg_d = din("tbg", [128, 2, 2, 128]); tcg_d = din("tcg", [128, 4, 5, 512]); chv_d = din("chv", [128, 4])
    negs_d = din("negs", [128, 3, 128]); negc_d = din("negc", [128, 5, 512])
    P = C.P
    C.do = Dep()
    bank, dbank = C.bank, C.dbank
    top = ExitStack()
    C.epsb = top.enter_context(SBT(nc, "epsb", [128, 1], F32))
    P.op("pool", lambda e: e.memset(C.epsb[:], 1e-6), w=[C.dconst])
    mod = top.enter_context(SBT(nc, "mod", [128, 2048], F32))
    srow = top.enter_context(SBT(nc, "srow", [128, 1024], F32))
    dmod = Dep()
    with ExitStack() as stk:
        ada_rows(C, stk, cT, wada, bada, 2048, mod, dmod)
    P.barrier()
    bcast_row(C, None, "gpre", gpre, 1024, srow, dmod)
    P.op("dve", lambda e: e.scalar_tensor_tensor(out=srow[:], in0=mod[:, 1024:2048], scalar=1.0, in1=srow[:], op0=OP.add, op1=OP.mult),
         r=[dmod], w=[dmod])
    shrow = mod[:, 0:1024]
    negs = top.enter_context(SBT(nc, "negs", [128, 3, 128], BF16))
    NIU = top.enter_context(SBT(nc, "NIU", [128, 128], BF16))
    P.dma("pool", negs[:], negs_d[:, :, :], w=[C.dconst])
    P.op("pool", lambda e: e.memset(NIU[:], -1.0), w=[C.dconst])
    P.op("pool", lambda e: e.affine_select(out=NIU[:], in_=NIU[:], pattern=[[-1, 128]], compare_op=OP.is_ge, fill=0.0,
                                           base=0, channel_multiplier=1), r=[C.dconst], w=[C.dconst])
    ot = [top.enter_context(SBT(nc, "ot%d" % i, [128, 4, 64], F32)) for i in range(2)]
    dot = deps(2)
    otn = 0

    with ExitStack() as stk:
        qk = stk.enter_context(SBT(nc, "qk", [128, 2, S], BF16))
        vsb = stk.enter_context(SBT(nc, "vsb", [128, NT, 128], BF16))
        dqk, dv = Dep(), Dep()

        def fm_evac(g, ch, bk, dbk):
            sc = 0.125 if g == 0 else 1.0
            P.op("act", lambda e: e.activation(out=qk[:, g, ch * 512:(ch + 1) * 512], in_=bk[:], func=AF.Copy, scale=sc),
                 r=[dbk], w=[dqk])

        def tm_evac(tt, bk, dbk):
            P.op("dve", lambda e: e.tensor_copy(out=vsb[:, tt, :], in_=bk[:, 0:128]), r=[dbk], w=[dv])

        prenorm_proj(C, stk, S, x, srow, shrow, dmod, wfm_sb, 256, wtm_sb, 128, fm_evac, tm_evac)
        if DEBUG:
            dq = nc.dram_tensor("dbg_qk", [128, 2, S], BF16, kind="ExternalOutput").ap()
            dvv = nc.dram_tensor("dbg_v", [128, NT, 128], BF16, kind="ExternalOutput").ap()
            dmo = nc.dram_tensor("dbg_mod", [128, 2048], F32, kind="ExternalOutput").ap()
            P.dma("sp", dq[:, :, :], qk[:], r=[dqk])
            P.dma("sp", dvv[:, :, :], vsb[:], r=[dv])
            P.dma("sp", dmo[:, :], mod[:], r=[dmod])

        NB = 3
        nones = stk.enter_context(SBT(nc, "sb_nones", [128, 128], BF16))
        P.op("pool", lambda e: e.memset(nones[:], -1.0), w=[C.dconst])
        esb = [stk.enter_context(SBT(nc, "sb_e%d" % i, [128, 2, 512], F32)) for i in range(2)]
        spb = [stk.enter_context(SBT(nc, "sb_sp%d" % i, [128, 2, 512], BF16)) for i in range(NB)]
        wb = [stk.enter_context(SBT(nc, "sb_w%d" % i, [128, 2, 512], BF16)) for i in range(NB)]
        ssum = stk.enter_context(SBT(nc, "sb_ss", [128, 2, 512], F32))
        sbf = [stk.enter_context(SBT(nc, "sb_sb%d" % i, [128, 2, 512], BF16)) for i in range(3)]
        de, dsp, dwb, dsbf = deps(2), deps(NB), deps(NB), deps(3)
        dss = Dep()
        dpair = deps(3)
        psall = C.psall
        otc = [otn]
        steps = []
        cnt = 0
        for qc in range(NQC):
            for si, kb in enumerate(range(4 * qc + 3, -1, -1)):
                k3 = cnt % NB
                k2 = cnt % 2
                pk = (cnt - 1) % 3
                ck = cnt % 3
                cnt += 1
                first = si == 0
                last = kb == 0
                i0 = max(0, kb - 4 * qc)
                c0 = 128 * i0
                cs = slice(c0, 512)
                qs = slice(qc * 512 + c0, (qc + 1) * 512)
                diag = kb >= 4 * qc

                def stA(k3=k3, k2=k2, first=first, last=last, c0=c0, cs=cs, qs=qs, diag=diag, kb=kb, ck=ck):
                    pair = psall[:, 2 * k3:2 * k3 + 2, :]
                    if first:
                        P.op("pool", lambda e: e.memset(ssum[:].rearrange("p h c -> p (h c)"), 0.0), w=[dss])
                    for h in range(2):
                        hp = slice(h * 64, (h + 1) * 64)
                        P.op("pe", lambda e, h=h, hp=hp: e.matmul(pair[:, h, cs], lhsT=qk[hp, 1, kb * 128:(kb + 1) * 128], rhs=qk[hp, 0, qs], start=True, stop=not diag),
                             r=[dqk], w=[dpair[k3]])
                        if diag:
                            P.op("pe", lambda e, h=h: e.matmul(pair[:, h, c0:c0 + 128], lhsT=C.ident[:], rhs=negs[:, 0, :], start=False, stop=True, skip_group_check=True),
                                 r=[C.dconst], w=[dpair[k3]])
                    P.op("act", lambda e: e.activation(out=esb[k2][:, :, cs], in_=pair[:, :, cs], func=AF.Exp), r=[dpair[k3]], w=[de[k2]])
                    P.op("act", lambda e: e.activation(out=spb[k3][:, :, cs], in_=esb[k2][:, :, cs], func=AF.Ln, bias=1.0), r=[de[k2]], w=[dsp[k3]])
                    if not last:
                        P.op("pool", lambda e: e.tensor_tensor(out=ssum[:, :, cs], in0=spb[k3][:, :, cs], in1=ssum[:, :, cs], op=OP.add), r=[dsp[k3]], w=[dss])
                        P.op("dve", lambda e: e.tensor_copy(out=sbf[ck][:].rearrange("p h c -> p (h c)"), in_=ssum[:].rearrange("p h c -> p (h c)")), r=[dss], w=[dsbf[ck]])

                def stB(k3=k3, first=first, cs=cs, pk=pk):
                    pair = psall[:, 2 * k3:2 * k3 + 2, :]
                    for h in range(2):
                        P.op("pe", lambda e, h=h: e.matmul(pair[:, h, cs], lhsT=NIU[:], rhs=spb[k3][:, h, cs], start=False, stop=True, skip_group_check=True),
                             r=[dsp[k3], C.dconst], w=[dpair[k3]])
                        if not first:
                            P.op("pe", lambda e, h=h: e.matmul(pair[:, h, cs], lhsT=nones[:], rhs=sbf[pk][:, h, cs], start=False, stop=True, skip_group_check=True),
                                 r=[dsbf[pk], C.dconst], w=[dpair[k3]])
                    P.op("act", lambda e: e.activation(out=wb[k3][:, :, cs], in_=pair[:, :, cs], func=AF.Exp), r=[dpair[k3]], w=[dwb[k3]])

                def stC(k3=k3, first=first, last=last, i0=i0, kb=kb, qc=qc):
                    for h in range(2):
                        hp = slice(h * 64, (h + 1) * 64)
                        OB = 6 + h
                        for i in range(i0, 4):
                            P.op("pe", lambda e, i=i, h=h, hp=hp, OB=OB: e.matmul(bank[OB][:, i * 64:(i + 1) * 64], lhsT=wb[k3][:, h, i * 128:(i + 1) * 128], rhs=vsb[:, kb, hp],
                                                                               start=(first and i == i0), stop=(last and i == 3), skip_group_check=True),
                                 r=[dwb[k3], dv], w=[dbank[OB]])
                    if last:
                        for h in range(2):
                            OB = 6 + h
                            ob = otc[0] % 2
                            otc[0] += 1
                            P.op("dve", lambda e, ob=ob, OB=OB: e.tensor_copy(out=ot[ob][:].rearrange("p i c -> p (i c)"), in_=bank[OB][:, 0:256]), r=[dbank[OB]], w=[dot[ob]])
                            P.dma("sp", o[qc * 512:(qc + 1) * 512, h * 64:(h + 1) * 64].rearrange("(i p) c -> p i c", p=128), ot[ob][:], r=[dot[ob]], w=[C.do])

                steps.append((stA, stB, stC))
        run_pipeline(steps, 3)
        otn = otc[0]
    P.barrier()
    C.top = top
    C.ot, C.dot, C.otn = ot, dot, otn
    C.io = dict(x=x, o=o, srow=srow, shrow=shrow, dmod=dmod, negs=negs, wfm_n=wfm_n, wtm_n=wtm_n, w1_d=w1_d, posT_d=posT_d, w2_d=w2_d,
                rconst_d=rconst_d, wsel_d=wsel_d, negs_d=negs_d, tbg_d=tbg_d, tcg_d=tcg_d, chv_d=chv_d, negc_d=negc_d)
    C.S = S
    C.zown = [0, 1]
    return C


def build_A_nsa(C):
    from contextlib import ExitStack
    nc, P, S = C.nc, C.P, C.S
    bank, dbank = C.bank, C.dbank
    io = C.io
    NT, NQC = S // 128, S // 512
    NC_ = S // 16 - 1
    NCH = (NC_ + 127) // 128
    x, o, srow, shrow, dmod, negs = io["x"], io["o"], io["srow"], io["shrow"], io["dmod"], io["negs"]
    ot, dot = C.ot, C.dot
    stk = ExitStack()
    qn = stk.enter_context(SBT(nc, "qn", [128, 2, S], BF16))
    kk = stk.enter_context(SBT(nc, "kk", [128, 2, S], BF16))
    vs1 = stk.enter_context(SBT(nc, "vs1", [128, NT, 2, 65], BF16))
    gat = stk.enter_context(SBT(nc, "gat", [128, NT, 6], F32))
    kcT = stk.enter_context(SBT(nc, "kcT", [128, 512], BF16))
    Rc = stk.enter_context(SBT(nc, "Rc", [128, 4, 193], BF16))
    dqn, dkk, dvs, dgat, dkc, dRc = Dep(), Dep(), Dep(), Dep(), Dep(), Dep()
    P.op("pool", lambda e: e.memset(vs1[:].rearrange("p t b c -> p (t b c)"), 1.0), w=[dvs])
    P.op("pool", lambda e: e.memset(kcT[:], 0.0), w=[dkc])
    P.dma("pool", Rc[:], io["rconst_d"][:, :, :], w=[dRc])
    with ExitStack() as stk_raw:
        raw = stk_raw.enter_context(SBT(nc, "raw", [128, S], BF16))
        draw = Dep()

        def fm_evac(g, ch, bk, dbk):
            if g < 2:
                P.op("act", lambda e: e.activation(out=qn[:, g, ch * 512:(ch + 1) * 512], in_=bk[:], func=AF.Copy, scale=0.125), r=[dbk], w=[dqn])
            elif g == 2:
                P.op("act", lambda e: e.activation(out=raw[:, ch * 512:(ch + 1) * 512], in_=bk[:], func=AF.Copy), r=[dbk], w=[draw])
            else:
                P.op("act", lambda e: e.activation(out=kk[:, g - 3, ch * 512:(ch + 1) * 512], in_=bk[:], func=AF.Copy), r=[dbk], w=[dkk])

        def tm_evac(tt, bk, dbk):
            P.op("dve", lambda e: e.tensor_copy(out=vs1[:, tt, :, 0:64], in_=bk[:, 0:128].rearrange("p (b c) -> p b c", b=2)), r=[dbk], w=[dvs])
            P.op("act", lambda e: e.activation(out=gat[:, tt, :], in_=bk[:, 128:134], func=AF.Sigmoid), r=[dbk], w=[dgat])

        prenorm_proj(C, stk, S, x, srow, shrow, dmod, io["wfm_n"], 640, io["wtm_n"], 134, fm_evac, tm_evac)

        with ExitStack() as s2:
            w1 = s2.enter_context(SBT(nc, "w1", [128, 32, 256], BF16))
            posT = s2.enter_context(SBT(nc, "posT", [128, 32], BF16))
            w2 = s2.enter_context(SBT(nc, "w2", [128, 2, 2, 64], BF16))
            gT = s2.enter_context(SBT(nc, "gT", [128, 2, 2, 512], BF16))
            bv = s2.enter_context(SBT(nc, "bv", [128, 4], F32))
            t0 = s2.enter_context(SBT(nc, "cm_t0", [128, 512], F32))
            t1 = s2.enter_context(SBT(nc, "cm_t1", [128, 512], F32))
            dw1, dgT, dbv, dt0, dt1 = Dep(), Dep(), Dep(), Dep(), Dep()
            P.dma("pool", w1[:], io["w1_d"][:, :, :], w=[dw1])
            P.dma("pool", posT[:], io["posT_d"][:, :], w=[dw1])
            P.dma("pool", w2[:], io["w2_d"][:, :, :, :], w=[dw1])
            for kv in range(2):
                rp = slice(kv * 64, (kv + 1) * 64)
                for half in range(2):
                    hs = slice(half * 128, (half + 1) * 128)
                    for l in range(32):
                        P.op("pe", lambda e, l=l: e.matmul(bank[1][:, 0:1], lhsT=w1[rp, l, hs], rhs=posT[rp, l:l + 1], start=(l == 0), stop=(l == 31)),
                             r=[dw1], w=[dbank[1]])
                    ci = kv * 2 + half
                    P.op("dve", lambda e, ci=ci: e.tensor_copy(out=bv[:, ci:ci + 1], in_=bank[1][:, 0:1]), r=[dbank[1]], w=[dbv])
                    for l in range(32):
                        P.op("pe", lambda e, l=l: e.matmul(bank[0][:, 0:NC_], lhsT=w1[rp, l, hs], rhs=raw[rp, l:l + 16 * (NC_ - 1) + 1:16],
                                                           start=(l == 0), stop=(l == 31)), r=[dw1, draw], w=[dbank[0]])
                    P.op("act", lambda e, ci=ci: e.activation(out=t0[:, 0:NC_], in_=bank[0][:, 0:NC_], func=AF.Identity, bias=bv[:, ci:ci + 1]),
                         r=[dbank[0], dbv], w=[dt0])
                    P.op("dve", lambda e: e.tensor_tensor(out=t1[:, 0:NC_], in0=t0[:, 0:NC_], in1=t0[:, 0:NC_], op=OP.mult), r=[dt0], w=[dt1])
                    P.op("dve", lambda e: e.tensor_scalar(out=t1[:, 0:NC_], in0=t1[:, 0:NC_], scalar1=0.044715, scalar2=1.0, op0=OP.mult, op1=OP.add),
                         r=[dt1], w=[dt1])
                    P.op("dve", lambda e: e.tensor_tensor(out=t1[:, 0:NC_], in0=t1[:, 0:NC_], in1=t0[:, 0:NC_], op=OP.mult), r=[dt1, dt0], w=[dt1])
                    P.op("act", lambda e: e.activation(out=t1[:, 0:NC_], in_=t1[:, 0:NC_], func=AF.Sigmoid, scale=1.5957691216057308), r=[dt1], w=[dt1])
                    P.op("dve", lambda e, kv=kv, half=half: e.tensor_tensor(out=gT[:, kv, half, 0:NC_], in0=t1[:, 0:NC_], in1=t0[:, 0:NC_], op=OP.mult),
                         r=[dt1, dt0], w=[dgT])
            w2kd = s2.enter_context(SBT(nc, "w2kd", [128, 2, 128], BF16))
            dw2 = Dep()
            for half in range(2):
                for dup in range(2):
                    P.op("dve", lambda e, half=half, dup=dup: e.tensor_copy(out=w2kd[:, half, dup * 64:(dup + 1) * 64], in_=w2[:, 0, half, :]), r=[dw1], w=[dw2])
            for half in range(2):
                P.op("pe", lambda e, half=half: e.matmul(bank[2][:, 0:NC_], lhsT=w2kd[:, half, :], rhs=gT[:, 0, half, 0:NC_],
                                                         start=(half == 0), stop=(half == 1)), r=[dw2, dgT], w=[dbank[2]])
            P.op("act", lambda e: e.activation(out=kcT[:, 0:NC_], in_=bank[2][:, 0:NC_], func=AF.Copy), r=[dbank[2]], w=[dkc])
            for c in range(NCH):
                nv = min(128, NC_ - c * 128)
                for half in range(2):
                    P.op("pe", lambda e, half=half, c=c, nv=nv: e.matmul(bank[3][0:nv, 0:64], lhsT=gT[:, 1, half, c * 128:c * 128 + nv], rhs=w2[:, 1, half, :],
                                                                       start=(half == 0), stop=(half == 1)), r=[dw1, dgT], w=[dbank[3]])
                P.op("dve", lambda e, c=c, nv=nv: e.tensor_copy(out=Rc[0:nv, c, 0:64], in_=bank[3][0:nv, 0:64]), r=[dbank[3]], w=[dRc])
    P.barrier()

    EW = stk.enter_context(SBT(nc, "EW", [128, S], BF16))
    Tc = stk.enter_context(SBT(nc, "Tc", [128, 4, 5, 512], BF16))
    TB = stk.enter_context(SBT(nc, "TB", [128, 2, 2, 128], BF16))
    chv = stk.enter_context(SBT(nc, "chv", [128, 4], F32))
    wsel = stk.enter_context(SBT(nc, "wsel", [128, 255], F32))
    dK = Dep()
    P.dma("sp", chv[:], io["chv_d"][:, :], w=[dK])
    P.dma("sp", wsel[:], io["wsel_d"][:, :], w=[dK])
    P.op("pool", lambda e: e.memset(EW[:], -NEG), w=[dK])
    P.op("pool", lambda e: e.affine_select(out=EW[:], in_=EW[:], pattern=[[1, S]], compare_op=OP.is_ge, fill=0.0, base=0, channel_multiplier=-64),
         r=[dK], w=[dK])
    P.op("pool", lambda e: e.affine_select(out=EW[:], in_=EW[:], pattern=[[-1, S]], compare_op=OP.is_ge, fill=0.0, base=63, channel_multiplier=64),
         r=[dK], w=[dK])
    with ExitStack() as s3:
        tcf = s3.enter_context(SBT(nc, "tcf", [128, 5, 512], F32))
        ncf = s3.enter_context(SBT(nc, "ncf", [128, 5, 512], F32))
        tbf = s3.enter_context(SBT(nc, "tbf", [128, 2, 2, 128], F32))
        ngf = s3.enter_context(SBT(nc, "ngf", [128, 3, 128], F32))
        dtc, dnf, dtb = Dep(), Dep(), Dep()
        P.dma("sp", ncf[:], io["negc_d"][:, :, :], w=[dnf])
        P.dma("sp", ngf[:], io["negs_d"][:, :, :], w=[dnf])
        P.dma("sp", tbf[:], io["tbg_d"][:, :, :, :], w=[dtb])
        for z in range(4):
            P.dma("sp", tcf[:], io["tcg_d"][:, z, :, :], w=[dtc])
            P.op("dve", lambda e, z=z: e.scalar_tensor_tensor(out=Tc[:, z, :, :], in0=tcf[:], scalar=chv[:, z:z + 1], in1=ncf[:],
                                                              op0=OP.subtract, op1=OP.add), r=[dtc, dnf, dK], w=[dK])
        for zz in range(2):
            zc = C.zown[zz]
            P.op("dve", lambda e, zz=zz, zc=zc: e.scalar_tensor_tensor(out=TB[:, zz, 0, :], in0=tbf[:, zz, 0, :], scalar=chv[:, zc:zc + 1], in1=ngf[:, 1, :],
                                                                       op0=OP.subtract, op1=OP.add), r=[dtb, dnf, dK], w=[dK])
            P.op("dve", lambda e, zz=zz, zc=zc: e.tensor_scalar(out=TB[:, zz, 1, :], in0=tbf[:, zz, 1, :], scalar1=chv[:, zc:zc + 1], scalar2=None,
                                                                op0=OP.subtract), r=[dtb, dK], w=[dK])
    P.barrier()

    pb = [stk.enter_context(SBT(nc, "n_p%d" % i, [128, 512], BF16)) for i in range(2)]
    dpb = deps(2)
    imp = stk.enter_context(SBT(nc, "imp", [128, 4, 128], F32))
    onsa = stk.enter_context(SBT(nc, "onsa", [128, 2, 4, 64], F32))
    sm = stk.enter_context(SBT(nc, "n_sm", [128, 16], F32))
    sc = stk.enter_context(SBT(nc, "n_sc", [128, 128], F32))
    sc2 = stk.enter_context(SBT(nc, "n_sc2", [128, 128], F32))
    m8 = stk.enter_context(SBT(nc, "n_m8", [128, 16], F32))
    selb = [stk.enter_context(SBT(nc, "n_sel%d" % i, [128, 128], BF16)) for i in range(4)]
    dselb = deps(4)
    selT = stk.enter_context(SBT(nc, "n_selT", [128, 512], BF16))
    dimp, donsa, dsm, dsc, dsel, dselT = Dep(), Dep(), Dep(), Dep(), Dep(), Dep()
    pit = 0
    zown = C.zown
    otn = C.otn

    def finalize(OBv, zz, br, qc, first_branch):
        P.op("dve", lambda e: e.tensor_scalar(out=sm[:, 0:4], in0=OBv[:, :, 64], scalar1=1e-30, scalar2=None, op0=OP.max), r=[dbank_of[0]], w=[dsm])
        P.op("dve", lambda e: e.reciprocal(out=sm[:, 4:8], in_=sm[:, 0:4]), r=[dsm], w=[dsm])
        P.op("dve", lambda e: e.tensor_tensor(out=sm[:, 8:12], in0=sm[:, 4:8], in1=gat[:, 4 * qc:4 * qc + 4, zz * 3 + br], op=OP.mult), r=[dsm, dgat], w=[dsm])
        for i in range(4):
            if first_branch:
                P.op("dve", lambda e, i=i: e.tensor_scalar(out=onsa[:, zz, i, :], in0=OBv[:, i, 0:64], scalar1=sm[:, 8 + i:9 + i], scalar2=None, op0=OP.mult),
                     r=[dbank_of[0], dsm], w=[donsa])
            else:
                P.op("dve", lambda e, i=i: e.scalar_tensor_tensor(out=onsa[:, zz, i, :], in0=OBv[:, i, 0:64], scalar=sm[:, 8 + i:9 + i], in1=onsa[:, zz, i, :],
                                                                  op0=OP.mult, op1=OP.add), r=[dbank_of[0], dsm], w=[donsa])

    dbank_of = [None]
    pb3 = stk.enter_context(SBT(nc, "n_p2", [128, 512], BF16))
    pb = [pb[0], pb[1], pb3]
    dpb = dpb + [Dep()]
    otc = [otn]
    for qc in range(NQC):
        qsl = slice(qc * 512, (qc + 1) * 512)
        cmax = min(NCH - 1, qc // 4)
        steps = []
        for z in range(4):
            zp = slice((z % 2) * 64, (z % 2) * 64 + 64)
            O0 = 4 + 2 * (z % 2)
            for c in range(cmax + 1):
                b = pit % 3
                A = pit % 4
                pit += 1
                dl = qc - 4 * c
                near = 0 <= dl <= 4

                def stA(z=z, zp=zp, c=c, b=b, A=A, dl=dl, near=near, qsl=qsl):
                    P.op("pe", lambda e: e.matmul(bank[A][:], lhsT=kcT[zp, c * 128:(c + 1) * 128], rhs=qn[zp, z // 2, qsl], start=True, stop=not near),
                         r=[dkc, dqn], w=[dbank[A]])
                    if near:
                        P.op("pe", lambda e: e.matmul(bank[A][:], lhsT=C.ident[:], rhs=Tc[:, z, dl, :], start=False, stop=True), r=[dK, C.dconst], w=[dbank[A]])
                    P.op("act", lambda e: e.activation(out=pb[b][:], in_=bank[A][:], func=AF.Exp, bias=chv[:, z:z + 1]), r=[dbank[A], dK], w=[dpb[b]])

                def stB(z=z, c=c, b=b, O0=O0, cmax=cmax, qc=qc):
                    for i in range(4):
                        ob = O0 + i // 2
                        P.op("pe", lambda e, i=i, ob=ob: e.matmul(bank[ob][:, (i % 2) * 193:(i % 2) * 193 + 193], lhsT=pb[b][:, i * 128:(i + 1) * 128], rhs=Rc[:, c, :],
                                                                  start=(c == 0 and i % 2 == 0), stop=(c == cmax and i % 2 == 1), skip_group_check=True),
                             r=[dpb[b], dRc], w=[dbank[ob]])
                    if c != cmax:
                        return
                    own = z in zown
                    for i in range(4):
                        ob = O0 + i // 2
                        v = bank[ob][:, (i % 2) * 193:(i % 2) * 193 + 193]
                        P.op("dve", lambda e, v=v, i=i: e.tensor_scalar(out=sm[:, i:i + 1], in0=v[:, 64:65], scalar1=1e-30, scalar2=None, op0=OP.max), r=[dbank[ob]], w=[dsm])
                        P.op("dve", lambda e, i=i: e.reciprocal(out=sm[:, 4 + i:5 + i], in_=sm[:, i:i + 1]), r=[dsm], w=[dsm])
                        if z == 0:
                            P.op("dve", lambda e, v=v, i=i: e.tensor_scalar(out=imp[:, i, :], in0=v[:, 65:193], scalar1=sm[:, 4 + i:5 + i], scalar2=None, op0=OP.mult),
                                 r=[dbank[ob], dsm], w=[dimp])
                        else:
                            P.op("dve", lambda e, v=v, i=i: e.scalar_tensor_tensor(out=imp[:, i, :], in0=v[:, 65:193], scalar=sm[:, 4 + i:5 + i], in1=imp[:, i, :],
                                                                                   op0=OP.mult, op1=OP.add), r=[dbank[ob], dsm], w=[dimp])
                        if own:
                            zz = zown.index(z)
                            P.op("dve", lambda e, i=i, zz=zz: e.tensor_tensor(out=sm[:, 8 + i:9 + i], in0=sm[:, 4 + i:5 + i], in1=gat[:, 4 * qc + i, zz * 3:zz * 3 + 1], op=OP.mult),
                                 r=[dsm, dgat], w=[dsm])
                            P.op("dve", lambda e, v=v, i=i, zz=zz: e.tensor_scalar(out=onsa[:, zz, i, :], in0=v[:, 0:64], scalar1=sm[:, 8 + i:9 + i], scalar2=None, op0=OP.mult),
                                 r=[dbank[ob], dsm], w=[donsa])

                steps.append((stA, stB))
        run_pipeline(steps, 2)
        for i in range(4):
            qb = 4 * qc + i
            P.op("dve", lambda e, i=i, qb=qb: e.tensor_tensor(out=sc[:], in0=imp[:, i, :], in1=wsel[:, 127 - 2 * qb:255 - 2 * qb], op=OP.add), r=[dimp, dK], w=[dsc])
            if qb >= 1:
                P.op("dve", lambda e: e.tensor_scalar(out=sc[:, 0:1], in0=sc[:, 0:1], scalar1=1e4, scalar2=None, op0=OP.add), r=[dsc], w=[dsc])
            P.op("dve", lambda e: e.max(out=m8[:, 0:8], in_=sc[:]), r=[dsc], w=[dsm])
            P.op("dve", lambda e: e.match_replace(out=sc2[:], in_to_replace=m8[:, 0:8], in_values=sc[:], imm_value=-1e9), r=[dsc, dsm], w=[dsc])
            P.op("dve", lambda e: e.max(out=m8[:, 8:16], in_=sc2[:]), r=[dsc], w=[dsm])
            P.op("dve", lambda e: e.tensor_scalar(out=m8[:, 15:16], in0=m8[:, 15:16], scalar1=0.0, scalar2=None, op0=OP.max), r=[dsm], w=[dsm])
            P.op("dve", lambda e, i=i: e.tensor_scalar(out=selb[i][:], in0=sc[:], scalar1=m8[:, 15:16], scalar2=None, op0=OP.is_ge), r=[dsc, dsm], w=[dselb[i]])

        def sel_transposes():
            for i in range(4):
                pT = bank[0][:].bitcast(BF16)
                P.op("pe", lambda e, pT=pT, i=i: e.transpose(out=pT[:, 0:128], in_=selb[i][:], identity=C.ident[:]), r=[dselb[i], C.dconst], w=[dbank[0]])
                P.op("dve", lambda e, pT=pT, i=i: e.tensor_scalar(out=selT[:, i * 128:(i + 1) * 128], in0=pT[:, 0:128], scalar1=-1.0, scalar2=None, op0=OP.add),
                     r=[dbank[0]], w=[dselT])

        for br in (2, 1):
            steps = []
            if br == 1:
                sel_transposes()
            klo = 0 if br == 1 else max(0, 4 * qc - 4)
            for kb in range(klo, 4 * qc + 4):
                for zz in range(2):
                    z = zown[zz]
                    zp = slice((z % 2) * 64, (z % 2) * 64 + 64)
                    OB = (6 if br == 1 else 4) + zz
                    i0 = max(0, kb - 4 * qc)
                    i1 = 3 if br == 1 else min(3, kb - 4 * qc + 4)
                    c0, c1 = 128 * i0, 128 * (i1 + 1)
                    cs = slice(c0, c1)
                    b = pit % 3
                    A = pit % 3
                    pit += 1
                    extra = []
                    if br == 1:
                        extra.append((cs, EW[:, kb * 128:(kb + 1) * 128], selT[:, cs], [dK, dselT]))
                    for i in range(i0, i1 + 1):
                        d = kb - (4 * qc + i)
                        isl = slice(i * 128, (i + 1) * 128)
                        if d == 0:
                            extra.append((isl, C.ident[:], TB[:, zz, 0, :], [dK, C.dconst]))
                        elif d == -1:
                            extra.append((isl, C.ident[:], TB[:, zz, 1, :], [dK, C.dconst]))
                        elif d == -4 and br == 2:
                            extra.append((isl, C.ident[:], negs[:, 2, :], [C.dconst]))
                    firstk = kb == klo
                    lastk = kb == 4 * qc + 3

                    def stA(z=z, zp=zp, br=br, kb=kb, cs=cs, c0=c0, c1=c1, A=A, b=b, extra=extra, qc=qc):
                        P.op("pe", lambda e: e.matmul(bank[A][:, cs], lhsT=kk[zp, br - 1, kb * 128:(kb + 1) * 128], rhs=qn[zp, z // 2, qc * 512 + c0:qc * 512 + c1],
                                                      start=True, stop=(len(extra) == 0)), r=[dkk, dqn], w=[dbank[A]])
                        for xi, (sl, lt, rh, dd) in enumerate(extra):
                            P.op("pe", lambda e, sl=sl, lt=lt, rh=rh, lastx=(xi == len(extra) - 1): e.matmul(bank[A][:, sl], lhsT=lt, rhs=rh, start=False, stop=lastx,
                                                                                                           skip_group_check=True), r=dd, w=[dbank[A]])
                        P.op("act", lambda e: e.activation(out=pb[b][:, cs], in_=bank[A][:, cs], func=AF.Exp, bias=chv[:, z:z + 1]), r=[dbank[A], dK], w=[dpb[b]])
                        P.op("pe", lambda e: e.matmul(bank[3][:, 0:512], lhsT=C.ident[:], rhs=EW[:, 0:512], start=True, stop=True), r=[], w=[])

                    def stB(zz=zz, br=br, kb=kb, i0=i0, i1=i1, b=b, OB=OB, firstk=firstk, lastk=lastk, qc=qc):
                        for i in range(i0, i1 + 1):
                            P.op("pe", lambda e, i=i: e.matmul(bank[OB][:, i * 65:(i + 1) * 65], lhsT=pb[b][:, i * 128:(i + 1) * 128], rhs=vs1[:, kb, br - 1, :],
                                                               start=(firstk and i == i0), stop=(lastk and i == i1), skip_group_check=True),
                                 r=[dpb[b], dvs], w=[dbank[OB]])
                        if not lastk:
                            return
                        dbank_of[0] = dbank[OB]
                        finalize(bank[OB][:, 0:260].rearrange("p (i c) -> p i c", c=65), zz, br, qc, False)
                        if br == 1:
                            ob = otc[0] % 2
                            otc[0] += 1
                            P.op("act", lambda e: e.activation(out=ot[ob][:], in_=onsa[:, zz, :, :], func=AF.Copy), r=[donsa], w=[dot[ob]])
                            P.dma("sp", o[qc * 512:(qc + 1) * 512, 128 + zz * 64:128 + (zz + 1) * 64].rearrange("(i p) c -> p i c", p=128), ot[ob][:],
                                  r=[dot[ob]], w=[C.do])
                            if zz == 1 and qc % 2 == 1 and C.og is not None:
                                c_ = qc // 2
                                P.coll("AllGather", o[c_ * 1024:(c_ + 1) * 1024, :], C.og[c_ * 4096:(c_ + 1) * 4096, :], GROUPS, r=[C.do], w=[C.dog])

                    steps.append((stA, stB))
            run_pipeline(steps, 2)
    stk.close()
    C.top.close()
    P.barrier()


import math

D_MODEL = 1024
W_SB = 512
OFF_SB_Q, OFF_SB_K, OFF_SB_V, OFF_NSA_Q = 0, 512, 1024, 1536
OFF_NSA_KV = 2048
OFF_NSA_GATE = OFF_NSA_KV + 3 * 2 * 2 * 64
IN_COLS = OFF_NSA_GATE + 24


def t5_bucket_np(dist):
    n = np.maximum(dist, 0)
    large = 16 + (np.log(np.maximum(n, 1).astype(np.float32) / np.float32(16)) / np.float32(math.log(128 / 16))
                  * np.float32(16)).astype(np.int32)
    large = np.minimum(large, 31)
    return np.where(n < 16, n, large)


_CONST_CACHE = {}


def consts_A(S):
    if S in _CONST_CACHE:
        return _CONST_CACHE[S]
    NC_ = S // 16 - 1
    k = np.arange(128)[:, None]
    q = np.arange(128)[None, :]
    negs = np.zeros((128, 3, 128), np.float32)
    negs[:, 0, :] = np.where(q > k, 0.0, NEG)
    negs[:, 1, :] = np.where(q >= k, 0.0, NEG)
    negs[:, 2, :] = np.where(q < k, 0.0, NEG)
    idx_tb = np.zeros((128, 2, 128), np.int64)
    idx_tb[:, 0, :] = t5_bucket_np(np.maximum(q - k, 0))
    idx_tb[:, 1, :] = t5_bucket_np(128 + q - k)
    q5 = np.arange(512)[None, None, :]
    dl = np.arange(5)[None, :, None]
    n_ = np.arange(128)[:, None, None]
    dist_c = 512 * dl + q5 - 16 * n_ - 31
    idx_tc = t5_bucket_np(np.maximum(dist_c, 0))
    negc = np.where(dist_c >= 0, 0.0, NEG).astype(np.float32)
    t = np.arange(128)[:, None]
    jp = np.arange(255)[None, :] - 127
    cur = (t >= 64).astype(np.int64)
    wsel = np.zeros((128, 255), np.float32)
    wsel[(jp == cur) | (jp == cur - 1)] = 1e4
    wsel[jp > cur] = -1.0
    rconst = np.zeros((128, 4, 193), np.float32)
    for c in range(4):
        n = c * 128 + np.arange(128)
        valid = n < NC_
        rconst[:, c, 64] = valid
        j = np.arange(128)[None, :]
        ov = (n[:, None] >= 4 * j - 1) & (n[:, None] <= 4 * j + 3) & valid[:, None]
        rconst[:, c, 65:193] = ov
    out = dict(negs=negs, idx_tb=idx_tb, idx_tc=idx_tc, negc=negc, wsel=wsel, rconst=rconst)
    _CONST_CACHE[S] = out
    return out


def prep_A(inp, layer, b, hg, S):
    cs = consts_A(S)
    g = hg // 2
    zo = [2 * (hg % 2), 2 * (hg % 2) + 1]
    L = zo + [z for z in range(4) if z not in zo]
    w_in = inp["w_in"][layer]
    hs = [2 * hg, 2 * hg + 1]

    def cols(off, n=64):
        return list(range(off, off + n))

    c_sb_fm = sum([cols(OFF_SB_Q + h * 64) for h in hs] + [cols(OFF_SB_K + h * 64) for h in hs], [])
    c_sb_tm = sum([cols(OFF_SB_V + h * 64) for h in hs], [])

    def kvcol(br, kvi):
        return cols(OFF_NSA_KV + ((br * 2 + kvi) * 2 + g) * 64)

    c_n_fm = sum([cols(OFF_NSA_Q + (g * 4 + z) * 64) for z in L], []) + kvcol(0, 0) + kvcol(0, 1) + kvcol(1, 0) + kvcol(1, 0) + kvcol(2, 0) + kvcol(2, 0)
    c_n_tm = kvcol(1, 1) + kvcol(2, 1) + sum([cols(OFF_NSA_GATE + (g * 4 + z) * 3, 3) for z in zo], [])
    heads = [g * 4 + z for z in L]
    tab = inp["rel_table"]
    tbg = np.stack([tab[cs["idx_tb"], heads[zz]] for zz in range(2)], axis=1)
    tcg = np.stack([tab[cs["idx_tc"], heads[z]] for z in range(4)], axis=1)
    chv = np.broadcast_to(tab[31, heads][None, :], (128, 4))
    w1 = np.concatenate([inp["cmp_w1_k"][layer].reshape(32, 64, 256).transpose(1, 0, 2),
                         inp["cmp_w1_v"][layer].reshape(32, 64, 256).transpose(1, 0, 2)], axis=0)
    posT = np.concatenate([inp["cmp_pos_k"][layer].T, inp["cmp_pos_v"][layer].T], axis=0)
    w2 = np.stack([inp["cmp_w2_k"][layer].reshape(2, 128, 64).transpose(1, 0, 2),
                   inp["cmp_w2_v"][layer].reshape(2, 128, 64).transpose(1, 0, 2)], axis=1)
    f = lambda a: np.ascontiguousarray(a, dtype=np.float32)
    return {
        "cT": f(inp["c"][b].reshape(8, 128).T), "wada": f(inp["w_ada"][layer][:, 0:2048]),
        "bada": f(inp["b_ada"][layer][None, 0:2048]), "gpre": f(inp["g_pre_mix"][layer][None]),
        "wfm_sb": f(w_in[:, c_sb_fm]), "wtm_sb": f(w_in[:, c_sb_tm]), "wfm_n": f(w_in[:, c_n_fm]), "wtm_n": f(w_in[:, c_n_tm]),
        "w1": f(w1), "posT": f(posT), "w2": f(w2), "rconst": cs["rconst"], "wsel": cs["wsel"],
        "tbg": f(tbg), "tcg": f(tcg), "chv": f(chv), "negs": cs["negs"], "negc": cs["negc"],
    }


def build_B(C, L, NTOK, n_exp, dff, x, dxres, og, dog, idx_d, y, dy, din):
    from contextlib import ExitStack
    NT = NTOK // 128
    NFC = dff // 128
    G = 4
    moe = n_exp > 1
    nc = C.nc
    cT = din("cTb", [128, 8]); wada = din("wadab", [1024, 4096]); bada = din("badab", [1, 4096])
    grow_d = din("grow", [1, 1024]); gpm_d = din("gpm", [1, 1024]); gpf_d = din("gpf", [1, 1024]); gqf_d = din("gqf", [1, 1024])
    wout_d = din("wout", [1024, 1024])
    wg_d = din("wg", [n_exp, 1024, dff]); wu_d = din("wu", [n_exp, 1024, dff]); wd_d = din("wd", [n_exp, dff, 1024])
    if moe:
        wr_d = din("wr", [8, 1024]); br_d = din("br", [1, 8])
    x1s = nc.dram_tensor("x1s_%d" % L, [NTOK, 1024], F32).ap()
    P = C.P
    bank, dbank = C.bank, C.dbank
    top = ExitStack()
    epsb = top.enter_context(SBT(nc, "epsb", [128, 1], F32))
    P.op("pool", lambda e: e.memset(epsb[:], 1e-6), w=[C.dconst])
    mod = top.enter_context(SBT(nc, "mod", [128, 4096], F32))
    dmod = Dep()
    with ExitStack() as stk:
        ada_rows(C, stk, cT, wada, bada, 4096, mod, dmod)
    P.barrier()
    h2T = top.enter_context(SBT(nc, "h2T", [128, 8, NTOK], BF16))
    gw = top.enter_context(SBT(nc, "gw", [128, NT, 8], F32))
    dh2T, dgw, dx1s = Dep(), Dep(), Dep()
    gm = mod[:, 0:1024]; shf = mod[:, 1024:2048]; srf = mod[:, 2048:3072]; gf = mod[:, 3072:4096]
    with ExitStack() as stk:
        rows = stk.enter_context(SBT(nc, "rows", [128, 4, 1024], F32))
        drows = Dep()
        for i, d in enumerate((grow_d, gpm_d, gpf_d, gqf_d)):
            P.dma("sp", rows[:, i, :], d[0:1, 0:1024].to_broadcast([128, 1024]), w=[drows])
        P.op("dve", lambda e: e.tensor_tensor(out=gm, in0=gm, in1=rows[:, 1, :], op=OP.mult), r=[drows, dmod], w=[dmod])
        P.op("dve", lambda e: e.scalar_tensor_tensor(out=srf, in0=srf, scalar=1.0, in1=rows[:, 2, :], op0=OP.add, op1=OP.mult), r=[drows, dmod], w=[dmod])
        P.op("dve", lambda e: e.tensor_tensor(out=gf, in0=gf, in1=rows[:, 3, :], op=OP.mult), r=[drows, dmod], w=[dmod])
        wout = stk.enter_context(SBT(nc, "wout", [128, 8, 1024], BF16))
        dwo = Dep()
        P.dma("pool", wout[:], wout_d.rearrange("(kc p) n -> p kc n", p=128), w=[dwo])
        if moe:
            wrr = stk.enter_context(SBT(nc, "wrr", [128, 8, 1024], F32))
            brr = stk.enter_context(SBT(nc, "brr", [128, 8], F32))
            dwr = Dep()
            for e_ in range(8):
                P.dma("sp", wrr[:, e_, :], wr_d[e_:e_ + 1, 0:1024].to_broadcast([128, 1024]), w=[dwr])
            P.dma("sp", brr[:], br_d[0:1, 0:8].to_broadcast([128, 8]), w=[dwr])
        o2 = [stk.enter_context(SBT(nc, "o2_%d" % i, [128, 4, 256], F32)) for i in range(2)]
        idxs = stk.enter_context(SBT(nc, "idxs", [128, NT * 4], mybir.dt.int32))
        didx = Dep()
        P.dma("sp", idxs[:], idx_d[:, :], w=[didx])
        xt = [stk.enter_context(SBT(nc, "xt_%d" % i, [128, 1024], F32)) for i in range(2)]
        x1t = [stk.enter_context(SBT(nc, "x1t_%d" % i, [128, 1024], F32)) for i in range(2)]
        junk = stk.enter_context(SBT(nc, "junk", [128, 1024], BF16))
        junkf = stk.enter_context(SBT(nc, "junkf", [128, 1024], F32))
        tmp = stk.enter_context(SBT(nc, "tmp", [128, 1024], F32))
        h2f = stk.enter_context(SBT(nc, "h2f", [128, 1024], F32))
        mg = stk.enter_context(SBT(nc, "mg", [128, 1024], BF16))
        h2b = stk.enter_context(SBT(nc, "h2b", [128, 1024], BF16))
        mT = stk.enter_context(SBT(nc, "mT", [128, 8, 128], BF16))
        st = stk.enter_context(SBT(nc, "st", [128, 16], F32))
        lg = stk.enter_context(SBT(nc, "lg", [128, 32], F32))
        do2, dxt, dx1t = deps(2), deps(2), deps(2)
        djunk, djf, dtmp, dh2f, dmg, dh2b, dmT, dst_, dlg = (Dep() for _ in range(9))

        def rms(src_aps, n, col):
            nn = len(src_aps)
            for k_, (ap_, dd) in enumerate(src_aps):
                jo = junk[:, 0:n] if len(ap_.shape) == 2 else junk[:, 0:n].rearrange("p (r c) -> p r c", r=ap_.shape[1])
                P.op("act", lambda e, ap_=ap_, k_=k_, jo=jo: e.activation(out=jo, in_=ap_, func=AF.Square, accum_out=st[:, col + k_:col + k_ + 1]),
                     r=[dd], w=[djunk, dst_])
            P.op("act", lambda e: e.activation(out=st[:, col:col + nn], in_=st[:, col:col + nn], func=AF.Sqrt, scale=1.0 / n, bias=epsb[:, 0:1]),
                 r=[dst_, C.dconst], w=[dst_])
            P.op("dve", lambda e: e.reciprocal(out=st[:, col:col + nn], in_=st[:, col:col + nn]), r=[dst_], w=[dst_])

        steps = []
        for tt in range(NT):
            b = tt % 2
            tsl = slice(tt * 128, (tt + 1) * 128)
            wo = 2 * (tt % 2)

            def stA(tt=tt, b=b, tsl=tsl, wo=wo):
                for r_ in range(4):
                    P.dma("pool", o2[b][:, r_, :], None, r=[dog, didx], w=[do2[b]])
                    waits_, fn_, inc_ = P.q["pool"][-1]
                    P.q["pool"][-1] = (waits_, freeze(lambda e, r_=r_, b=b, tt=tt: e.indirect_dma_start(
                        out=o2[b][:, r_, :], out_offset=None, in_=og[:, :], in_offset=bass.IndirectOffsetOnAxis(ap=idxs[:, tt * 4 + r_:tt * 4 + r_ + 1], axis=0))), inc_)
                P.dma("sp", xt[b][:], x[tsl, :], r=[dxres], w=[dxt[b]])
                rms([(o2[b][:, :, 0:128], do2[b]), (o2[b][:, :, 128:256], do2[b])], 512, 0)
                for hf in range(2):
                    hs = slice(hf * 512, (hf + 1) * 512)
                    P.op("dve", lambda e, hf=hf, hs=hs, b=b: e.scalar_tensor_tensor(out=mg[:, hs].rearrange("p (r c) -> p r c", r=4), in0=o2[b][:, :, hf * 128:(hf + 1) * 128], scalar=st[:, hf:hf + 1],
                                                                                   in1=rows[:, 0, hs].rearrange("p (r c) -> p r c", r=4),
                                                                                   op0=OP.mult, op1=OP.mult), r=[do2[b], dst_, drows], w=[dmg])
                pT = bank[6][:].bitcast(BF16)
                for kc in range(8):
                    P.op("pe", lambda e, kc=kc, pT=pT: e.transpose(out=pT[:, kc * 128:(kc + 1) * 128], in_=mg[:, kc * 128:(kc + 1) * 128], identity=C.ident[:]),
                         r=[dmg, C.dconst], w=[dbank[6]])
                P.op("act", lambda e, pT=pT: e.activation(out=mT[:], in_=pT.rearrange("p (k t) -> p k t", k=8), func=AF.Copy), r=[dbank[6]], w=[dmT])
                for hf in range(2):
                    for kc in range(8):
                        P.op("pe", lambda e, kc=kc, hf=hf: e.matmul(bank[hf + wo][:], lhsT=mT[:, kc, :], rhs=wout[:, kc, hf * 512:(hf + 1) * 512], start=(kc == 0), stop=(kc == 7)),
                             r=[dmT, dwo], w=[dbank[hf + wo]])

            def stB(tt=tt, b=b, tsl=tsl, wo=wo):
                for hf in range(2):
                    P.op("act", lambda e, hf=hf: e.activation(out=junk[:, 0:512], in_=bank[hf + wo][:], func=AF.Square, accum_out=st[:, 4 + hf:5 + hf]), r=[dbank[hf + wo]], w=[djunk, dst_])
                P.op("dve", lambda e: e.tensor_tensor(out=st[:, 6:7], in0=st[:, 4:5], in1=st[:, 5:6], op=OP.add), r=[dst_], w=[dst_])
                P.op("act", lambda e: e.activation(out=st[:, 6:7], in_=st[:, 6:7], func=AF.Sqrt, scale=1.0 / 1024, bias=epsb[:, 0:1]), r=[dst_, C.dconst], w=[dst_])
                P.op("dve", lambda e: e.reciprocal(out=st[:, 6:7], in_=st[:, 6:7]), r=[dst_], w=[dst_])
                for hf in range(2):
                    hs = slice(hf * 512, (hf + 1) * 512)
                    P.op("dve", lambda e, hf=hf, hs=hs: e.scalar_tensor_tensor(out=tmp[:, hs], in0=bank[hf + wo][:], scalar=st[:, 6:7], in1=gm[:, hs], op0=OP.mult, op1=OP.mult),
                         r=[dbank[hf + wo], dst_, dmod], w=[dtmp])
                P.op("dve", lambda e, b=b: e.tensor_tensor(out=x1t[b][:], in0=tmp[:], in1=xt[b][:], op=OP.add), r=[dtmp, dxt[b]], w=[dx1t[b]])
                P.dma("sp", x1s[tsl, :], x1t[b][:], r=[dx1t[b]], w=[dx1s])
                rms([(x1t[b][:], dx1t[b])], 1024, 8)
                P.op("dve", lambda e, b=b: e.scalar_tensor_tensor(out=tmp[:], in0=x1t[b][:], scalar=st[:, 8:9], in1=srf, op0=OP.mult, op1=OP.mult),
                     r=[dx1t[b], dst_, dmod], w=[dtmp])
                P.op("dve", lambda e: e.tensor_tensor(out=h2f[:], in0=tmp[:], in1=shf, op=OP.add), r=[dtmp, dmod], w=[dh2f])
                P.op("act", lambda e: e.activation(out=h2b[:], in_=h2f[:], func=AF.Copy), r=[dh2f], w=[dh2b])
                pT2 = bank[7][:].bitcast(BF16)
                for kc in range(8):
                    P.op("pe", lambda e, kc=kc, pT2=pT2: e.transpose(out=pT2[:, kc * 128:(kc + 1) * 128], in_=h2b[:, kc * 128:(kc + 1) * 128], identity=C.ident[:]),
                         r=[dh2b, C.dconst], w=[dbank[7]])
                P.op("act", lambda e, pT2=pT2, tsl=tsl: e.activation(out=h2T[:, :, tsl], in_=pT2.rearrange("p (k t) -> p k t", k=8), func=AF.Copy), r=[dbank[7]], w=[dh2T])
                if moe:
                    for e_ in range(8):
                        P.op("dve", lambda e, e_=e_: e.scalar_tensor_tensor(out=junkf[:], in0=h2f[:], scalar=1.0, in1=wrr[:, e_, :], op0=OP.mult, op1=OP.mult,
                                                                            accum_out=lg[:, e_:e_ + 1]), r=[dh2f, dwr], w=[djf, dlg])
                    P.op("dve", lambda e: e.tensor_tensor(out=lg[:, 0:8], in0=lg[:, 0:8], in1=brr[:], op=OP.add), r=[dlg, dwr], w=[dlg])
                    P.op("dve", lambda e: e.max(out=lg[:, 8:16], in_=lg[:, 0:8]), r=[dlg], w=[dlg])
                    P.op("dve", lambda e: e.tensor_scalar(out=lg[:, 16:24], in0=lg[:, 0:8], scalar1=lg[:, 8:9], scalar2=None, op0=OP.subtract), r=[dlg], w=[dlg])
                    P.op("act", lambda e: e.activation(out=lg[:, 16:24], in_=lg[:, 16:24], func=AF.Exp), r=[dlg], w=[dlg])
                    P.op("dve", lambda e: e.scalar_tensor_tensor(out=lg[:, 16:24], in0=lg[:, 0:8], scalar=lg[:, 9:10], in1=lg[:, 16:24], op0=OP.is_ge, op1=OP.mult,
                                                                 accum_out=lg[:, 24:25]), r=[dlg], w=[dlg])
                    P.op("dve", lambda e: e.reciprocal(out=lg[:, 25:26], in_=lg[:, 24:25]), r=[dlg], w=[dlg])
                    P.op("dve", lambda e, tt=tt: e.tensor_scalar(out=gw[:, tt, :], in0=lg[:, 16:24], scalar1=lg[:, 25:26], scalar2=None, op0=OP.mult), r=[dlg], w=[dgw])

            steps.append((stA, stB))
        run_pipeline(steps, 2)
    P.barrier()

    acc = top.enter_context(SBT(nc, "acc", [128, NT, 1024], F32))
    dacc = Dep()
    with ExitStack() as stk:
        wgb = [stk.enter_context(SBT(nc, "wg%d" % i, [128, 8, G * 128], BF16)) for i in range(2)]
        wub = [stk.enter_context(SBT(nc, "wu%d" % i, [128, 8, G * 128], BF16)) for i in range(2)]
        wdb = [stk.enter_context(SBT(nc, "wd%d" % i, [128, G, 1024], BF16)) for i in range(2)]
        sg = [stk.enter_context(SBT(nc, "sg%d" % i, [128, 256], F32)) for i in range(2)]
        aT = [stk.enter_context(SBT(nc, "aT%d" % i, [128, 256], BF16)) for i in range(2)]
        dwb, dsg, daT = deps(2), deps(2), deps(2)
        groups_ = [(ex, g0, min(G, NFC - g0)) for ex in range(n_exp) for g0 in range(0, NFC, G)]

        def load_group(gi):
            ex, g0, ng = groups_[gi]
            wb_ = gi % 2
            cs = slice(g0 * 128, (g0 + ng) * 128)
            P.dma("pool", wgb[wb_][:, :, 0:ng * 128], wg_d[ex, :, cs].rearrange("(kc p) n -> p kc n", p=128), w=[dwb[wb_]])
            P.dma("pool", wub[wb_][:, :, 0:ng * 128], wu_d[ex, :, cs].rearrange("(kc p) n -> p kc n", p=128), w=[dwb[wb_]])
            P.dma("pool", wdb[wb_][:, 0:ng, :], wd_d[ex, cs, :].rearrange("(f p) n -> p f n", p=128), w=[dwb[wb_]])

        load_group(0)
        steps = []
        it = 0
        for gi, (ex, g0, ng) in enumerate(groups_):
            wb_ = gi % 2
            firstgrp = gi == 0
            for tc_ in range(NTOK // 256):
                tks = slice(tc_ * 256, (tc_ + 1) * 256)
                for f in range(ng):
                    b = it % 2
                    it += 1
                    GB, UB = b, 2 + b
                    pre = gi + 1 if (tc_ == 0 and f == 0 and gi + 1 < len(groups_)) else None

                    def stA(f=f, b=b, GB=GB, UB=UB, wb_=wb_, tks=tks):
                        for kc in range(8):
                            P.op("pe", lambda e, kc=kc: e.matmul(bank[GB][:, 0:256], lhsT=wgb[wb_][:, kc, f * 128:(f + 1) * 128], rhs=h2T[:, kc, tks],
                                                                 start=(kc == 0), stop=(kc == 7)), r=[dwb[wb_], dh2T], w=[dbank[GB]])
                        for kc in range(8):
                            P.op("pe", lambda e, kc=kc: e.matmul(bank[UB][:, 0:256], lhsT=wub[wb_][:, kc, f * 128:(f + 1) * 128], rhs=h2T[:, kc, tks],
                                                                 start=(kc == 0), stop=(kc == 7)), r=[dwb[wb_], dh2T], w=[dbank[UB]])
                        P.op("act", lambda e: e.activation(out=sg[b][:], in_=bank[GB][:, 0:256], func=AF.Silu), r=[dbank[GB]], w=[dsg[b]])
                        P.op("dve", lambda e: e.tensor_tensor(out=aT[b][:], in0=bank[UB][:, 0:256], in1=sg[b][:], op=OP.mult), r=[dbank[UB], dsg[b]], w=[daT[b]])

                    def stB(f=f, b=b, wb_=wb_, ng=ng, tc_=tc_, ex=ex, firstgrp=firstgrp, pre=pre):
                        if pre is not None:
                            load_group(pre)
                        for sub in range(2):
                            for hf in range(2):
                                ob = 4 + sub * 2 + hf
                                P.op("pe", lambda e, sub=sub, hf=hf, ob=ob: e.matmul(
                                    bank[ob][:], lhsT=aT[b][:, sub * 128:(sub + 1) * 128], rhs=wdb[wb_][:, f, hf * 512:(hf + 1) * 512],
                                    start=(f == 0), stop=(f == ng - 1)), r=[daT[b], dwb[wb_]], w=[dbank[ob]])
                        if f != ng - 1:
                            return
                        for sub in range(2):
                            tt = tc_ * 2 + sub
                            for hf in range(2):
                                ob = 4 + sub * 2 + hf
                                hs = slice(hf * 512, (hf + 1) * 512)
                                if moe:
                                    if firstgrp:
                                        P.op("dve", lambda e, ob=ob, tt=tt, hs=hs: e.tensor_scalar(out=acc[:, tt, hs], in0=bank[ob][:], scalar1=gw[:, tt, ex:ex + 1], scalar2=None, op0=OP.mult),
                                             r=[dbank[ob], dgw], w=[dacc])
                                    else:
                                        P.op("dve", lambda e, ob=ob, tt=tt, hs=hs: e.scalar_tensor_tensor(out=acc[:, tt, hs], in0=bank[ob][:], scalar=gw[:, tt, ex:ex + 1], in1=acc[:, tt, hs],
                                                                                                        op0=OP.mult, op1=OP.add), r=[dbank[ob], dgw], w=[dacc])
                                else:
                                    if firstgrp:
                                        P.op("dve", lambda e, ob=ob, tt=tt, hs=hs: e.tensor_copy(out=acc[:, tt, hs], in_=bank[ob][:]), r=[dbank[ob]], w=[dacc])
                                    else:
                                        P.op("dve", lambda e, ob=ob, tt=tt, hs=hs: e.tensor_tensor(out=acc[:, tt, hs], in0=bank[ob][:], in1=acc[:, tt, hs], op=OP.add), r=[dbank[ob]], w=[dacc])

                    steps.append((stA, stB))
        run_pipeline(steps, 2)
    P.barrier()
    with ExitStack() as stk:
        xt = [stk.enter_context(SBT(nc, "fx%d" % i, [128, 1024], F32)) for i in range(2)]
        yt = [stk.enter_context(SBT(nc, "fy%d" % i, [128, 1024], F32)) for i in range(2)]
        junk = stk.enter_context(SBT(nc, "fjunk", [128, 1024], BF16))
        st = stk.enter_context(SBT(nc, "fst", [128, 4], F32))
        dxt, dyt = deps(2), deps(2)
        djunk, dst_ = Dep(), Dep()
        for tt in range(NT):
            b = tt % 2
            tsl = slice(tt * 128, (tt + 1) * 128)
            P.dma("sp", xt[b][:], x1s[tsl, :], r=[dx1s], w=[dxt[b]])
            P.op("act", lambda e, tt=tt: e.activation(out=junk[:], in_=acc[:, tt, :], func=AF.Square, accum_out=st[:, 0:1]), r=[dacc], w=[djunk, dst_])
            P.op("act", lambda e: e.activation(out=st[:, 1:2], in_=st[:, 0:1], func=AF.Sqrt, scale=1.0 / 1024, bias=epsb[:, 0:1]), r=[dst_, C.dconst], w=[dst_])
            P.op("dve", lambda e: e.reciprocal(out=st[:, 2:3], in_=st[:, 1:2]), r=[dst_], w=[dst_])
            P.op("dve", lambda e, tt=tt, b=b: e.scalar_tensor_tensor(out=yt[b][:], in0=acc[:, tt, :], scalar=st[:, 2:3], in1=gf, op0=OP.mult, op1=OP.mult),
                 r=[dacc, dst_, dmod], w=[dyt[b]])
            P.op("dve", lambda e, b=b: e.tensor_tensor(out=yt[b][:], in0=yt[b][:], in1=xt[b][:], op=OP.add), r=[dxt[b], dyt[b]], w=[dyt[b]])
            P.dma("sp", y[tsl, :], yt[b][:], r=[dyt[b]], w=[dy])
    top.close()
    P.barrier()


def prep_B(inp, layer, b):
    f = lambda a: np.ascontiguousarray(a, dtype=np.float32)
    m = {
        "cTb": f(inp["c"][b].reshape(8, 128).T), "wadab": f(inp["w_ada"][layer][:, 2048:6144]), "badab": f(inp["b_ada"][layer][None, 2048:6144]),
        "grow": f(np.concatenate([inp["g_sb"][layer], inp["g_nsa"][layer]])[None]), "gpm": f(inp["g_post_mix"][layer][None]),
        "gpf": f(inp["g_pre_ffn"][layer][None]), "gqf": f(inp["g_post_ffn"][layer][None]), "wout": f(inp["w_out"][layer]),
    }
    i = layer // 2
    if layer % 2 == 0:
        m["wg"] = f(inp["ffn_w_gate"][i][None]); m["wu"] = f(inp["ffn_w_up"][i][None]); m["wd"] = f(inp["ffn_w_down"][i][None])
    else:
        m["wg"] = f(inp["moe_w_gate"][i]); m["wu"] = f(inp["moe_w_up"][i]); m["wd"] = f(inp["moe_w_down"][i])
        m["wr"] = f(inp["moe_w_router"][i].T); m["br"] = f(inp["moe_b_router"][i][None])
    return m


_PROG_CACHE = {}
GROUPS = [[0, 1, 2, 3], [4, 5, 6, 7]]


def build_fused(S):
    NTOK = 2 * S // 8
    NT = NTOK // 128
    nc = bass.Bass("TRN2", target_bir_lowering=False)
    C = mk_ctx(nc)
    P = C.P

    def mk_din(L):
        def din(name, shape):
            return nc.dram_tensor("%s_%d" % (name, L), shape, F32, kind="ExternalInput").ap()
        return din

    xb = nc.dram_tensor("xb", [S, 1024], F32, kind="ExternalInput").ap()
    xtok = nc.dram_tensor("xtok", [NTOK, 1024], F32, kind="ExternalInput").ap()
    idx_d = nc.dram_tensor("idxg", [128, NT * 4], mybir.dt.int32, kind="ExternalInput").ap()
    y = nc.dram_tensor("y", [NTOK, 1024], F32, kind="ExternalOutput").ap()
    o0 = nc.dram_tensor("o0", [S, 256], F32).ap(); og0 = nc.dram_tensor("og0", [4 * S, 256], F32).ap()
    o1 = nc.dram_tensor("o1", [S, 256], F32).ap(); og1 = nc.dram_tensor("og1", [4 * S, 256], F32).ap()
    xo0 = nc.dram_tensor("xo0", [NTOK, 1024], F32).ap(); xg1 = nc.dram_tensor("xg1", [4 * NTOK, 1024], F32).ap()
    def gather_o(o, og, dsrc, ddst):
        for c in range(S // 1024):
            P.coll("AllGather", o[c * 1024:(c + 1) * 1024, :], og[c * 4096:(c + 1) * 4096, :], GROUPS, r=[dsrc], w=[ddst])

    dx0 = Dep()
    C.dxsrc = lambda tt: dx0
    C.xtile = lambda tt: xb[tt * 128:(tt + 1) * 128, :]
    dog0 = Dep()
    C.og, C.dog = og0, dog0
    build_A(C, 0, S, xb, o0, mk_din(0)); build_A_nsa(C)
    dxo0 = Dep()
    build_B(C, 0, NTOK, 1, 2816, xtok, Dep(), og0, dog0, idx_d, xo0, dxo0, mk_din(0))
    dxg1 = deps(NTOK // 256)
    for c in range(NTOK // 256):
        P.coll("AllGather", xo0[c * 256:(c + 1) * 256, :], xg1[c * 1024:(c + 1) * 1024, :], GROUPS, r=[dxo0], w=[dxg1[c]])
    C.dxsrc = lambda tt: dxg1[((tt * 128) % NTOK) // 256]

    def xtile1(tt):
        t = tt * 128
        rank, row = t // NTOK, t % NTOK
        c, rr = row // 256, row % 256
        r0 = c * 1024 + rank * 256 + rr
        return xg1[r0:r0 + 128, :]

    C.xtile = xtile1
    dog1 = Dep()
    C.og, C.dog = og1, dog1
    build_A(C, 1, S, xg1, o1, mk_din(1)); build_A_nsa(C)
    dy = Dep()
    build_B(C, 1, NTOK, 8, 3584, xo0, dxo0, og1, dog1, idx_d, y, dy, mk_din(1))
    P.finish("sp")
    P.emit()
    return nc


def kernel(**inp):
    inp = {k: np.asarray(v) for k, v in inp.items()}
    x = np.ascontiguousarray(inp["x"], dtype=np.float32)
    B, S, _ = x.shape
    NTOK = B * S // 8
    NT = NTOK // 128
    if S not in _PROG_CACHE:
        _PROG_CACHE[S] = build_fused(S)
    nc = _PROG_CACHE[S]
    maps = []
    for cid in range(8):
        b, part = cid // 4, cid % 4
        m = {"xb": x[b], "xtok": np.ascontiguousarray(x[b, part * NTOK:(part + 1) * NTOK])}
        p = np.arange(128)[:, None, None]
        tt = np.arange(NT)[None, :, None]
        r = np.arange(4)[None, None, :]
        t0 = part * NTOK + tt * 128
        m["idxg"] = ((t0 // 1024) * 4096 + r * 1024 + (t0 % 1024) + p).reshape(128, NT * 4).astype(np.int32)
        for L in range(2):
            for k, v in prep_A(inp, L, b, part, S).items():
                m["%s_%d" % (k, L)] = v
            for k, v in prep_B(inp, L, b).items():
                m["%s_%d" % (k, L)] = v
        maps.append(m)
    res = run_bass_kernel_spmd(nc, maps, core_ids=list(range(8)))
    out = np.zeros((B, S, 1024), np.float32)
    for cid in range(8):
        b, part = cid // 4, cid % 4
        out[b, part * NTOK:(part + 1) * NTOK] = res.results[cid]["y"]
    return out
```

```python
import numpy as np
import ml_dtypes
import concourse.bass as bass
import concourse.mybir as mybir
from concourse.bass_utils import run_bass_kernel_spmd

F32 = mybir.dt.float32
BF16 = mybir.dt.bfloat16
AF = mybir.ActivationFunctionType
OP = mybir.AluOpType
AX = mybir.AxisListType

ENGS = ("pe", "act", "dve", "pool", "sp")
NEG = -30000.0
DEBUG = False


_NM = [0]


def SBT(nc, name, shape, dt):
    _NM[0] += 1
    return nc.sbuf_tensor("t%d_%s" % (_NM[0], name), shape, dt)


import types


def freeze(fn):
    if fn.__closure__ is None:
        return fn
    cells = []
    for c in fn.__closure__:
        try:
            cells.append(types.CellType(c.cell_contents))
        except ValueError:
            cells.append(c)
    return types.FunctionType(fn.__code__, fn.__globals__, fn.__name__, fn.__defaults__, tuple(cells))


class Dep:
    __slots__ = ("lw", "rd")

    def __init__(self):
        self.lw = []
        self.rd = []


def deps(n):
    return [Dep() for _ in range(n)]


class Prog:
    def __init__(self, nc, ring=6):
        self.nc = nc
        self.q = {e: [] for e in ENGS}
        self.cnt = {e: 0 for e in ENGS}
        self.sems = {}
        self.waited = {e: {} for e in ENGS}
        self.ring = ring
        self.dma_n = {"sp": 0, "pool": 0}
        self.fence = []
        self.colls = []
        for e in ("pe", "act", "dve", "pool"):
            self.sems[e] = nc.alloc_semaphore("s_" + e)
        for qn in ("sp", "pool"):
            for i in range(ring):
                self.sems[(qn, i)] = nc.alloc_semaphore("d_%s%d" % (qn, i))

    def barrier(self):
        ev = []
        for e in ("pe", "act", "dve", "pool"):
            if self.cnt[e] > 0:
                ev.append((e, self.cnt[e]))
        for qn in ("sp", "pool"):
            n = self.dma_n[qn]
            for slot in range(min(n, self.ring)):
                uses = (n - slot + self.ring - 1) // self.ring
                ev.append(((qn, slot), 16 * uses))
        ev.extend((c, 1) for c in self.colls)
        self.fence = ev

    def _deps(self, eng, r, w):
        evs = list(self.fence)
        for d in r:
            evs.extend(d.lw)
        for d in w:
            evs.extend(d.lw)
            evs.extend(d.rd)
        wd = self.waited[eng]
        best = {}
        for (k, v) in evs:
            if k == eng and eng == "pe":
                continue
            if wd.get(k, 0) >= v:
                continue
            if best.get(k, 0) < v:
                best[k] = v
        waits = []
        for k, v in best.items():
            wd[k] = v
            waits.append((k, v))
        return waits

    def _commit(self, ev, r, w):
        for d in r:
            d.rd.append(ev)
            if len(d.rd) > 64:
                d.rd = d.rd[-32:] if False else d.rd
        comp = ("pe", "act", "dve", "pool")
        isdma = ev[0] not in comp
        for d in w:
            if isdma and d.lw and d.lw[0][0] not in comp:
                d.lw = d.lw[-11:] + [ev]
            else:
                d.lw = [ev]
            d.rd = []

    def op(self, eng, fn, r=(), w=()):
        waits = self._deps(eng, r, w)
        self.cnt[eng] += 1
        ev = (eng, self.cnt[eng])
        self.q[eng].append((waits, freeze(fn), (eng, 1)))
        self._commit(ev, r, w)

    def dma(self, qn, out, in_, r=(), w=(), **kw):
        n = self.dma_n[qn]
        slot = n % self.ring
        key = (qn, slot)
        val = 16 * (n // self.ring + 1)
        waits = self._deps(qn, r, w)
        if n >= self.ring:
            pv = val - 16
            if self.waited[qn].get(key, 0) < pv:
                self.waited[qn][key] = pv
                waits.append((key, pv))
        self.dma_n[qn] = n + 1
        fn = (lambda e, out=out, in_=in_, kw=kw: e.dma_start(out=out, in_=in_, **kw))
        self.q[qn].append((waits, fn, (key, 16)))
        self._commit((key, val), r, w)

    def coll(self, kind, src, dst, groups, r=(), w=()):
        name = "coll%d" % len(self.colls)
        self.colls.append(name)
        self.sems[name] = self.nc.alloc_semaphore(name)
        waits = self._deps("pool", r, w)
        fn = freeze(lambda e: e.collective_compute(kind, OP.bypass, replica_groups=groups, ins=[src], outs=[dst]))
        self.q["pool"].append((waits, fn, (name, 1)))
        self._commit((name, 1), r, w)

    def finish(self, eng="sp"):
        waits = [(c, 1) for c in self.colls]
        for e in ("pe", "act", "dve", "pool"):
            if self.cnt[e] > 0 and e != eng:
                waits.append((e, self.cnt[e]))
        for qn in ("sp", "pool"):
            n = self.dma_n[qn]
            for slot in range(min(n, self.ring)):
                uses = (n - slot + self.ring - 1) // self.ring
                waits.append(((qn, slot), 16 * uses))
        self.q[eng].append((waits, None, None))

    def emit(self):
        nc = self.nc
        names = {"pe": "tensor", "act": "scalar", "dve": "vector", "pool": "gpsimd", "sp": "sync"}
        with nc.Block() as block:
            for e in ENGS:
                items = self.q[e]
                if not items:
                    continue

                def body(engobj, items=items):
                    for waits, fn, inc in items:
                        for (k, v) in waits:
                            engobj.wait_ge(self.sems[k], v)
                        if isinstance(fn, tuple):
                            fn[1](engobj)
                        elif fn is not None:
                            ins = fn(engobj)
                            ins.then_inc(self.sems[inc[0]], inc[1])

                getattr(block, names[e])(body)


def run_pipeline(steps, nst):
    n = len(steps)
    for it in range(n + nst - 1):
        for k in range(nst):
            i = it - k
            if 0 <= i < n and steps[i][k] is not None:
                steps[i][k]()


class Ctx:
    pass


def mk_ctx(nc):
    C = Ctx()
    C.nc = nc
    C.P = Prog(nc)
    C.psall = nc.alloc_psum_tensor("psall", [128, 8, 512], F32)
    C.bank = [C.psall[:, i, :] for i in range(8)]
    C.dbank = deps(8)
    P = C.P
    C.onesf = nc.alloc_sbuf_tensor("onesf", [128, 128], F32)
    C.ident = nc.alloc_sbuf_tensor("ident", [128, 128], BF16)
    C.onesb = nc.alloc_sbuf_tensor("onesb", [128, 128], BF16)
    C.dconst = Dep()
    P.op("pool", lambda e: e.memset(C.onesf[:], 1.0), w=[C.dconst])
    P.op("pool", lambda e: e.memset(C.onesb[:], 1.0), w=[C.dconst])
    P.op("pool", lambda e: e.affine_select(out=C.ident[:], in_=C.onesf[:], pattern=[[-1, 128]],
                                           compare_op=OP.is_equal, fill=0.0, base=0,
                                           channel_multiplier=1), r=[C.dconst], w=[C.dconst])
    return C


def ada_rows(C, stk, cT, wada, bada, ncols, out_tile, dout):
    nc, P = C.nc, C.P
    csb = stk.enter_context(SBT(nc, "ada_c", [128, 8], F32))
    ca = stk.enter_context(SBT(nc, "ada_ca", [128, 8], F32))
    crep = stk.enter_context(SBT(nc, "ada_crep", [128, 8, 128], F32))
    wbuf = [stk.enter_context(SBT(nc, "ada_w%d" % i, [128, 8, 512], F32)) for i in range(2)]
    brow = stk.enter_context(SBT(nc, "ada_b", [1, ncols], F32))
    dc, dca, dcr, db = Dep(), Dep(), Dep(), Dep()
    dw = deps(2)
    P.dma("sp", csb[:], cT[:, :], w=[dc])
    P.dma("sp", brow[:], bada[0:1, 0:ncols], w=[db])
    P.op("act", lambda e: e.activation(out=ca[:], in_=csb[:], func=AF.Silu), r=[dc], w=[dca])
    for kc in range(8):
        P.op("dve", lambda e, kc=kc: e.tensor_scalar(out=crep[:, kc, :], in0=C.onesf[:], scalar1=ca[:, kc:kc + 1],
                                                     scalar2=None, op0=OP.mult), r=[dca, C.dconst], w=[dcr])
    wv = wada.rearrange("(kc p) n -> p kc n", p=128)
    for j in range(ncols // 512):
        b = j % 2
        P.dma("sp", wbuf[b][:], wv[:, :, j * 512:(j + 1) * 512], w=[dw[b]])
        bk = C.bank[j % 2]
        dbk = C.dbank[j % 2]
        for kc in range(8):
            P.op("pe", lambda e, kc=kc, b=b, bk=bk: e.matmul(bk[:], lhsT=crep[:, kc, :], rhs=wbuf[b][:, kc, :],
                                                            start=(kc == 0), stop=False), r=[dcr, dw[b]], w=[dbk])
        P.op("pe", lambda e, j=j, bk=bk: e.matmul(bk[:], lhsT=C.onesf[0:1, :], rhs=brow[0:1, j * 512:(j + 1) * 512],
                                                  start=False, stop=True), r=[db, C.dconst], w=[dbk])
        P.op("dve", lambda e, j=j, bk=bk: e.tensor_copy(out=out_tile[:, j * 512:(j + 1) * 512], in_=bk[:]),
             r=[dbk], w=[dout])


def bcast_row(C, stk, name, src, n, out_tile, dout, col0=0):
    C.P.dma("sp", out_tile[:, col0:col0 + n], src[0:1, 0:n].to_broadcast([128, n]), w=[dout])


def prenorm_proj(C, stk0, S, x, srow, shrow, dmod, wfm_d, nfm, wtm_d, ntm, fm_evac, tm_evac):
    nc, P = C.nc, C.P
    from contextlib import ExitStack
    with ExitStack() as stk:
        wfm = stk.enter_context(SBT(nc, "pp_wfm", [128, 8, nfm], BF16))
        wtm = stk.enter_context(SBT(nc, "pp_wtm", [128, 8, ntm], BF16))
        xt = [stk.enter_context(SBT(nc, "pp_x%d" % i, [128, 1024], F32)) for i in range(2)]
        junk = stk.enter_context(SBT(nc, "pp_junk", [128, 1024], BF16))
        tmp = stk.enter_context(SBT(nc, "pp_tmp", [128, 1024], F32))
        hb = [stk.enter_context(SBT(nc, "pp_hb%d" % i, [128, 1024], BF16)) for i in range(2)]
        hT = [stk.enter_context(SBT(nc, "pp_hT%d" % i, [128, 8, 512], BF16)) for i in range(2)]
        st = stk.enter_context(SBT(nc, "pp_st", [128, 8], F32))
        dwf, dwt, djunk, dtmp, dst_ = Dep(), Dep(), Dep(), Dep(), Dep()
        dx, dhb, dhT = deps(2), deps(2), deps(2)
        P.dma("pool", wfm[:], wfm_d.rearrange("(kc p) n -> p kc n", p=128), w=[dwf])
        P.dma("pool", wtm[:], wtm_d.rearrange("(kc p) n -> p kc n", p=128), w=[dwt])
        xt.append(stk.enter_context(SBT(nc, "pp_x2", [128, 1024], F32)))
        hb.append(stk.enter_context(SBT(nc, "pp_hb2", [128, 1024], BF16)))
        st3 = [stk.enter_context(SBT(nc, "pp_st%d" % i, [128, 8], F32)) for i in range(3)]
        dx.append(Dep()); dhb.append(Dep())
        dst3 = deps(3)
        steps = []
        for tt in range(S // 128):
            ch, i = tt // 4, tt % 4
            hb_ = ch % 2
            b = tt % 3
            bk = 6 + (tt % 2)

            def stA(tt=tt, b=b):
                st_, dstx = st3[b], dst3[b]
                P.dma("sp", xt[b][:], C.xtile(tt), r=[C.dxsrc(tt)], w=[dx[b]])
                P.op("act", lambda e: e.activation(out=junk[:], in_=xt[b][:], func=AF.Square, accum_out=st_[:, 0:1]), r=[dx[b]], w=[djunk, dstx])
                P.op("act", lambda e: e.activation(out=st_[:, 1:2], in_=st_[:, 0:1], func=AF.Sqrt, scale=1.0 / 1024, bias=C.epsb[:, 0:1]), r=[dstx, C.dconst], w=[dstx])
                P.op("dve", lambda e: e.reciprocal(out=st_[:, 2:3], in_=st_[:, 1:2]), r=[dstx], w=[dstx])
                P.op("dve", lambda e: e.scalar_tensor_tensor(out=tmp[:], in0=xt[b][:], scalar=st_[:, 2:3], in1=srow[:], op0=OP.mult, op1=OP.mult),
                     r=[dx[b], dstx, dmod], w=[dtmp])
                P.op("dve", lambda e: e.tensor_tensor(out=hb[b][:], in0=tmp[:], in1=shrow[:], op=OP.add), r=[dtmp, dmod], w=[dhb[b]])

            def stB(tt=tt, b=b, bk=bk, i=i, hb_=hb_):
                pT = C.bank[bk][:].bitcast(BF16)
                for kc in range(8):
                    P.op("pe", lambda e, kc=kc: e.transpose(out=pT[:, kc * 128:(kc + 1) * 128], in_=hb[b][:, kc * 128:(kc + 1) * 128], identity=C.ident[:]),
                         r=[dhb[b], C.dconst], w=[C.dbank[bk]])
                P.op("act", lambda e: e.activation(out=hT[hb_][:, :, i * 128:(i + 1) * 128], in_=pT.rearrange("p (k t) -> p k t", k=8), func=AF.Copy),
                     r=[C.dbank[bk]], w=[dhT[hb_]])

            def stC(ch=ch, hb_=hb_):
                for g in range(nfm // 128):
                    bk2 = g % 3
                    for kc in range(8):
                        P.op("pe", lambda e, g=g, kc=kc, bk2=bk2: e.matmul(C.bank[bk2][:], lhsT=wfm[:, kc, g * 128:(g + 1) * 128], rhs=hT[hb_][:, kc, :],
                                                                         start=(kc == 0), stop=(kc == 7)), r=[dwf, dhT[hb_]], w=[C.dbank[bk2]])
                    fm_evac(g, ch, C.bank[bk2], C.dbank[bk2])
                for i2 in range(4):
                    bk2 = 3 + (i2 % 3)
                    for kc in range(8):
                        P.op("pe", lambda e, i2=i2, kc=kc, bk2=bk2: e.matmul(C.bank[bk2][:, 0:ntm], lhsT=hT[hb_][:, kc, i2 * 128:(i2 + 1) * 128], rhs=wtm[:, kc, :],
                                                                           start=(kc == 0), stop=(kc == 7)), r=[dwt, dhT[hb_]], w=[C.dbank[bk2]])
                    tm_evac(ch * 4 + i2, C.bank[bk2], C.dbank[bk2])

            steps.append((stA, stB, stC if i == 3 else None))
        run_pipeline(steps, 3)
    P.barrier()


def build_A(C, L, S, x, o, din):
    from contextlib import ExitStack
    NT, NQC = S // 128, S // 512
    NC_ = S // 16 - 1
    NCH = (NC_ + 127) // 128
    nc = C.nc
    cT = din("cT", [128, 8]); wada = din("wada", [1024, 2048]); bada = din("bada", [1, 2048])
    gpre = din("gpre", [1, 1024])
    wfm_sb = din("wfm_sb", [1024, 256]); wtm_sb = din("wtm_sb", [1024, 128])
    wfm_n = din("wfm_n", [1024, 640]); wtm_n = din("wtm_n", [1024, 134])
    w1_d = din("w1", [128, 32, 256]); posT_d = din("posT", [128, 32]); w2_d = din("w2", [128, 2, 2, 64])
    rconst_d = din("rconst", [128, 4, 193]); wsel_d = din("wsel", [128, 255])
    tbg_d = din("tbg", [128, 2, 2, 128]); tcg_d = din("tcg", [128, 4, 5, 512]); chv_d = din("chv", [128, 4])
    negs_d = din("negs", [128, 3, 128]); negc_d = din("negc", [128, 5, 512])
    P = C.P
    C.do = Dep()
    bank, dbank = C.bank, C.dbank
    top = ExitStack()
    C.epsb = top.enter_context(SBT(nc, "epsb", [128, 1], F32))
    P.op("pool", lambda e: e.memset(C.epsb[:], 1e-6), w=[C.dconst])
    mod = top.enter_context(SBT(nc, "mod", [128, 2048], F32))
    srow = top.enter_context(SBT(nc, "srow", [128, 1024], F32))
    dmod = Dep()
    with ExitStack() as stk:
        ada_rows(C, stk, cT, wada, bada, 2048, mod, dmod)
    P.barrier()
    bcast_row(C, None, "gpre", gpre, 1024, srow, dmod)
    P.op("dve", lambda e: e.scalar_tensor_tensor(out=srow[:], in0=mod[:, 1024:2048], scalar=1.0, in1=srow[:], op0=OP.add, op1=OP.mult),
         r=[dmod], w=[dmod])
    shrow = mod[:, 0:1024]
    negs = top.enter_context(SBT(nc, "negs", [128, 3, 128], BF16))
    NIU = top.enter_context(SBT(nc, "NIU", [128, 128], BF16))
    P.dma("pool", negs[:], negs_d[:, :, :], w=[C.dconst])
    P.op("pool", lambda e: e.memset(NIU[:], -1.0), w=[C.dconst])
    P.op("pool", lambda e: e.affine_select(out=NIU[:], in_=NIU[:], pattern=[[-1, 128]], compare_op=OP.is_ge, fill=0.0,
                                           base=0, channel_multiplier=1), r=[C.dconst], w=[C.dconst])
    ot = [top.enter_context(SBT(nc, "ot%d" % i, [128, 4, 64], F32)) for i in range(2)]
    dot = deps(2)
    otn = 0

    with ExitStack() as stk:
        qk = stk.enter_context(SBT(nc, "qk", [128, 2, S], BF16))
        vsb = stk.enter_context(SBT(nc, "vsb", [128, NT, 128], BF16))
        dqk, dv = Dep(), Dep()

        def fm_evac(g, ch, bk, dbk):
            sc = 0.125 if g == 0 else 1.0
            P.op("act", lambda e: e.activation(out=qk[:, g, ch * 512:(ch + 1) * 512], in_=bk[:], func=AF.Copy, scale=sc),
                 r=[dbk], w=[dqk])

        def tm_evac(tt, bk, dbk):
            P.op("dve", lambda e: e.tensor_copy(out=vsb[:, tt, :], in_=bk[:, 0:128]), r=[dbk], w=[dv])

        prenorm_proj(C, stk, S, x, srow, shrow, dmod, wfm_sb, 256, wtm_sb, 128, fm_evac, tm_evac)
        if DEBUG:
            dq = nc.dram_tensor("dbg_qk", [128, 2, S], BF16, kind="ExternalOutput").ap()
            dvv = nc.dram_tensor("dbg_v", [128, NT, 128], BF16, kind="ExternalOutput").ap()
            dmo = nc.dram_tensor("dbg_mod", [128, 2048], F32, kind="ExternalOutput").ap()
            P.dma("sp", dq[:, :, :], qk[:], r=[dqk])
            P.dma("sp", dvv[:, :, :], vsb[:], r=[dv])
            P.dma("sp", dmo[:, :], mod[:], r=[dmod])

        NB = 3
        nones = stk.enter_context(SBT(nc, "sb_nones", [128, 128], BF16))
        P.op("pool", lambda e: e.memset(nones[:], -1.0), w=[C.dconst])
        esb = [stk.enter_context(SBT(nc, "sb_e%d" % i, [128, 2, 512], F32)) for i in range(2)]
        spb = [stk.enter_context(SBT(nc, "sb_sp%d" % i, [128, 2, 512], BF16)) for i in range(NB)]
        wb = [stk.enter_context(SBT(nc, "sb_w%d" % i, [128, 2, 512], BF16)) for i in range(NB)]
        ssum = stk.enter_context(SBT(nc, "sb_ss", [128, 2, 512], F32))
        sbf = [stk.enter_context(SBT(nc, "sb_sb%d" % i, [128, 2, 512], BF16)) for i in range(3)]
        de, dsp, dwb, dsbf = deps(2), deps(NB), deps(NB), deps(3)
        dss = Dep()
        dpair = deps(3)
        psall = C.psall
        otc = [otn]
        steps = []
        cnt = 0
        for qc in range(NQC):
            for si, kb in enumerate(range(4 * qc + 3, -1, -1)):
                k3 = cnt % NB
                k2 = cnt % 2
                pk = (cnt - 1) % 3
                ck = cnt % 3
                cnt += 1
                first = si == 0
                last = kb == 0
                i0 = max(0, kb - 4 * qc)
                c0 = 128 * i0
                cs = slice(c0, 512)
                qs = slice(qc * 512 + c0, (qc + 1) * 512)
                diag = kb >= 4 * qc

                def stA(k3=k3, k2=k2, first=first, last=last, c0=c0, cs=cs, qs=qs, diag=diag, kb=kb, ck=ck):
                    pair = psall[:, 2 * k3:2 * k3 + 2, :]
                    if first:
                        P.op("pool", lambda e: e.memset(ssum[:].rearrange("p h c -> p (h c)"), 0.0), w=[dss])
                    for h in range(2):
                        hp = slice(h * 64, (h + 1) * 64)
                        P.op("pe", lambda e, h=h, hp=hp: e.matmul(pair[:, h, cs], lhsT=qk[hp, 1, kb * 128:(kb + 1) * 128], rhs=qk[hp, 0, qs], start=True, stop=not diag),
                             r=[dqk], w=[dpair[k3]])
                        if diag:
                            P.op("pe", lambda e, h=h: e.matmul(pair[:, h, c0:c0 + 128], lhsT=C.ident[:], rhs=negs[:, 0, :], start=False, stop=True, skip_group_check=True),
                                 r=[C.dconst], w=[dpair[k3]])
                    P.op("act", lambda e: e.activation(out=esb[k2][:, :, cs], in_=pair[:, :, cs], func=AF.Exp), r=[dpair[k3]], w=[de[k2]])
                    P.op("act", lambda e: e.activation(out=spb[k3][:, :, cs], in_=esb[k2][:, :, cs], func=AF.Ln, bias=1.0), r=[de[k2]], w=[dsp[k3]])
                    if not last:
                        P.op("pool", lambda e: e.tensor_tensor(out=ssum[:, :, cs], in0=spb[k3][:, :, cs], in1=ssum[:, :, cs], op=OP.add), r=[dsp[k3]], w=[dss])
                        P.op("dve", lambda e: e.tensor_copy(out=sbf[ck][:].rearrange("p h c -> p (h c)"), in_=ssum[:].rearrange("p h c -> p (h c)")), r=[dss], w=[dsbf[ck]])

                def stB(k3=k3, first=first, cs=cs, pk=pk):
                    pair = psall[:, 2 * k3:2 * k3 + 2, :]
                    for h in range(2):
                        P.op("pe", lambda e, h=h: e.matmul(pair[:, h, cs], lhsT=NIU[:], rhs=spb[k3][:, h, cs], start=False, stop=True, skip_group_check=True),
                             r=[dsp[k3], C.dconst], w=[dpair[k3]])
                        if not first:
                            P.op("pe", lambda e, h=h: e.matmul(pair[:, h, cs], lhsT=nones[:], rhs=sbf[pk][:, h, cs], start=False, stop=True, skip_group_check=True),
                                 r=[dsbf[pk], C.dconst], w=[dpair[k3]])
                    P.op("act", lambda e: e.activation(out=wb[k3][:, :, cs], in_=pair[:, :, cs], func=AF.Exp), r=[dpair[k3]], w=[dwb[k3]])

                def stC(k3=k3, first=first, last=last, i0=i0, kb=kb, qc=qc):
                    for h in range(2):
                        hp = slice(h * 64, (h + 1) * 64)
                        OB = 6 + h
                        for i in range(i0, 4):
                            P.op("pe", lambda e, i=i, h=h, hp=hp, OB=OB: e.matmul(bank[OB][:, i * 64:(i + 1) * 64], lhsT=wb[k3][:, h, i * 128:(i + 1) * 128], rhs=vsb[:, kb, hp],
                                                                               start=(first and i == i0), stop=(last and i == 3), skip_group_check=True),
                                 r=[dwb[k3], dv], w=[dbank[OB]])
                    if last:
                        for h in range(2):
                            OB = 6 + h
                            ob = otc[0] % 2
                            otc[0] += 1
                            P.op("dve", lambda e, ob=ob, OB=OB: e.tensor_copy(out=ot[ob][:].rearrange("p i c -> p (i c)"), in_=bank[OB][:, 0:256]), r=[dbank[OB]], w=[dot[ob]])
                            P.dma("sp", o[qc * 512:(qc + 1) * 512, h * 64:(h + 1) * 64].rearrange("(i p) c -> p i c", p=128), ot[ob][:], r=[dot[ob]], w=[C.do])

                steps.append((stA, stB, stC))
        run_pipeline(steps, 3)
        otn = otc[0]
    P.barrier()
    C.top = top
    C.ot, C.dot, C.otn = ot, dot, otn
    C.io = dict(x=x, o=o, srow=srow, shrow=shrow, dmod=dmod, negs=negs, wfm_n=wfm_n, wtm_n=wtm_n, w1_d=w1_d, posT_d=posT_d, w2_d=w2_d,
                rconst_d=rconst_d, wsel_d=wsel_d, negs_d=negs_d, tbg_d=tbg_d, tcg_d=tcg_d, chv_d=chv_d, negc_d=negc_d)
    C.S = S
    C.zown = [0, 1]
    return C


def build_A_nsa(C):
    from contextlib import ExitStack
    nc, P, S = C.nc, C.P, C.S
    bank, dbank = C.bank, C.dbank
    io = C.io
    NT, NQC = S // 128, S // 512
    NC_ = S // 16 - 1
    NCH = (NC_ + 127) // 128
    x, o, srow, shrow, dmod, negs = io["x"], io["o"], io["srow"], io["shrow"], io["dmod"], io["negs"]
    ot, dot = C.ot, C.dot
    stk = ExitStack()
    qn = stk.enter_context(SBT(nc, "qn", [128, 2, S], BF16))
    kk = stk.enter_context(SBT(nc, "kk", [128, 2, S], BF16))
    vs1 = stk.enter_context(SBT(nc, "vs1", [128, NT, 2, 65], BF16))
    gat = stk.enter_context(SBT(nc, "gat", [128, NT, 6], F32))
    kcT = stk.enter_context(SBT(nc, "kcT", [128, 512], BF16))
    Rc = stk.enter_context(SBT(nc, "Rc", [128, 4, 193], BF16))
    dqn, dkk, dvs, dgat, dkc, dRc = Dep(), Dep(), Dep(), Dep(), Dep(), Dep()
    P.op("pool", lambda e: e.memset(vs1[:].rearrange("p t b c -> p (t b c)"), 1.0), w=[dvs])
    P.op("pool", lambda e: e.memset(kcT[:], 0.0), w=[dkc])
    P.dma("pool", Rc[:], io["rconst_d"][:, :, :], w=[dRc])
    with ExitStack() as stk_raw:
        raw = stk_raw.enter_context(SBT(nc, "raw", [128, S], BF16))
        draw = Dep()

        def fm_evac(g, ch, bk, dbk):
            if g < 2:
                P.op("act", lambda e: e.activation(out=qn[:, g, ch * 512:(ch + 1) * 512], in_=bk[:], func=AF.Copy, scale=0.125), r=[dbk], w=[dqn])
            elif g == 2:
                P.op("act", lambda e: e.activation(out=raw[:, ch * 512:(ch + 1) * 512], in_=bk[:], func=AF.Copy), r=[dbk], w=[draw])
            else:
                P.op("act", lambda e: e.activation(out=kk[:, g - 3, ch * 512:(ch + 1) * 512], in_=bk[:], func=AF.Copy), r=[dbk], w=[dkk])

        def tm_evac(tt, bk, dbk):
            P.op("dve", lambda e: e.tensor_copy(out=vs1[:, tt, :, 0:64], in_=bk[:, 0:128].rearrange("p (b c) -> p b c", b=2)), r=[dbk], w=[dvs])
            P.op("act", lambda e: e.activation(out=gat[:, tt, :], in_=bk[:, 128:134], func=AF.Sigmoid), r=[dbk], w=[dgat])

        prenorm_proj(C, stk, S, x, srow, shrow, dmod, io["wfm_n"], 640, io["wtm_n"], 134, fm_evac, tm_evac)

        with ExitStack() as s2:
            w1 = s2.enter_context(SBT(nc, "w1", [128, 32, 256], BF16))
            posT = s2.enter_context(SBT(nc, "posT", [128, 32], BF16))
            w2 = s2.enter_context(SBT(nc, "w2", [128, 2, 2, 64], BF16))
            gT = s2.enter_context(SBT(nc, "gT", [128, 2, 2, 512], BF16))
            bv = s2.enter_context(SBT(nc, "bv", [128, 4], F32))
            t0 = s2.enter_context(SBT(nc, "cm_t0", [128, 512], F32))
            t1 = s2.enter_context(SBT(nc, "cm_t1", [128, 512], F32))
            dw1, dgT, dbv, dt0, dt1 = Dep(), Dep(), Dep(), Dep(), Dep()
            P.dma("pool", w1[:], io["w1_d"][:, :, :], w=[dw1])
            P.dma("pool", posT[:], io["posT_d"][:, :], w=[dw1])
            P.dma("pool", w2[:], io["w2_d"][:, :, :, :], w=[dw1])
            for kv in range(2):
                rp = slice(kv * 64, (kv + 1) * 64)
                for half in range(2):
                    hs = slice(half * 128, (half + 1) * 128)
                    for l in range(32):
                        P.op("pe", lambda e, l=l: e.matmul(bank[1][:, 0:1], lhsT=w1[rp, l, hs], rhs=posT[rp, l:l + 1], start=(l == 0), stop=(l == 31)),
                             r=[dw1], w=[dbank[1]])
                    ci = kv * 2 + half
                    P.op("dve", lambda e, ci=ci: e.tensor_copy(out=bv[:, ci:ci + 1], in_=bank[1][:, 0:1]), r=[dbank[1]], w=[dbv])
                    for l in range(32):
                        P.op("pe", lambda e, l=l: e.matmul(bank[0][:, 0:NC_], lhsT=w1[rp, l, hs], rhs=raw[rp, l:l + 16 * (NC_ - 1) + 1:16],
                                                           start=(l == 0), stop=(l == 31)), r=[dw1, draw], w=[dbank[0]])
                    P.op("act", lambda e, ci=ci: e.activation(out=t0[:, 0:NC_], in_=bank[0][:, 0:NC_], func=AF.Identity, bias=bv[:, ci:ci + 1]),
                         r=[dbank[0], dbv], w=[dt0])
                    P.op("dve", lambda e: e.tensor_tensor(out=t1[:, 0:NC_], in0=t0[:, 0:NC_], in1=t0[:, 0:NC_], op=OP.mult), r=[dt0], w=[dt1])
                    P.op("dve", lambda e: e.tensor_scalar(out=t1[:, 0:NC_], in0=t1[:, 0:NC_], scalar1=0.044715, scalar2=1.0, op0=OP.mult, op1=OP.add),
                         r=[dt1], w=[dt1])
                    P.op("dve", lambda e: e.tensor_tensor(out=t1[:, 0:NC_], in0=t1[:, 0:NC_], in1=t0[:, 0:NC_], op=OP.mult), r=[dt1, dt0], w=[dt1])
                    P.op("act", lambda e: e.activation(out=t1[:, 0:NC_], in_=t1[:, 0:NC_], func=AF.Sigmoid, scale=1.5957691216057308), r=[dt1], w=[dt1])
                    P.op("dve", lambda e, kv=kv, half=half: e.tensor_tensor(out=gT[:, kv, half, 0:NC_], in0=t1[:, 0:NC_], in1=t0[:, 0:NC_], op=OP.mult),
                         r=[dt1, dt0], w=[dgT])
            w2kd = s2.enter_context(SBT(nc, "w2kd", [128, 2, 128], BF16))
            dw2 = Dep()
            for half in range(2):
                for dup in range(2):
                    P.op("dve", lambda e, half=half, dup=dup: e.tensor_copy(out=w2kd[:, half, dup * 64:(dup + 1) * 64], in_=w2[:, 0, half, :]), r=[dw1], w=[dw2])
            for half in range(2):
                P.op("pe", lambda e, half=half: e.matmul(bank[2][:, 0:NC_], lhsT=w2kd[:, half, :], rhs=gT[:, 0, half, 0:NC_],
                                                         start=(half == 0), stop=(half == 1)), r=[dw2, dgT], w=[dbank[2]])
            P.op("act", lambda e: e.activation(out=kcT[:, 0:NC_], in_=bank[2][:, 0:NC_], func=AF.Copy), r=[dbank[2]], w=[dkc])
            for c in range(NCH):
                nv = min(128, NC_ - c * 128)
                for half in range(2):
                    P.op("pe", lambda e, half=half, c=c, nv=nv: e.matmul(bank[3][0:nv, 0:64], lhsT=gT[:, 1, half, c * 128:c * 128 + nv], rhs=w2[:, 1, half, :],
                                                                       start=(half == 0), stop=(half == 1)), r=[dw1, dgT], w=[dbank[3]])
                P.op("dve", lambda e, c=c, nv=nv: e.tensor_copy(out=Rc[0:nv, c, 0:64], in_=bank[3][0:nv, 0:64]), r=[dbank[3]], w=[dRc])
    P.barrier()

    EW = stk.enter_context(SBT(nc, "EW", [128, S], BF16))
    Tc = stk.enter_context(SBT(nc, "Tc", [128, 4, 5, 512], BF16))
    TB = stk.enter_context(SBT(nc, "TB", [128, 2, 2, 128], BF16))
    chv = stk.enter_context(SBT(nc, "chv", [128, 4], F32))
    wsel = stk.enter_context(SBT(nc, "wsel", [128, 255], F32))
    dK = Dep()
    P.dma("sp", chv[:], io["chv_d"][:, :], w=[dK])
    P.dma("sp", wsel[:], io["wsel_d"][:, :], w=[dK])
    P.op("pool", lambda e: e.memset(EW[:], -NEG), w=[dK])
    P.op("pool", lambda e: e.affine_select(out=EW[:], in_=EW[:], pattern=[[1, S]], compare_op=OP.is_ge, fill=0.0, base=0, channel_multiplier=-64),
         r=[dK], w=[dK])
    P.op("pool", lambda e: e.affine_select(out=EW[:], in_=EW[:], pattern=[[-1, S]], compare_op=OP.is_ge, fill=0.0, base=63, channel_multiplier=64),
         r=[dK], w=[dK])
    with ExitStack() as s3:
        tcf = s3.enter_context(SBT(nc, "tcf", [128, 5, 512], F32))
        ncf = s3.enter_context(SBT(nc, "ncf", [128, 5, 512], F32))
        tbf = s3.enter_context(SBT(nc, "tbf", [128, 2, 2, 128], F32))
        ngf = s3.enter_context(SBT(nc, "ngf", [128, 3, 128], F32))
        dtc, dnf, dtb = Dep(), Dep(), Dep()
        P.dma("sp", ncf[:], io["negc_d"][:, :, :], w=[dnf])
        P.dma("sp", ngf[:], io["negs_d"][:, :, :], w=[dnf])
        P.dma("sp", tbf[:], io["tbg_d"][:, :, :, :], w=[dtb])
        for z in range(4):
            P.dma("sp", tcf[:], io["tcg_d"][:, z, :, :], w=[dtc])
            P.op("dve", lambda e, z=z: e.scalar_tensor_tensor(out=Tc[:, z, :, :], in0=tcf[:], scalar=chv[:, z:z + 1], in1=ncf[:],
                                                              op0=OP.subtract, op1=OP.add), r=[dtc, dnf, dK], w=[dK])
        for zz in range(2):
            zc = C.zown[zz]
            P.op("dve", lambda e, zz=zz, zc=zc: e.scalar_tensor_tensor(out=TB[:, zz, 0, :], in0=tbf[:, zz, 0, :], scalar=chv[:, zc:zc + 1], in1=ngf[:, 1, :],
                                                                       op0=OP.subtract, op1=OP.add), r=[dtb, dnf, dK], w=[dK])
            P.op("dve", lambda e, zz=zz, zc=zc: e.tensor_scalar(out=TB[:, zz, 1, :], in0=tbf[:, zz, 1, :], scalar1=chv[:, zc:zc + 1], scalar2=None,
                                                                op0=OP.subtract), r=[dtb, dK], w=[dK])
    P.barrier()

    pb = [stk.enter_context(SBT(nc, "n_p%d" % i, [128, 512], BF16)) for i in range(2)]
    dpb = deps(2)
    imp = stk.enter_context(SBT(nc, "imp", [128, 4, 128], F32))
    onsa = stk.enter_context(SBT(nc, "onsa", [128, 2, 4, 64], F32))
    sm = stk.enter_context(SBT(nc, "n_sm", [128, 16], F32))
    sc = stk.enter_context(SBT(nc, "n_sc", [128, 128], F32))
    sc2 = stk.enter_context(SBT(nc, "n_sc2", [128, 128], F32))
    m8 = stk.enter_context(SBT(nc, "n_m8", [128, 16], F32))
    selb = [stk.enter_context(SBT(nc, "n_sel%d" % i, [128, 128], BF16)) for i in range(4)]
    dselb = deps(4)
    selT = stk.enter_context(SBT(nc, "n_selT", [128, 512], BF16))
    dimp, donsa, dsm, dsc, dsel, dselT = Dep(), Dep(), Dep(), Dep(), Dep(), Dep()
    pit = 0
    zown = C.zown
    otn = C.otn

    def finalize(OBv, zz, br, qc, first_branch):
        P.op("dve", lambda e: e.tensor_scalar(out=sm[:, 0:4], in0=OBv[:, :, 64], scalar1=1e-30, scalar2=None, op0=OP.max), r=[dbank_of[0]], w=[dsm])
        P.op("dve", lambda e: e.reciprocal(out=sm[:, 4:8], in_=sm[:, 0:4]), r=[dsm], w=[dsm])
        P.op("dve", lambda e: e.tensor_tensor(out=sm[:, 8:12], in0=sm[:, 4:8], in1=gat[:, 4 * qc:4 * qc + 4, zz * 3 + br], op=OP.mult), r=[dsm, dgat], w=[dsm])
        for i in range(4):
            if first_branch:
                P.op("dve", lambda e, i=i: e.tensor_scalar(out=onsa[:, zz, i, :], in0=OBv[:, i, 0:64], scalar1=sm[:, 8 + i:9 + i], scalar2=None, op0=OP.mult),
                     r=[dbank_of[0], dsm], w=[donsa])
            else:
                P.op("dve", lambda e, i=i: e.scalar_tensor_tensor(out=onsa[:, zz, i, :], in0=OBv[:, i, 0:64], scalar=sm[:, 8 + i:9 + i], in1=onsa[:, zz, i, :],
                                                                  op0=OP.mult, op1=OP.add), r=[dbank_of[0], dsm], w=[donsa])

    dbank_of = [None]
    pb3 = stk.enter_context(SBT(nc, "n_p2", [128, 512], BF16))
    pb = [pb[0], pb[1], pb3]
    dpb = dpb + [Dep()]
    otc = [otn]
    for qc in range(NQC):
        qsl = slice(qc * 512, (qc + 1) * 512)
        cmax = min(NCH - 1, qc // 4)
        steps = []
        for z in range(4):
            zp = slice((z % 2) * 64, (z % 2) * 64 + 64)
            O0 = 4 + 2 * (z % 2)
            for c in range(cmax + 1):
                b = pit % 3
                A = pit % 4
                pit += 1
                dl = qc - 4 * c
                near = 0 <= dl <= 4

                def stA(z=z, zp=zp, c=c, b=b, A=A, dl=dl, near=near, qsl=qsl):
                    P.op("pe", lambda e: e.matmul(bank[A][:], lhsT=kcT[zp, c * 128:(c + 1) * 128], rhs=qn[zp, z // 2, qsl], start=True, stop=not near),
                         r=[dkc, dqn], w=[dbank[A]])
                    if near:
                        P.op("pe", lambda e: e.matmul(bank[A][:], lhsT=C.ident[:], rhs=Tc[:, z, dl, :], start=False, stop=True), r=[dK, C.dconst], w=[dbank[A]])
                    P.op("act", lambda e: e.activation(out=pb[b][:], in_=bank[A][:], func=AF.Exp, bias=chv[:, z:z + 1]), r=[dbank[A], dK], w=[dpb[b]])

                def stB(z=z, c=c, b=b, O0=O0, cmax=cmax, qc=qc):
                    for i in range(4):
                        ob = O0 + i // 2
                        P.op("pe", lambda e, i=i, ob=ob: e.matmul(bank[ob][:, (i % 2) * 193:(i % 2) * 193 + 193], lhsT=pb[b][:, i * 128:(i + 1) * 128], rhs=Rc[:, c, :],
                                                                  start=(c == 0 and i % 2 == 0), stop=(c == cmax and i % 2 == 1), skip_group_check=True),
                             r=[dpb[b], dRc], w=[dbank[ob]])
                    if c != cmax:
                        return
                    own = z in zown
                    for i in range(4):
                        ob = O0 + i // 2
                        v = bank[ob][:, (i % 2) * 193:(i % 2) * 193 + 193]
                        P.op("dve", lambda e, v=v, i=i: e.tensor_scalar(out=sm[:, i:i + 1], in0=v[:, 64:65], scalar1=1e-30, scalar2=None, op0=OP.max), r=[dbank[ob]], w=[dsm])
                        P.op("dve", lambda e, i=i: e.reciprocal(out=sm[:, 4 + i:5 + i], in_=sm[:, i:i + 1]), r=[dsm], w=[dsm])
                        if z == 0:
                            P.op("dve", lambda e, v=v, i=i: e.tensor_scalar(out=imp[:, i, :], in0=v[:, 65:193], scalar1=sm[:, 4 + i:5 + i], scalar2=None, op0=OP.mult),
                                 r=[dbank[ob], dsm], w=[dimp])
                        else:
                            P.op("dve", lambda e, v=v, i=i: e.scalar_tensor_tensor(out=imp[:, i, :], in0=v[:, 65:193], scalar=sm[:, 4 + i:5 + i], in1=imp[:, i, :],
                                                                                   op0=OP.mult, op1=OP.add), r=[dbank[ob], dsm], w=[dimp])
                        if own:
                            zz = zown.index(z)
                            P.op("dve", lambda e, i=i, zz=zz: e.tensor_tensor(out=sm[:, 8 + i:9 + i], in0=sm[:, 4 + i:5 + i], in1=gat[:, 4 * qc + i, zz * 3:zz * 3 + 1], op=OP.mult),
                                 r=[dsm, dgat], w=[dsm])
                            P.op("dve", lambda e, v=v, i=i, zz=zz: e.tensor_scalar(out=onsa[:, zz, i, :], in0=v[:, 0:64], scalar1=sm[:, 8 + i:9 + i], scalar2=None, op0=OP.mult),
                                 r=[dbank[ob], dsm], w=[donsa])

                steps.append((stA, stB))
        run_pipeline(steps, 2)
        for i in range(4):
            qb = 4 * qc + i
            P.op("dve", lambda e, i=i, qb=qb: e.tensor_tensor(out=sc[:], in0=imp[:, i, :], in1=wsel[:, 127 - 2 * qb:255 - 2 * qb], op=OP.add), r=[dimp, dK], w=[dsc])
            if qb >= 1:
                P.op("dve", lambda e: e.tensor_scalar(out=sc[:, 0:1], in0=sc[:, 0:1], scalar1=1e4, scalar2=None, op0=OP.add), r=[dsc], w=[dsc])
            P.op("dve", lambda e: e.max(out=m8[:, 0:8], in_=sc[:]), r=[dsc], w=[dsm])
            P.op("dve", lambda e: e.match_replace(out=sc2[:], in_to_replace=m8[:, 0:8], in_values=sc[:], imm_value=-1e9), r=[dsc, dsm], w=[dsc])
            P.op("dve", lambda e: e.max(out=m8[:, 8:16], in_=sc2[:]), r=[dsc], w=[dsm])
            P.op("dve", lambda e: e.tensor_scalar(out=m8[:, 15:16], in0=m8[:, 15:16], scalar1=0.0, scalar2=None, op0=OP.max), r=[dsm], w=[dsm])
            P.op("dve", lambda e, i=i: e.tensor_scalar(out=selb[i][:], in0=sc[:], scalar1=m8[:, 15:16], scalar2=None, op0=OP.is_ge), r=[dsc, dsm], w=[dselb[i]])

        def sel_transposes():
            for i in range(4):
                pT = bank[0][:].bitcast(BF16)
                P.op("pe", lambda e, pT=pT, i=i: e.transpose(out=pT[:, 0:128], in_=selb[i][:], identity=C.ident[:]), r=[dselb[i], C.dconst], w=[dbank[0]])
                P.op("dve", lambda e, pT=pT, i=i: e.tensor_scalar(out=selT[:, i * 128:(i + 1) * 128], in0=pT[:, 0:128], scalar1=-1.0, scalar2=None, op0=OP.add),
                     r=[dbank[0]], w=[dselT])

        for br in (2, 1):
            steps = []
            if br == 1:
                sel_transposes()
            klo = 0 if br == 1 else max(0, 4 * qc - 4)
            for kb in range(klo, 4 * qc + 4):
                for zz in range(2):
                    z = zown[zz]
                    zp = slice((z % 2) * 64, (z % 2) * 64 + 64)
                    OB = (6 if br == 1 else 4) + zz
                    i0 = max(0, kb - 4 * qc)
                    i1 = 3 if br == 1 else min(3, kb - 4 * qc + 4)
                    c0, c1 = 128 * i0, 128 * (i1 + 1)
                    cs = slice(c0, c1)
                    b = pit % 3
                    A = pit % 3
                    pit += 1
                    extra = []
                    if br == 1:
                        extra.append((cs, EW[:, kb * 128:(kb + 1) * 128], selT[:, cs], [dK, dselT]))
                    for i in range(i0, i1 + 1):
                        d = kb - (4 * qc + i)
                        isl = slice(i * 128, (i + 1) * 128)
                        if d == 0:
                            extra.append((isl, C.ident[:], TB[:, zz, 0, :], [dK, C.dconst]))
                        elif d == -1:
                            extra.append((isl, C.ident[:], TB[:, zz, 1, :], [dK, C.dconst]))
                        elif d == -4 and br == 2:
                            extra.append((isl, C.ident[:], negs[:, 2, :], [C.dconst]))
                    firstk = kb == klo
                    lastk = kb == 4 * qc + 3

                    def stA(z=z, zp=zp, br=br, kb=kb, cs=cs, c0=c0, c1=c1, A=A, b=b, extra=extra, qc=qc):
                        P.op("pe", lambda e: e.matmul(bank[A][:, cs], lhsT=kk[zp, br - 1, kb * 128:(kb + 1) * 128], rhs=qn[zp, z // 2, qc * 512 + c0:qc * 512 + c1],
                                                      start=True, stop=(len(extra) == 0)), r=[dkk, dqn], w=[dbank[A]])
                        for xi, (sl, lt, rh, dd) in enumerate(extra):
                            P.op("pe", lambda e, sl=sl, lt=lt, rh=rh, lastx=(xi == len(extra) - 1): e.matmul(bank[A][:, sl], lhsT=lt, rhs=rh, start=False, stop=lastx,
                                                                                                           skip_group_check=True), r=dd, w=[dbank[A]])
                        P.op("act", lambda e: e.activation(out=pb[b][:, cs], in_=bank[A][:, cs], func=AF.Exp, bias=chv[:, z:z + 1]), r=[dbank[A], dK], w=[dpb[b]])
                        P.op("pe", lambda e: e.matmul(bank[3][:, 0:512], lhsT=C.ident[:], rhs=EW[:, 0:512], start=True, stop=True), r=[], w=[])

                    def stB(zz=zz, br=br, kb=kb, i0=i0, i1=i1, b=b, OB=OB, firstk=firstk, lastk=lastk, qc=qc):
                        for i in range(i0, i1 + 1):
                            P.op("pe", lambda e, i=i: e.matmul(bank[OB][:, i * 65:(i + 1) * 65], lhsT=pb[b][:, i * 128:(i + 1) * 128], rhs=vs1[:, kb, br - 1, :],
                                                               start=(firstk and i == i0), stop=(lastk and i == i1), skip_group_check=True),
                                 r=[dpb[b], dvs], w=[dbank[OB]])
                        if not lastk:
                            return
                        dbank_of[0] = dbank[OB]
                        finalize(bank[OB][:, 0:260].rearrange("p (i c) -> p i c", c=65), zz, br, qc, False)
                        if br == 1:
                            ob = otc[0] % 2
                            otc[0] += 1
                            P.op("act", lambda e: e.activation(out=ot[ob][:], in_=onsa[:, zz, :, :], func=AF.Copy), r=[donsa], w=[dot[ob]])
                            P.dma("sp", o[qc * 512:(qc + 1) * 512, 128 + zz * 64:128 + (zz + 1) * 64].rearrange("(i p) c -> p i c", p=128), ot[ob][:],
                                  r=[dot[ob]], w=[C.do])
                            if zz == 1 and qc % 2 == 1 and C.og is not None:
                                c_ = qc // 2
                                P.coll("AllGather", o[c_ * 1024:(c_ + 1) * 1024, :], C.og[c_ * 4096:(c_ + 1) * 4096, :], GROUPS, r=[C.do], w=[C.dog])

                    steps.append((stA, stB))
            run_pipeline(steps, 2)
    stk.close()
    C.top.close()
    P.barrier()


import math

D_MODEL = 1024
W_SB = 512
OFF_SB_Q, OFF_SB_K, OFF_SB_V, OFF_NSA_Q = 0, 512, 1024, 1536
OFF_NSA_KV = 2048
OFF_NSA_GATE = OFF_NSA_KV + 3 * 2 * 2 * 64
IN_COLS = OFF_NSA_GATE + 24


def t5_bucket_np(dist):
    n = np.maximum(dist, 0)
    large = 16 + (np.log(np.maximum(n, 1).astype(np.float32) / np.float32(16)) / np.float32(math.log(128 / 16))
                  * np.float32(16)).astype(np.int32)
    large = np.minimum(large, 31)
    return np.where(n < 16, n, large)


_CONST_CACHE = {}


def consts_A(S):
    if S in _CONST_CACHE:
        return _CONST_CACHE[S]
    NC_ = S // 16 - 1
    k = np.arange(128)[:, None]
    q = np.arange(128)[None, :]
    negs = np.zeros((128, 3, 128), np.float32)
    negs[:, 0, :] = np.where(q > k, 0.0, NEG)
    negs[:, 1, :] = np.where(q >= k, 0.0, NEG)
    negs[:, 2, :] = np.where(q < k, 0.0, NEG)
    idx_tb = np.zeros((128, 2, 128), np.int64)
    idx_tb[:, 0, :] = t5_bucket_np(np.maximum(q - k, 0))
    idx_tb[:, 1, :] = t5_bucket_np(128 + q - k)
    q5 = np.arange(512)[None, None, :]
    dl = np.arange(5)[None, :, None]
    n_ = np.arange(128)[:, None, None]
    dist_c = 512 * dl + q5 - 16 * n_ - 31
    idx_tc = t5_bucket_np(np.maximum(dist_c, 0))
    negc = np.where(dist_c >= 0, 0.0, NEG).astype(np.float32)
    t = np.arange(128)[:, None]
    jp = np.arange(255)[None, :] - 127
    cur = (t >= 64).astype(np.int64)
    wsel = np.zeros((128, 255), np.float32)
    wsel[(jp == cur) | (jp == cur - 1)] = 1e4
    wsel[jp > cur] = -1.0
    rconst = np.zeros((128, 4, 193), np.float32)
    for c in range(4):
        n = c * 128 + np.arange(128)
        valid = n < NC_
        rconst[:, c, 64] = valid
        j = np.arange(128)[None, :]
        ov = (n[:, None] >= 4 * j - 1) & (n[:, None] <= 4 * j + 3) & valid[:, None]
        rconst[:, c, 65:193] = ov
    out = dict(negs=negs, idx_tb=idx_tb, idx_tc=idx_tc, negc=negc, wsel=wsel, rconst=rconst)
    _CONST_CACHE[S] = out
    return out


def prep_A(inp, layer, b, hg, S):
    cs = consts_A(S)
    g = hg // 2
    zo = [2 * (hg % 2), 2 * (hg % 2) + 1]
    L = zo + [z for z in range(4) if z not in zo]
    w_in = inp["w_in"][layer]
    hs = [2 * hg, 2 * hg + 1]

    def cols(off, n=64):
        return list(range(off, off + n))

    c_sb_fm = sum([cols(OFF_SB_Q + h * 64) for h in hs] + [cols(OFF_SB_K + h * 64) for h in hs], [])
    c_sb_tm = sum([cols(OFF_SB_V + h * 64) for h in hs], [])

    def kvcol(br, kvi):
        return cols(OFF_NSA_KV + ((br * 2 + kvi) * 2 + g) * 64)

    c_n_fm = sum([cols(OFF_NSA_Q + (g * 4 + z) * 64) for z in L], []) + kvcol(0, 0) + kvcol(0, 1) + kvcol(1, 0) + kvcol(1, 0) + kvcol(2, 0) + kvcol(2, 0)
    c_n_tm = kvcol(1, 1) + kvcol(2, 1) + sum([cols(OFF_NSA_GATE + (g * 4 + z) * 3, 3) for z in zo], [])
    heads = [g * 4 + z for z in L]
    tab = inp["rel_table"]
    tbg = np.stack([tab[cs["idx_tb"], heads[zz]] for zz in range(2)], axis=1)
    tcg = np.stack([tab[cs["idx_tc"], heads[z]] for z in range(4)], axis=1)
    chv = np.broadcast_to(tab[31, heads][None, :], (128, 4))
    w1 = np.concatenate([inp["cmp_w1_k"][layer].reshape(32, 64, 256).transpose(1, 0, 2),
                         inp["cmp_w1_v"][layer].reshape(32, 64, 256).transpose(1, 0, 2)], axis=0)
    posT = np.concatenate([inp["cmp_pos_k"][layer].T, inp["cmp_pos_v"][layer].T], axis=0)
    w2 = np.stack([inp["cmp_w2_k"][layer].reshape(2, 128, 64).transpose(1, 0, 2),
                   inp["cmp_w2_v"][layer].reshape(2, 128, 64).transpose(1, 0, 2)], axis=1)
    f = lambda a: np.ascontiguousarray(a, dtype=np.float32)
    return {
        "cT": f(inp["c"][b].reshape(8, 128).T), "wada": f(inp["w_ada"][layer][:, 0:2048]),
        "bada": f(inp["b_ada"][layer][None, 0:2048]), "gpre": f(inp["g_pre_mix"][layer][None]),
        "wfm_sb": f(w_in[:, c_sb_fm]), "wtm_sb": f(w_in[:, c_sb_tm]), "wfm_n": f(w_in[:, c_n_fm]), "wtm_n": f(w_in[:, c_n_tm]),
        "w1": f(w1), "posT": f(posT), "w2": f(w2), "rconst": cs["rconst"], "wsel": cs["wsel"],
        "tbg": f(tbg), "tcg": f(tcg), "chv": f(chv), "negs": cs["negs"], "negc": cs["negc"],
    }


def build_B(C, L, NTOK, n_exp, dff, x, dxres, og, dog, idx_d, y, dy, din):
    from contextlib import ExitStack
    NT = NTOK // 128
    NFC = dff // 128
    G = 4
    moe = n_exp > 1
    nc = C.nc
    cT = din("cTb", [128, 8]); wada = din("wadab", [1024, 4096]); bada = din("badab", [1, 4096])
    grow_d = din("grow", [1, 1024]); gpm_d = din("gpm", [1, 1024]); gpf_d = din("gpf", [1, 1024]); gqf_d = din("gqf", [1, 1024])
    wout_d = din("wout", [1024, 1024])
    grouped = (dff % 512 == 0)
    if grouped:
        wg_d = din("wg", [n_exp, dff // 512, 128, 4096]); wu_d = din("wu", [n_exp, dff // 512, 128, 4096]); wd_d = din("wd", [n_exp, dff // 512, 128, 4096])
    else:
        wg_d = din("wg", [n_exp, 1024, dff]); wu_d = din("wu", [n_exp, 1024, dff]); wd_d = din("wd", [n_exp, dff, 1024])
    if moe:
        wr_d = din("wr", [8, 1024]); br_d = din("br", [1, 8])
    x1s = nc.dram_tensor("x1s_%d" % L, [NTOK, 1024], F32).ap()
    P = C.P
    bank, dbank = C.bank, C.dbank
    top = ExitStack()
    epsb = top.enter_context(SBT(nc, "epsb", [128, 1], F32))
    P.op("pool", lambda e: e.memset(epsb[:], 1e-6), w=[C.dconst])
    mod = top.enter_context(SBT(nc, "mod", [128, 4096], F32))
    dmod = Dep()
    with ExitStack() as stk:
        ada_rows(C, stk, cT, wada, bada, 4096, mod, dmod)
    P.barrier()
    h2T = top.enter_context(SBT(nc, "h2T", [128, 8, NTOK], BF16))
    gw = top.enter_context(SBT(nc, "gw", [128, NT, 8], F32))
    dh2T, dgw, dx1s = Dep(), Dep(), Dep()
    gm = mod[:, 0:1024]; shf = mod[:, 1024:2048]; srf = mod[:, 2048:3072]; gf = mod[:, 3072:4096]
    with ExitStack() as stk:
        rows = stk.enter_context(SBT(nc, "rows", [128, 4, 1024], F32))
        drows = Dep()
        for i, d in enumerate((grow_d, gpm_d, gpf_d, gqf_d)):
            P.dma("sp", rows[:, i, :], d[0:1, 0:1024].to_broadcast([128, 1024]), w=[drows])
        P.op("dve", lambda e: e.tensor_tensor(out=gm, in0=gm, in1=rows[:, 1, :], op=OP.mult), r=[drows, dmod], w=[dmod])
        P.op("dve", lambda e: e.scalar_tensor_tensor(out=srf, in0=srf, scalar=1.0, in1=rows[:, 2, :], op0=OP.add, op1=OP.mult), r=[drows, dmod], w=[dmod])
        P.op("dve", lambda e: e.tensor_tensor(out=gf, in0=gf, in1=rows[:, 3, :], op=OP.mult), r=[drows, dmod], w=[dmod])
        wout = stk.enter_context(SBT(nc, "wout", [128, 8, 1024], BF16))
        dwo = Dep()
        P.dma("pool", wout[:], wout_d.rearrange("(kc p) n -> p kc n", p=128), w=[dwo])
        if moe:
            wrr = stk.enter_context(SBT(nc, "wrr", [128, 8, 1024], F32))
            brr = stk.enter_context(SBT(nc, "brr", [128, 8], F32))
            dwr = Dep()
            for e_ in range(8):
                P.dma("sp", wrr[:, e_, :], wr_d[e_:e_ + 1, 0:1024].to_broadcast([128, 1024]), w=[dwr])
            P.dma("sp", brr[:], br_d[0:1, 0:8].to_broadcast([128, 8]), w=[dwr])
        o2 = [stk.enter_context(SBT(nc, "o2_%d" % i, [128, 4, 256], F32)) for i in range(2)]
        idxs = stk.enter_context(SBT(nc, "idxs", [128, NT * 4], mybir.dt.int32))
        didx = Dep()
        P.dma("sp", idxs[:], idx_d[:, :], w=[didx])
        xt = [stk.enter_context(SBT(nc, "xt_%d" % i, [128, 1024], F32)) for i in range(2)]
        x1t = [stk.enter_context(SBT(nc, "x1t_%d" % i, [128, 1024], F32)) for i in range(2)]
        junk = stk.enter_context(SBT(nc, "junk", [128, 1024], BF16))
        junkf = stk.enter_context(SBT(nc, "junkf", [128, 1024], F32))
        tmp = stk.enter_context(SBT(nc, "tmp", [128, 1024], F32))
        h2f = stk.enter_context(SBT(nc, "h2f", [128, 1024], F32))
        mg = stk.enter_context(SBT(nc, "mg", [128, 1024], BF16))
        h2b = stk.enter_context(SBT(nc, "h2b", [128, 1024], BF16))
        mT = stk.enter_context(SBT(nc, "mT", [128, 8, 128], BF16))
        st = stk.enter_context(SBT(nc, "st", [128, 16], F32))
        lg = stk.enter_context(SBT(nc, "lg", [128, 32], F32))
        do2, dxt, dx1t = deps(2), deps(2), deps(2)
        djunk, djf, dtmp, dh2f, dmg, dh2b, dmT, dst_, dlg = (Dep() for _ in range(9))

        def rms(src_aps, n, col):
            nn = len(src_aps)
            for k_, (ap_, dd) in enumerate(src_aps):
                jo = junk[:, 0:n] if len(ap_.shape) == 2 else junk[:, 0:n].rearrange("p (r c) -> p r c", r=ap_.shape[1])
                P.op("act", lambda e, ap_=ap_, k_=k_, jo=jo: e.activation(out=jo, in_=ap_, func=AF.Square, accum_out=st[:, col + k_:col + k_ + 1]),
                     r=[dd], w=[djunk, dst_])
            P.op("act", lambda e: e.activation(out=st[:, col:col + nn], in_=st[:, col:col + nn], func=AF.Sqrt, scale=1.0 / n, bias=epsb[:, 0:1]),
                 r=[dst_, C.dconst], w=[dst_])
            P.op("dve", lambda e: e.reciprocal(out=st[:, col:col + nn], in_=st[:, col:col + nn]), r=[dst_], w=[dst_])

        steps = []
        for tt in range(NT):
            b = tt % 2
            tsl = slice(tt * 128, (tt + 1) * 128)
            wo = 2 * (tt % 2)

            def stA(tt=tt, b=b, tsl=tsl, wo=wo):
                for r_ in range(4):
                    P.dma("pool", o2[b][:, r_, :], None, r=[dog, didx], w=[do2[b]])
                    waits_, fn_, inc_ = P.q["pool"][-1]
                    P.q["pool"][-1] = (waits_, freeze(lambda e, r_=r_, b=b, tt=tt: e.indirect_dma_start(
                        out=o2[b][:, r_, :], out_offset=None, in_=og[:, :], in_offset=bass.IndirectOffsetOnAxis(ap=idxs[:, tt * 4 + r_:tt * 4 + r_ + 1], axis=0))), inc_)
                P.dma("sp", xt[b][:], x[tsl, :], r=[dxres], w=[dxt[b]])
                rms([(o2[b][:, :, 0:128], do2[b]), (o2[b][:, :, 128:256], do2[b])], 512, 0)
                for hf in range(2):
                    hs = slice(hf * 512, (hf + 1) * 512)
                    P.op("dve", lambda e, hf=hf, hs=hs, b=b: e.scalar_tensor_tensor(out=mg[:, hs].rearrange("p (r c) -> p r c", r=4), in0=o2[b][:, :, hf * 128:(hf + 1) * 128], scalar=st[:, hf:hf + 1],
                                                                                   in1=rows[:, 0, hs].rearrange("p (r c) -> p r c", r=4),
                                                                                   op0=OP.mult, op1=OP.mult), r=[do2[b], dst_, drows], w=[dmg])
                pT = bank[6][:].bitcast(BF16)
                for kc in range(8):
                    P.op("pe", lambda e, kc=kc, pT=pT: e.transpose(out=pT[:, kc * 128:(kc + 1) * 128], in_=mg[:, kc * 128:(kc + 1) * 128], identity=C.ident[:]),
                         r=[dmg, C.dconst], w=[dbank[6]])
                P.op("act", lambda e, pT=pT: e.activation(out=mT[:], in_=pT.rearrange("p (k t) -> p k t", k=8), func=AF.Copy), r=[dbank[6]], w=[dmT])
                for hf in range(2):
                    for kc in range(8):
                        P.op("pe", lambda e, kc=kc, hf=hf: e.matmul(bank[hf + wo][:], lhsT=mT[:, kc, :], rhs=wout[:, kc, hf * 512:(hf + 1) * 512], start=(kc == 0), stop=(kc == 7)),
                             r=[dmT, dwo], w=[dbank[hf + wo]])

            def stB(tt=tt, b=b, tsl=tsl, wo=wo):
                for hf in range(2):
                    P.op("act", lambda e, hf=hf: e.activation(out=junk[:, 0:512], in_=bank[hf + wo][:], func=AF.Square, accum_out=st[:, 4 + hf:5 + hf]), r=[dbank[hf + wo]], w=[djunk, dst_])
                P.op("dve", lambda e: e.tensor_tensor(out=st[:, 6:7], in0=st[:, 4:5], in1=st[:, 5:6], op=OP.add), r=[dst_], w=[dst_])
                P.op("act", lambda e: e.activation(out=st[:, 6:7], in_=st[:, 6:7], func=AF.Sqrt, scale=1.0 / 1024, bias=epsb[:, 0:1]), r=[dst_, C.dconst], w=[dst_])
                P.op("dve", lambda e: e.reciprocal(out=st[:, 6:7], in_=st[:, 6:7]), r=[dst_], w=[dst_])
                for hf in range(2):
                    hs = slice(hf * 512, (hf + 1) * 512)
                    P.op("dve", lambda e, hf=hf, hs=hs: e.scalar_tensor_tensor(out=tmp[:, hs], in0=bank[hf + wo][:], scalar=st[:, 6:7], in1=gm[:, hs], op0=OP.mult, op1=OP.mult),
                         r=[dbank[hf + wo], dst_, dmod], w=[dtmp])
                P.op("dve", lambda e, b=b: e.tensor_tensor(out=x1t[b][:], in0=tmp[:], in1=xt[b][:], op=OP.add), r=[dtmp, dxt[b]], w=[dx1t[b]])
                P.dma("sp", x1s[tsl, :], x1t[b][:], r=[dx1t[b]], w=[dx1s])
                rms([(x1t[b][:], dx1t[b])], 1024, 8)
                P.op("dve", lambda e, b=b: e.scalar_tensor_tensor(out=tmp[:], in0=x1t[b][:], scalar=st[:, 8:9], in1=srf, op0=OP.mult, op1=OP.mult),
                     r=[dx1t[b], dst_, dmod], w=[dtmp])
                P.op("dve", lambda e: e.tensor_tensor(out=h2f[:], in0=tmp[:], in1=shf, op=OP.add), r=[dtmp, dmod], w=[dh2f])
                P.op("act", lambda e: e.activation(out=h2b[:], in_=h2f[:], func=AF.Copy), r=[dh2f], w=[dh2b])
                pT2 = bank[7][:].bitcast(BF16)
                for kc in range(8):
                    P.op("pe", lambda e, kc=kc, pT2=pT2: e.transpose(out=pT2[:, kc * 128:(kc + 1) * 128], in_=h2b[:, kc * 128:(kc + 1) * 128], identity=C.ident[:]),
                         r=[dh2b, C.dconst], w=[dbank[7]])
                P.op("act", lambda e, pT2=pT2, tsl=tsl: e.activation(out=h2T[:, :, tsl], in_=pT2.rearrange("p (k t) -> p k t", k=8), func=AF.Copy), r=[dbank[7]], w=[dh2T])
                if moe:
                    for e_ in range(8):
                        P.op("dve", lambda e, e_=e_: e.scalar_tensor_tensor(out=junkf[:], in0=h2f[:], scalar=1.0, in1=wrr[:, e_, :], op0=OP.mult, op1=OP.mult,
                                                                            accum_out=lg[:, e_:e_ + 1]), r=[dh2f, dwr], w=[djf, dlg])
                    P.op("dve", lambda e: e.tensor_tensor(out=lg[:, 0:8], in0=lg[:, 0:8], in1=brr[:], op=OP.add), r=[dlg, dwr], w=[dlg])
                    P.op("dve", lambda e: e.max(out=lg[:, 8:16], in_=lg[:, 0:8]), r=[dlg], w=[dlg])
                    P.op("dve", lambda e: e.tensor_scalar(out=lg[:, 16:24], in0=lg[:, 0:8], scalar1=lg[:, 8:9], scalar2=None, op0=OP.subtract), r=[dlg], w=[dlg])
                    P.op("act", lambda e: e.activation(out=lg[:, 16:24], in_=lg[:, 16:24], func=AF.Exp), r=[dlg], w=[dlg])
                    P.op("dve", lambda e: e.scalar_tensor_tensor(out=lg[:, 16:24], in0=lg[:, 0:8], scalar=lg[:, 9:10], in1=lg[:, 16:24], op0=OP.is_ge, op1=OP.mult,
                                                                 accum_out=lg[:, 24:25]), r=[dlg], w=[dlg])
                    P.op("dve", lambda e: e.reciprocal(out=lg[:, 25:26], in_=lg[:, 24:25]), r=[dlg], w=[dlg])
                    P.op("dve", lambda e, tt=tt: e.tensor_scalar(out=gw[:, tt, :], in0=lg[:, 16:24], scalar1=lg[:, 25:26], scalar2=None, op0=OP.mult), r=[dlg], w=[dgw])

            steps.append((stA, stB))
        run_pipeline(steps, 2)
    P.barrier()

    acc = top.enter_context(SBT(nc, "acc", [128, NT, 1024], F32))
    dacc = Dep()
    with ExitStack() as stk:
        wgb = [stk.enter_context(SBT(nc, "wg%d" % i, [128, 8, G * 128], BF16)) for i in range(2)]
        wub = [stk.enter_context(SBT(nc, "wu%d" % i, [128, 8, G * 128], BF16)) for i in range(2)]
        wdb = [stk.enter_context(SBT(nc, "wd%d" % i, [128, G, 1024], BF16)) for i in range(2)]
        sg = [stk.enter_context(SBT(nc, "sg%d" % i, [128, 256], F32)) for i in range(2)]
        aT = [stk.enter_context(SBT(nc, "aT%d" % i, [128, 256], BF16)) for i in range(2)]
        dwb, dsg, daT = deps(2), deps(2), deps(2)
        groups_ = [(ex, g0, min(G, NFC - g0)) for ex in range(n_exp) for g0 in range(0, NFC, G)]

        def load_group(gi):
            ex, g0, ng = groups_[gi]
            wb_ = gi % 2
            cs = slice(g0 * 128, (g0 + ng) * 128)
            if grouped:
                gq = g0 // G
                P.dma("pool", wgb[wb_][:].rearrange("p k n -> p (k n)"), wg_d[ex, gq, :, :], w=[dwb[wb_]], max_dma_last_dim=8192)
                P.dma("pool", wub[wb_][:].rearrange("p k n -> p (k n)"), wu_d[ex, gq, :, :], w=[dwb[wb_]], max_dma_last_dim=8192)
                P.dma("pool", wdb[wb_][:].rearrange("p f n -> p (f n)"), wd_d[ex, gq, :, :], w=[dwb[wb_]], max_dma_last_dim=8192)
                return
            P.dma("pool", wgb[wb_][:, :, 0:ng * 128], wg_d[ex, :, cs].rearrange("(kc p) n -> p kc n", p=128), w=[dwb[wb_]])
            P.dma("pool", wub[wb_][:, :, 0:ng * 128], wu_d[ex, :, cs].rearrange("(kc p) n -> p kc n", p=128), w=[dwb[wb_]])
            P.dma("pool", wdb[wb_][:, 0:ng, :], wd_d[ex, cs, :].rearrange("(f p) n -> p f n", p=128), w=[dwb[wb_]])

        load_group(0)
        steps = []
        it = 0
        for gi, (ex, g0, ng) in enumerate(groups_):
            wb_ = gi % 2
            firstgrp = gi == 0
            for tc_ in range(NTOK // 256):
                tks = slice(tc_ * 256, (tc_ + 1) * 256)
                for f in range(ng):
                    b = it % 2
                    it += 1
                    GB, UB = b, 2 + b
                    pre = gi + 1 if (tc_ == 0 and f == 0 and gi + 1 < len(groups_)) else None

                    def stA(f=f, b=b, GB=GB, UB=UB, wb_=wb_, tks=tks):
                        for kc in range(8):
                            P.op("pe", lambda e, kc=kc: e.matmul(bank[GB][:, 0:256], lhsT=wgb[wb_][:, kc, f * 128:(f + 1) * 128], rhs=h2T[:, kc, tks],
                                                                 start=(kc == 0), stop=(kc == 7)), r=[dwb[wb_], dh2T], w=[dbank[GB]])
                        for kc in range(8):
                            P.op("pe", lambda e, kc=kc: e.matmul(bank[UB][:, 0:256], lhsT=wub[wb_][:, kc, f * 128:(f + 1) * 128], rhs=h2T[:, kc, tks],
                                                                 start=(kc == 0), stop=(kc == 7)), r=[dwb[wb_], dh2T], w=[dbank[UB]])
                        P.op("act", lambda e: e.activation(out=sg[b][:], in_=bank[GB][:, 0:256], func=AF.Silu), r=[dbank[GB]], w=[dsg[b]])
                        P.op("dve", lambda e: e.tensor_tensor(out=aT[b][:], in0=bank[UB][:, 0:256], in1=sg[b][:], op=OP.mult), r=[dbank[UB], dsg[b]], w=[daT[b]])

                    def stB(f=f, b=b, wb_=wb_, ng=ng, tc_=tc_, ex=ex, firstgrp=firstgrp, pre=pre):
                        if pre is not None:
                            load_group(pre)
                        for sub in range(2):
                            for hf in range(2):
                                ob = 4 + sub * 2 + hf
                                P.op("pe", lambda e, sub=sub, hf=hf, ob=ob: e.matmul(
                                    bank[ob][:], lhsT=aT[b][:, sub * 128:(sub + 1) * 128], rhs=wdb[wb_][:, f, hf * 512:(hf + 1) * 512],
                                    start=(f == 0), stop=(f == ng - 1)), r=[daT[b], dwb[wb_]], w=[dbank[ob]])
                        if f != ng - 1:
                            return
                        for sub in range(2):
                            tt = tc_ * 2 + sub
                            for hf in range(2):
                                ob = 4 + sub * 2 + hf
                                hs = slice(hf * 512, (hf + 1) * 512)
                                if moe:
                                    if firstgrp:
                                        P.op("dve", lambda e, ob=ob, tt=tt, hs=hs: e.tensor_scalar(out=acc[:, tt, hs], in0=bank[ob][:], scalar1=gw[:, tt, ex:ex + 1], scalar2=None, op0=OP.mult),
                                             r=[dbank[ob], dgw], w=[dacc])
                                    else:
                                        P.op("dve", lambda e, ob=ob, tt=tt, hs=hs: e.scalar_tensor_tensor(out=acc[:, tt, hs], in0=bank[ob][:], scalar=gw[:, tt, ex:ex + 1], in1=acc[:, tt, hs],
                                                                                                        op0=OP.mult, op1=OP.add), r=[dbank[ob], dgw], w=[dacc])
                                else:
                                    if firstgrp:
                                        P.op("dve", lambda e, ob=ob, tt=tt, hs=hs: e.tensor_copy(out=acc[:, tt, hs], in_=bank[ob][:]), r=[dbank[ob]], w=[dacc])
                                    else:
                                        P.op("dve", lambda e, ob=ob, tt=tt, hs=hs: e.tensor_tensor(out=acc[:, tt, hs], in0=bank[ob][:], in1=acc[:, tt, hs], op=OP.add), r=[dbank[ob]], w=[dacc])

                    steps.append((stA, stB))
        run_pipeline(steps, 2)
    P.barrier()
    with ExitStack() as stk:
        xt = [stk.enter_context(SBT(nc, "fx%d" % i, [128, 1024], F32)) for i in range(2)]
        yt = [stk.enter_context(SBT(nc, "fy%d" % i, [128, 1024], F32)) for i in range(2)]
        junk = stk.enter_context(SBT(nc, "fjunk", [128, 1024], BF16))
        st = stk.enter_context(SBT(nc, "fst", [128, 4], F32))
        dxt, dyt = deps(2), deps(2)
        djunk, dst_ = Dep(), Dep()
        for tt in range(NT):
            b = tt % 2
            tsl = slice(tt * 128, (tt + 1) * 128)
            P.dma("sp", xt[b][:], x1s[tsl, :], r=[dx1s], w=[dxt[b]])
            P.op("act", lambda e, tt=tt: e.activation(out=junk[:], in_=acc[:, tt, :], func=AF.Square, accum_out=st[:, 0:1]), r=[dacc], w=[djunk, dst_])
            P.op("act", lambda e: e.activation(out=st[:, 1:2], in_=st[:, 0:1], func=AF.Sqrt, scale=1.0 / 1024, bias=epsb[:, 0:1]), r=[dst_, C.dconst], w=[dst_])
            P.op("dve", lambda e: e.reciprocal(out=st[:, 2:3], in_=st[:, 1:2]), r=[dst_], w=[dst_])
            P.op("dve", lambda e, tt=tt, b=b: e.scalar_tensor_tensor(out=yt[b][:], in0=acc[:, tt, :], scalar=st[:, 2:3], in1=gf, op0=OP.mult, op1=OP.mult),
                 r=[dacc, dst_, dmod], w=[dyt[b]])
            P.op("dve", lambda e, b=b: e.tensor_tensor(out=yt[b][:], in0=yt[b][:], in1=xt[b][:], op=OP.add), r=[dxt[b], dyt[b]], w=[dyt[b]])
            P.dma("sp", y[tsl, :], yt[b][:], r=[dyt[b]], w=[dy])
    top.close()
    P.barrier()


def prep_B(inp, layer, b):
    f = lambda a: np.ascontiguousarray(a, dtype=np.float32)
    m = {
        "cTb": f(inp["c"][b].reshape(8, 128).T), "wadab": f(inp["w_ada"][layer][:, 2048:6144]), "badab": f(inp["b_ada"][layer][None, 2048:6144]),
        "grow": f(np.concatenate([inp["g_sb"][layer], inp["g_nsa"][layer]])[None]), "gpm": f(inp["g_post_mix"][layer][None]),
        "gpf": f(inp["g_pre_ffn"][layer][None]), "gqf": f(inp["g_post_ffn"][layer][None]), "wout": f(inp["w_out"][layer]),
    }
    i = layer // 2
    if layer % 2 == 0:
        m["wg"] = f(inp["ffn_w_gate"][i][None]); m["wu"] = f(inp["ffn_w_up"][i][None]); m["wd"] = f(inp["ffn_w_down"][i][None])
    else:
        def grp_in(w):
            E, _, dff = w.shape
            return w.reshape(E, 8, 128, dff // 512, 512).transpose(0, 3, 2, 1, 4).reshape(E, dff // 512, 128, 4096)

        def grp_dn(w):
            E, dff, _ = w.shape
            return w.reshape(E, dff // 512, 4, 128, 1024).transpose(0, 1, 3, 2, 4).reshape(E, dff // 512, 128, 4096)

        m["wg"] = f(grp_in(inp["moe_w_gate"][i])); m["wu"] = f(grp_in(inp["moe_w_up"][i])); m["wd"] = f(grp_dn(inp["moe_w_down"][i]))
        m["wr"] = f(inp["moe_w_router"][i].T); m["br"] = f(inp["moe_b_router"][i][None])
    return m


_PROG_CACHE = {}
GROUPS = [[0, 1, 2, 3], [4, 5, 6, 7]]


def build_fused(S):
    NTOK = 2 * S // 8
    NT = NTOK // 128
    nc = bass.Bass("TRN2", target_bir_lowering=False)
    C = mk_ctx(nc)
    P = C.P

    def mk_din(L):
        def din(name, shape):
            return nc.dram_tensor("%s_%d" % (name, L), shape, F32, kind="ExternalInput").ap()
        return din

    xb = nc.dram_tensor("xb", [S, 1024], F32, kind="ExternalInput").ap()
    xtok = nc.dram_tensor("xtok", [NTOK, 1024], F32, kind="ExternalInput").ap()
    idx_d = nc.dram_tensor("idxg", [128, NT * 4], mybir.dt.int32, kind="ExternalInput").ap()
    y = nc.dram_tensor("y", [NTOK, 1024], F32, kind="ExternalOutput").ap()
    o0 = nc.dram_tensor("o0", [S, 256], F32).ap(); og0 = nc.dram_tensor("og0", [4 * S, 256], F32).ap()
    o1 = nc.dram_tensor("o1", [S, 256], F32).ap(); og1 = nc.dram_tensor("og1", [4 * S, 256], F32).ap()
    xo0 = nc.dram_tensor("xo0", [NTOK, 1024], F32).ap(); xg1 = nc.dram_tensor("xg1", [4 * NTOK, 1024], F32).ap()
    def gather_o(o, og, dsrc, ddst):
        for c in range(S // 1024):
            P.coll("AllGather", o[c * 1024:(c + 1) * 1024, :], og[c * 4096:(c + 1) * 4096, :], GROUPS, r=[dsrc], w=[ddst])

    dx0 = Dep()
    C.dxsrc = lambda tt: dx0
    C.xtile = lambda tt: xb[tt * 128:(tt + 1) * 128, :]
    dog0 = Dep()
    C.og, C.dog = og0, dog0
    build_A(C, 0, S, xb, o0, mk_din(0)); build_A_nsa(C)
    dxo0 = Dep()
    build_B(C, 0, NTOK, 1, 2816, xtok, Dep(), og0, dog0, idx_d, xo0, dxo0, mk_din(0))
    dxg1 = deps(NTOK // 256)
    for c in range(NTOK // 256):
        P.coll("AllGather", xo0[c * 256:(c + 1) * 256, :], xg1[c * 1024:(c + 1) * 1024, :], GROUPS, r=[dxo0], w=[dxg1[c]])
    C.dxsrc = lambda tt: dxg1[((tt * 128) % NTOK) // 256]

    def xtile1(tt):
        t = tt * 128
        rank, row = t // NTOK, t % NTOK
        c, rr = row // 256, row % 256
        r0 = c * 1024 + rank * 256 + rr
        return xg1[r0:r0 + 128, :]

    C.xtile = xtile1
    dog1 = Dep()
    C.og, C.dog = og1, dog1
    build_A(C, 1, S, xg1, o1, mk_din(1)); build_A_nsa(C)
    dy = Dep()
    build_B(C, 1, NTOK, 8, 3584, xo0, dxo0, og1, dog1, idx_d, y, dy, mk_din(1))
    P.finish("sp")
    P.emit()
    return nc


def kernel(**inp):
    inp = {k: np.asarray(v) for k, v in inp.items()}
    x = np.ascontiguousarray(inp["x"], dtype=np.float32)
    B, S, _ = x.shape
    NTOK = B * S // 8
    NT = NTOK // 128
    if S not in _PROG_CACHE:
        _PROG_CACHE[S] = build_fused(S)
    nc = _PROG_CACHE[S]
    maps = []
    for cid in range(8):
        b, part = cid // 4, cid % 4
        m = {"xb": x[b], "xtok": np.ascontiguousarray(x[b, part * NTOK:(part + 1) * NTOK])}
        p = np.arange(128)[:, None, None]
        tt = np.arange(NT)[None, :, None]
        r = np.arange(4)[None, None, :]
        t0 = part * NTOK + tt * 128
        m["idxg"] = ((t0 // 1024) * 4096 + r * 1024 + (t0 % 1024) + p).reshape(128, NT * 4).astype(np.int32)
        for L in range(2):
            for k, v in prep_A(inp, L, b, part, S).items():
                m["%s_%d" % (k, L)] = v
            for k, v in prep_B(inp, L, b).items():
                m["%s_%d" % (k, L)] = v
        maps.append(m)
    res = run_bass_kernel_spmd(nc, maps, core_ids=list(range(8)))
    out = np.zeros((B, S, 1024), np.float32)
    for cid in range(8):
        b, part = cid // 4, cid % 4
        out[b, part * NTOK:(part + 1) * NTOK] = res.results[cid]["y"]
    return out
```

```python
import numpy as np
import ml_dtypes
import concourse.bass as bass
import concourse.mybir as mybir
from concourse.bass_utils import run_bass_kernel_spmd

F32 = mybir.dt.float32
BF16 = mybir.dt.bfloat16
AF = mybir.ActivationFunctionType
OP = mybir.AluOpType
AX = mybir.AxisListType

ENGS = ("pe", "act", "dve", "pool", "sp")
NEG = -30000.0
DEBUG = False


_NM = [0]


def SBT(nc, name, shape, dt):
    _NM[0] += 1
    return nc.sbuf_tensor("t%d_%s" % (_NM[0], name), shape, dt)


import types


def freeze(fn):
    if fn.__closure__ is None:
        return fn
    cells = []
    for c in fn.__closure__:
        try:
            cells.append(types.CellType(c.cell_contents))
        except ValueError:
            cells.append(c)
    return types.FunctionType(fn.__code__, fn.__globals__, fn.__name__, fn.__defaults__, tuple(cells))


class Dep:
    __slots__ = ("lw", "rd")

    def __init__(self):
        self.lw = []
        self.rd = []


def deps(n):
    return [Dep() for _ in range(n)]


class Prog:
    def __init__(self, nc, ring=6):
        self.nc = nc
        self.q = {e: [] for e in ENGS}
        self.cnt = {e: 0 for e in ENGS}
        self.sems = {}
        self.waited = {e: {} for e in ENGS}
        self.ring = ring
        self.dma_n = {"sp": 0, "pool": 0}
        self.fence = []
        self.colls = []
        for e in ("pe", "act", "dve", "pool"):
            self.sems[e] = nc.alloc_semaphore("s_" + e)
        for qn in ("sp", "pool"):
            for i in range(ring):
                self.sems[(qn, i)] = nc.alloc_semaphore("d_%s%d" % (qn, i))

    def barrier(self):
        ev = []
        for e in ("pe", "act", "dve", "pool"):
            if self.cnt[e] > 0:
                ev.append((e, self.cnt[e]))
        for qn in ("sp", "pool"):
            n = self.dma_n[qn]
            for slot in range(min(n, self.ring)):
                uses = (n - slot + self.ring - 1) // self.ring
                ev.append(((qn, slot), 16 * uses))
        ev.extend((c, 1) for c in self.colls)
        self.fence = ev

    def _deps(self, eng, r, w):
        evs = list(self.fence)
        for d in r:
            evs.extend(d.lw)
        for d in w:
            evs.extend(d.lw)
            evs.extend(d.rd)
        wd = self.waited[eng]
        best = {}
        for (k, v) in evs:
            if k == eng and eng == "pe":
                continue
            if wd.get(k, 0) >= v:
                continue
            if best.get(k, 0) < v:
                best[k] = v
        waits = []
        for k, v in best.items():
            wd[k] = v
            waits.append((k, v))
        return waits

    def _commit(self, ev, r, w):
        for d in r:
            d.rd.append(ev)
            if len(d.rd) > 64:
                d.rd = d.rd[-32:] if False else d.rd
        comp = ("pe", "act", "dve", "pool")
        isdma = ev[0] not in comp
        for d in w:
            if isdma and d.lw and d.lw[0][0] not in comp:
                d.lw = d.lw[-11:] + [ev]
            else:
                d.lw = [ev]
            d.rd = []

    def op(self, eng, fn, r=(), w=()):
        waits = self._deps(eng, r, w)
        self.cnt[eng] += 1
        ev = (eng, self.cnt[eng])
        self.q[eng].append((waits, freeze(fn), (eng, 1)))
        self._commit(ev, r, w)

    def dma(self, qn, out, in_, r=(), w=(), **kw):
        n = self.dma_n[qn]
        slot = n % self.ring
        key = (qn, slot)
        val = 16 * (n // self.ring + 1)
        waits = self._deps(qn, r, w)
        if n >= self.ring:
            pv = val - 16
            if self.waited[qn].get(key, 0) < pv:
                self.waited[qn][key] = pv
                waits.append((key, pv))
        self.dma_n[qn] = n + 1
        fn = (lambda e, out=out, in_=in_, kw=kw: e.dma_start(out=out, in_=in_, **kw))
        self.q[qn].append((waits, fn, (key, 16)))
        self._commit((key, val), r, w)

    def coll(self, kind, src, dst, groups, r=(), w=()):
        name = "coll%d" % len(self.colls)
        self.colls.append(name)
        self.sems[name] = self.nc.alloc_semaphore(name)
        waits = self._deps("pool", r, w)
        fn = freeze(lambda e: e.collective_compute(kind, OP.bypass, replica_groups=groups, ins=[src], outs=[dst]))
        self.q["pool"].append((waits, fn, (name, 1)))
        self._commit((name, 1), r, w)

    def finish(self, eng="sp"):
        waits = [(c, 1) for c in self.colls]
        for e in ("pe", "act", "dve", "pool"):
            if self.cnt[e] > 0 and e != eng:
                waits.append((e, self.cnt[e]))
        for qn in ("sp", "pool"):
            n = self.dma_n[qn]
            for slot in range(min(n, self.ring)):
                uses = (n - slot + self.ring - 1) // self.ring
                waits.append(((qn, slot), 16 * uses))
        self.q[eng].append((waits, None, None))

    def emit(self):
        nc = self.nc
        names = {"pe": "tensor", "act": "scalar", "dve": "vector", "pool": "gpsimd", "sp": "sync"}
        with nc.Block() as block:
            for e in ENGS:
                items = self.q[e]
                if not items:
                    continue

                def body(engobj, items=items):
                    for waits, fn, inc in items:
                        for (k, v) in waits:
                            engobj.wait_ge(self.sems[k], v)
                        if isinstance(fn, tuple):
                            fn[1](engobj)
                        elif fn is not None:
                            ins = fn(engobj)
                            ins.then_inc(self.sems[inc[0]], inc[1])

                getattr(block, names[e])(body)


def run_pipeline(steps, nst):
    n = len(steps)
    for it in range(n + nst - 1):
        for k in range(nst):
            i = it - k
            if 0 <= i < n and steps[i][k] is not None:
                steps[i][k]()


class Ctx:
    pass


def mk_ctx(nc):
    C = Ctx()
    C.nc = nc
    C.P = Prog(nc)
    C.psall = nc.alloc_psum_tensor("psall", [128, 8, 512], F32)
    C.bank = [C.psall[:, i, :] for i in range(8)]
    C.dbank = deps(8)
    P = C.P
    C.onesf = nc.alloc_sbuf_tensor("onesf", [128, 128], F32)
    C.ident = nc.alloc_sbuf_tensor("ident", [128, 128], BF16)
    C.onesb = nc.alloc_sbuf_tensor("onesb", [128, 128], BF16)
    C.dconst = Dep()
    P.op("pool", lambda e: e.memset(C.onesf[:], 1.0), w=[C.dconst])
    P.op("pool", lambda e: e.memset(C.onesb[:], 1.0), w=[C.dconst])
    P.op("pool", lambda e: e.affine_select(out=C.ident[:], in_=C.onesf[:], pattern=[[-1, 128]],
                                           compare_op=OP.is_equal, fill=0.0, base=0,
                                           channel_multiplier=1), r=[C.dconst], w=[C.dconst])
    return C


def ada_rows(C, stk, cT, wada, bada, ncols, out_tile, dout):
    nc, P = C.nc, C.P
    csb = stk.enter_context(SBT(nc, "ada_c", [128, 8], F32))
    ca = stk.enter_context(SBT(nc, "ada_ca", [128, 8], F32))
    crep = stk.enter_context(SBT(nc, "ada_crep", [128, 8, 128], F32))
    wbuf = [stk.enter_context(SBT(nc, "ada_w%d" % i, [128, 8, 512], F32)) for i in range(2)]
    brow = stk.enter_context(SBT(nc, "ada_b", [1, ncols], F32))
    dc, dca, dcr, db = Dep(), Dep(), Dep(), Dep()
    dw = deps(2)
    P.dma("sp", csb[:], cT[:, :], w=[dc])
    P.dma("sp", brow[:], bada[0:1, 0:ncols], w=[db])
    P.op("act", lambda e: e.activation(out=ca[:], in_=csb[:], func=AF.Silu), r=[dc], w=[dca])
    for kc in range(8):
        P.op("dve", lambda e, kc=kc: e.tensor_scalar(out=crep[:, kc, :], in0=C.onesf[:], scalar1=ca[:, kc:kc + 1],
                                                     scalar2=None, op0=OP.mult), r=[dca, C.dconst], w=[dcr])
    wv = wada.rearrange("(kc p) n -> p kc n", p=128)
    for j in range(ncols // 512):
        b = j % 2
        P.dma("sp", wbuf[b][:], wv[:, :, j * 512:(j + 1) * 512], w=[dw[b]])
        bk = C.bank[j % 2]
        dbk = C.dbank[j % 2]
        for kc in range(8):
            P.op("pe", lambda e, kc=kc, b=b, bk=bk: e.matmul(bk[:], lhsT=crep[:, kc, :], rhs=wbuf[b][:, kc, :],
                                                            start=(kc == 0), stop=False), r=[dcr, dw[b]], w=[dbk])
        P.op("pe", lambda e, j=j, bk=bk: e.matmul(bk[:], lhsT=C.onesf[0:1, :], rhs=brow[0:1, j * 512:(j + 1) * 512],
                                                  start=False, stop=True), r=[db, C.dconst], w=[dbk])
        P.op("dve", lambda e, j=j, bk=bk: e.tensor_copy(out=out_tile[:, j * 512:(j + 1) * 512], in_=bk[:]),
             r=[dbk], w=[dout])


def bcast_row(C, stk, name, src, n, out_tile, dout, col0=0):
    C.P.dma("sp", out_tile[:, col0:col0 + n], src[0:1, 0:n].to_broadcast([128, n]), w=[dout])


def prenorm_proj(C, stk0, S, x, srow, shrow, dmod, wfm_d, nfm, wtm_d, ntm, fm_evac, tm_evac):
    nc, P = C.nc, C.P
    from contextlib import ExitStack
    with ExitStack() as stk:
        wfm = stk.enter_context(SBT(nc, "pp_wfm", [128, 8, nfm], BF16))
        wtm = stk.enter_context(SBT(nc, "pp_wtm", [128, 8, ntm], BF16))
        xt = [stk.enter_context(SBT(nc, "pp_x%d" % i, [128, 1024], F32)) for i in range(2)]
        junk = stk.enter_context(SBT(nc, "pp_junk", [128, 1024], BF16))
        tmp = stk.enter_context(SBT(nc, "pp_tmp", [128, 1024], F32))
        hb = [stk.enter_context(SBT(nc, "pp_hb%d" % i, [128, 1024], BF16)) for i in range(2)]
        hT = [stk.enter_context(SBT(nc, "pp_hT%d" % i, [128, 8, 512], BF16)) for i in range(2)]
        st = stk.enter_context(SBT(nc, "pp_st", [128, 8], F32))
        dwf, dwt, djunk, dtmp, dst_ = Dep(), Dep(), Dep(), Dep(), Dep()
        dx, dhb, dhT = deps(2), deps(2), deps(2)
        P.dma("pool", wfm[:], wfm_d.rearrange("(kc p) n -> p kc n", p=128), w=[dwf])
        P.dma("pool", wtm[:], wtm_d.rearrange("(kc p) n -> p kc n", p=128), w=[dwt])
        xt.append(stk.enter_context(SBT(nc, "pp_x2", [128, 1024], F32)))
        hb.append(stk.enter_context(SBT(nc, "pp_hb2", [128, 1024], BF16)))
        st3 = [stk.enter_context(SBT(nc, "pp_st%d" % i, [128, 8], F32)) for i in range(3)]
        dx.append(Dep()); dhb.append(Dep())
        dst3 = deps(3)
        steps = []
        for tt in range(S // 128):
            ch, i = tt // 4, tt % 4
            hb_ = ch % 2
            b = tt % 3
            bk = 6 + (tt % 2)

            def stA(tt=tt, b=b):
                st_, dstx = st3[b], dst3[b]
                P.dma("sp", xt[b][:], C.xtile(tt), r=[C.dxsrc(tt)], w=[dx[b]])
                P.op("act", lambda e: e.activation(out=junk[:], in_=xt[b][:], func=AF.Square, accum_out=st_[:, 0:1]), r=[dx[b]], w=[djunk, dstx])
                P.op("act", lambda e: e.activation(out=st_[:, 1:2], in_=st_[:, 0:1], func=AF.Sqrt, scale=1.0 / 1024, bias=C.epsb[:, 0:1]), r=[dstx, C.dconst], w=[dstx])
                P.op("dve", lambda e: e.reciprocal(out=st_[:, 2:3], in_=st_[:, 1:2]), r=[dstx], w=[dstx])
                P.op("dve", lambda e: e.scalar_tensor_tensor(out=tmp[:], in0=xt[b][:], scalar=st_[:, 2:3], in1=srow[:], op0=OP.mult, op1=OP.mult),
                     r=[dx[b], dstx, dmod], w=[dtmp])
                P.op("dve", lambda e: e.tensor_tensor(out=hb[b][:], in0=tmp[:], in1=shrow[:], op=OP.add), r=[dtmp, dmod], w=[dhb[b]])

            def stB(tt=tt, b=b, bk=bk, i=i, hb_=hb_):
                pT = C.bank[bk][:].bitcast(BF16)
                for kc in range(8):
                    P.op("pe", lambda e, kc=kc: e.transpose(out=pT[:, kc * 128:(kc + 1) * 128], in_=hb[b][:, kc * 128:(kc + 1) * 128], identity=C.ident[:]),
                         r=[dhb[b], C.dconst], w=[C.dbank[bk]])
                P.op("act", lambda e: e.activation(out=hT[hb_][:, :, i * 128:(i + 1) * 128], in_=pT.rearrange("p (k t) -> p k t", k=8), func=AF.Copy),
                     r=[C.dbank[bk]], w=[dhT[hb_]])

            def stC(ch=ch, hb_=hb_):
                for g in range(nfm // 128):
                    bk2 = g % 3
                    for kc in range(8):
                        P.op("pe", lambda e, g=g, kc=kc, bk2=bk2: e.matmul(C.bank[bk2][:], lhsT=wfm[:, kc, g * 128:(g + 1) * 128], rhs=hT[hb_][:, kc, :],
                                                                         start=(kc == 0), stop=(kc == 7)), r=[dwf, dhT[hb_]], w=[C.dbank[bk2]])
                    fm_evac(g, ch, C.bank[bk2], C.dbank[bk2])
                for i2 in range(4):
                    bk2 = 3 + (i2 % 3)
                    for kc in range(8):
                        P.op("pe", lambda e, i2=i2, kc=kc, bk2=bk2: e.matmul(C.bank[bk2][:, 0:ntm], lhsT=hT[hb_][:, kc, i2 * 128:(i2 + 1) * 128], rhs=wtm[:, kc, :],
                                                                           start=(kc == 0), stop=(kc == 7)), r=[dwt, dhT[hb_]], w=[C.dbank[bk2]])
                    tm_evac(ch * 4 + i2, C.bank[bk2], C.dbank[bk2])

            steps.append((stA, stB, stC if i == 3 else None))
        run_pipeline(steps, 3)
    P.barrier()


def build_A(C, L, S, x, o, din):
    from contextlib import ExitStack
    NT, NQC = S // 128, S // 512
    NC_ = S // 16 - 1
    NCH = (NC_ + 127) // 128
    nc = C.nc
    cT = din("cT", [128, 8]); wada = din("wada", [1024, 2048]); bada = din("bada", [1, 2048])
    gpre = din("gpre", [1, 1024])
    wfm_sb = din("wfm_sb", [1024, 256]); wtm_sb = din("wtm_sb", [1024, 128])
    wfm_n = din("wfm_n", [1024, 640]); wtm_n = din("wtm_n", [1024, 134])
    w1_d = din("w1", [128, 32, 256]); posT_d = din("posT", [128, 32]); w2_d = din("w2", [128, 2, 2, 64])
    rconst_d = din("rconst", [128, 4, 193]); wsel_d = din("wsel", [128, 255])
    tbg_d = din("tbg", [128, 2, 2, 128]); tcg_d = din("tcg", [128, 4, 5, 512]); chv_d = din("chv", [128, 4])
    negs_d = din("negs", [128, 3, 128]); negc_d = din("negc", [128, 5, 512])
    P = C.P
    C.do = Dep()
    bank, dbank = C.bank, C.dbank
    top = ExitStack()
    C.epsb = top.enter_context(SBT(nc, "epsb", [128, 1], F32))
    P.op("pool", lambda e: e.memset(C.epsb[:], 1e-6), w=[C.dconst])
    mod = top.enter_context(SBT(nc, "mod", [128, 2048], F32))
    srow = top.enter_context(SBT(nc, "srow", [128, 1024], F32))
    dmod = Dep()
    with ExitStack() as stk:
        ada_rows(C, stk, cT, wada, bada, 2048, mod, dmod)
    P.barrier()
    bcast_row(C, None, "gpre", gpre, 1024, srow, dmod)
    P.op("dve", lambda e: e.scalar_tensor_tensor(out=srow[:], in0=mod[:, 1024:2048], scalar=1.0, in1=srow[:], op0=OP.add, op1=OP.mult),
         r=[dmod], w=[dmod])
    shrow = mod[:, 0:1024]
    negs = top.enter_context(SBT(nc, "negs", [128, 3, 128], BF16))
    NIU = top.enter_context(SBT(nc, "NIU", [128, 128], BF16))
    P.dma("pool", negs[:], negs_d[:, :, :], w=[C.dconst])
    P.op("pool", lambda e: e.memset(NIU[:], -1.0), w=[C.dconst])
    P.op("pool", lambda e: e.affine_select(out=NIU[:], in_=NIU[:], pattern=[[-1, 128]], compare_op=OP.is_ge, fill=0.0,
                                           base=0, channel_multiplier=1), r=[C.dconst], w=[C.dconst])
    ot = [top.enter_context(SBT(nc, "ot%d" % i, [128, 4, 64], F32)) for i in range(2)]
    dot = deps(2)
    otn = 0

    with ExitStack() as stk:
        qk = stk.enter_context(SBT(nc, "qk", [128, 2, S], BF16))
        vsb = stk.enter_context(SBT(nc, "vsb", [128, NT, 128], BF16))
        dqk, dv = Dep(), Dep()

        def fm_evac(g, ch, bk, dbk):
            sc = 0.125 if g == 0 else 1.0
            P.op("act", lambda e: e.activation(out=qk[:, g, ch * 512:(ch + 1) * 512], in_=bk[:], func=AF.Copy, scale=sc),
                 r=[dbk], w=[dqk])

        def tm_evac(tt, bk, dbk):
            P.op("dve", lambda e: e.tensor_copy(out=vsb[:, tt, :], in_=bk[:, 0:128]), r=[dbk], w=[dv])

        prenorm_proj(C, stk, S, x, srow, shrow, dmod, wfm_sb, 256, wtm_sb, 128, fm_evac, tm_evac)
        if DEBUG:
            dq = nc.dram_tensor("dbg_qk", [128, 2, S], BF16, kind="ExternalOutput").ap()
            dvv = nc.dram_tensor("dbg_v", [128, NT, 128], BF16, kind="ExternalOutput").ap()
            dmo = nc.dram_tensor("dbg_mod", [128, 2048], F32, kind="ExternalOutput").ap()
            P.dma("sp", dq[:, :, :], qk[:], r=[dqk])
            P.dma("sp", dvv[:, :, :], vsb[:], r=[dv])
            P.dma("sp", dmo[:, :], mod[:], r=[dmod])

        NB = 3
        nones = stk.enter_context(SBT(nc, "sb_nones", [128, 128], BF16))
        P.op("pool", lambda e: e.memset(nones[:], -1.0), w=[C.dconst])
        esb = [stk.enter_context(SBT(nc, "sb_e%d" % i, [128, 2, 512], F32)) for i in range(2)]
        spb = [stk.enter_context(SBT(nc, "sb_sp%d" % i, [128, 2, 512], BF16)) for i in range(NB)]
        wb = [stk.enter_context(SBT(nc, "sb_w%d" % i, [128, 2, 512], BF16)) for i in range(NB)]
        ssum = stk.enter_context(SBT(nc, "sb_ss", [128, 2, 512], F32))
        sbf = [stk.enter_context(SBT(nc, "sb_sb%d" % i, [128, 2, 512], BF16)) for i in range(3)]
        de, dsp, dwb, dsbf = deps(2), deps(NB), deps(NB), deps(3)
        dss = Dep()
        dpair = deps(3)
        psall = C.psall
        otc = [otn]
        steps = []
        cnt = 0
        for qc in range(NQC):
            for si, kb in enumerate(range(4 * qc + 3, -1, -1)):
                k3 = cnt % NB
                k2 = cnt % 2
                pk = (cnt - 1) % 3
                ck = cnt % 3
                cnt += 1
                first = si == 0
                last = kb == 0
                i0 = max(0, kb - 4 * qc)
                c0 = 128 * i0
                cs = slice(c0, 512)
                qs = slice(qc * 512 + c0, (qc + 1) * 512)
                diag = kb >= 4 * qc

                def stA(k3=k3, k2=k2, first=first, last=last, c0=c0, cs=cs, qs=qs, diag=diag, kb=kb, ck=ck):
                    pair = psall[:, 2 * k3:2 * k3 + 2, :]
                    if first:
                        P.op("pool", lambda e: e.memset(ssum[:].rearrange("p h c -> p (h c)"), 0.0), w=[dss])
                    for h in range(2):
                        hp = slice(h * 64, (h + 1) * 64)
                        P.op("pe", lambda e, h=h, hp=hp: e.matmul(pair[:, h, cs], lhsT=qk[hp, 1, kb * 128:(kb + 1) * 128], rhs=qk[hp, 0, qs], start=True, stop=not diag),
                             r=[dqk], w=[dpair[k3]])
                        if diag:
                            P.op("pe", lambda e, h=h: e.matmul(pair[:, h, c0:c0 + 128], lhsT=C.ident[:], rhs=negs[:, 0, :], start=False, stop=True, skip_group_check=True),
                                 r=[C.dconst], w=[dpair[k3]])
                    P.op("act", lambda e: e.activation(out=esb[k2][:, :, cs], in_=pair[:, :, cs], func=AF.Exp), r=[dpair[k3]], w=[de[k2]])
                    P.op("act", lambda e: e.activation(out=spb[k3][:, :, cs], in_=esb[k2][:, :, cs], func=AF.Ln, bias=1.0), r=[de[k2]], w=[dsp[k3]])
                    if not last:
                        P.op("pool", lambda e: e.tensor_tensor(out=ssum[:, :, cs], in0=spb[k3][:, :, cs], in1=ssum[:, :, cs], op=OP.add), r=[dsp[k3]], w=[dss])
                        P.op("dve", lambda e: e.tensor_copy(out=sbf[ck][:].rearrange("p h c -> p (h c)"), in_=ssum[:].rearrange("p h c -> p (h c)")), r=[dss], w=[dsbf[ck]])

                def stB(k3=k3, first=first, cs=cs, pk=pk):
                    pair = psall[:, 2 * k3:2 * k3 + 2, :]
                    for h in range(2):
                        P.op("pe", lambda e, h=h: e.matmul(pair[:, h, cs], lhsT=NIU[:], rhs=spb[k3][:, h, cs], start=False, stop=True, skip_group_check=True),
                             r=[dsp[k3], C.dconst], w=[dpair[k3]])
                        if not first:
                            P.op("pe", lambda e, h=h: e.matmul(pair[:, h, cs], lhsT=nones[:], rhs=sbf[pk][:, h, cs], start=False, stop=True, skip_group_check=True),
                                 r=[dsbf[pk], C.dconst], w=[dpair[k3]])
                    P.op("act", lambda e: e.activation(out=wb[k3][:, :, cs], in_=pair[:, :, cs], func=AF.Exp), r=[dpair[k3]], w=[dwb[k3]])

                def stC(k3=k3, first=first, last=last, i0=i0, kb=kb, qc=qc):
                    for h in range(2):
                        hp = slice(h * 64, (h + 1) * 64)
                        OB = 6 + h
                        for i in range(i0, 4):
                            P.op("pe", lambda e, i=i, h=h, hp=hp, OB=OB: e.matmul(bank[OB][:, i * 64:(i + 1) * 64], lhsT=wb[k3][:, h, i * 128:(i + 1) * 128], rhs=vsb[:, kb, hp],
                                                                               start=(first and i == i0), stop=(last and i == 3), skip_group_check=True),
                                 r=[dwb[k3], dv], w=[dbank[OB]])
                    if last:
                        for h in range(2):
                            OB = 6 + h
                            ob = otc[0] % 2
                            otc[0] += 1
                            P.op("dve", lambda e, ob=ob, OB=OB: e.tensor_copy(out=ot[ob][:].rearrange("p i c -> p (i c)"), in_=bank[OB][:, 0:256]), r=[dbank[OB]], w=[dot[ob]])
                            P.dma("sp", o[qc * 512:(qc + 1) * 512, h * 64:(h + 1) * 64].rearrange("(i p) c -> p i c", p=128), ot[ob][:], r=[dot[ob]], w=[C.do])

                steps.append((stA, stB, stC))
        run_pipeline(steps, 3)
        otn = otc[0]
    P.barrier()
    C.top = top
    C.ot, C.dot, C.otn = ot, dot, otn
    C.io = dict(x=x, o=o, srow=srow, shrow=shrow, dmod=dmod, negs=negs, wfm_n=wfm_n, wtm_n=wtm_n, w1_d=w1_d, posT_d=posT_d, w2_d=w2_d,
                rconst_d=rconst_d, wsel_d=wsel_d, negs_d=negs_d, tbg_d=tbg_d, tcg_d=tcg_d, chv_d=chv_d, negc_d=negc_d)
    C.S = S
    C.zown = [0, 1]
    return C


def build_A_nsa(C):
    from contextlib import ExitStack
    nc, P, S = C.nc, C.P, C.S
    bank, dbank = C.bank, C.dbank
    io = C.io
    NT, NQC = S // 128, S // 512
    NC_ = S // 16 - 1
    NCH = (NC_ + 127) // 128
    x, o, srow, shrow, dmod, negs = io["x"], io["o"], io["srow"], io["shrow"], io["dmod"], io["negs"]
    ot, dot = C.ot, C.dot
    stk = ExitStack()
    qn = stk.enter_context(SBT(nc, "qn", [128, 2, S], BF16))
    kk = stk.enter_context(SBT(nc, "kk", [128, 2, S], BF16))
    vs1 = stk.enter_context(SBT(nc, "vs1", [128, NT, 2, 65], BF16))
    gat = stk.enter_context(SBT(nc, "gat", [128, NT, 6], F32))
    kcT = stk.enter_context(SBT(nc, "kcT", [128, 512], BF16))
    Rc = stk.enter_context(SBT(nc, "Rc", [128, 4, 193], BF16))
    dqn, dkk, dvs, dgat, dkc, dRc = Dep(), Dep(), Dep(), Dep(), Dep(), Dep()
    P.op("pool", lambda e: e.memset(vs1[:].rearrange("p t b c -> p (t b c)"), 1.0), w=[dvs])
    P.op("pool", lambda e: e.memset(kcT[:], 0.0), w=[dkc])
    P.dma("pool", Rc[:], io["rconst_d"][:, :, :], w=[dRc])
    with ExitStack() as stk_raw:
        raw = stk_raw.enter_context(SBT(nc, "raw", [128, S], BF16))
        draw = Dep()

        def fm_evac(g, ch, bk, dbk):
            if g < 2:
                P.op("act", lambda e: e.activation(out=qn[:, g, ch * 512:(ch + 1) * 512], in_=bk[:], func=AF.Copy, scale=0.125), r=[dbk], w=[dqn])
            elif g == 2:
                P.op("act", lambda e: e.activation(out=raw[:, ch * 512:(ch + 1) * 512], in_=bk[:], func=AF.Copy), r=[dbk], w=[draw])
            else:
                P.op("act", lambda e: e.activation(out=kk[:, g - 3, ch * 512:(ch + 1) * 512], in_=bk[:], func=AF.Copy), r=[dbk], w=[dkk])

        def tm_evac(tt, bk, dbk):
            P.op("dve", lambda e: e.tensor_copy(out=vs1[:, tt, :, 0:64], in_=bk[:, 0:128].rearrange("p (b c) -> p b c", b=2)), r=[dbk], w=[dvs])
            P.op("act", lambda e: e.activation(out=gat[:, tt, :], in_=bk[:, 128:134], func=AF.Sigmoid), r=[dbk], w=[dgat])

        prenorm_proj(C, stk, S, x, srow, shrow, dmod, io["wfm_n"], 640, io["wtm_n"], 134, fm_evac, tm_evac)

        with ExitStack() as s2:
            w1 = s2.enter_context(SBT(nc, "w1", [128, 32, 256], BF16))
            posT = s2.enter_context(SBT(nc, "posT", [128, 32], BF16))
            w2 = s2.enter_context(SBT(nc, "w2", [128, 2, 2, 64], BF16))
            gT = s2.enter_context(SBT(nc, "gT", [128, 2, 2, 512], BF16))
            bv = s2.enter_context(SBT(nc, "bv", [128, 4], F32))
            t0 = s2.enter_context(SBT(nc, "cm_t0", [128, 512], F32))
            t1 = s2.enter_context(SBT(nc, "cm_t1", [128, 512], F32))
            dw1, dgT, dbv, dt0, dt1 = Dep(), Dep(), Dep(), Dep(), Dep()
            P.dma("pool", w1[:], io["w1_d"][:, :, :], w=[dw1])
            P.dma("pool", posT[:], io["posT_d"][:, :], w=[dw1])
            P.dma("pool", w2[:], io["w2_d"][:, :, :, :], w=[dw1])
            for kv in range(2):
                rp = slice(kv * 64, (kv + 1) * 64)
                for half in range(2):
                    hs = slice(half * 128, (half + 1) * 128)
                    for l in range(32):
                        P.op("pe", lambda e, l=l: e.matmul(bank[1][:, 0:1], lhsT=w1[rp, l, hs], rhs=posT[rp, l:l + 1], start=(l == 0), stop=(l == 31)),
                             r=[dw1], w=[dbank[1]])
                    ci = kv * 2 + half
                    P.op("dve", lambda e, ci=ci: e.tensor_copy(out=bv[:, ci:ci + 1], in_=bank[1][:, 0:1]), r=[dbank[1]], w=[dbv])
                    for l in range(32):
                        P.op("pe", lambda e, l=l: e.matmul(bank[0][:, 0:NC_], lhsT=w1[rp, l, hs], rhs=raw[rp, l:l + 16 * (NC_ - 1) + 1:16],
                                                           start=(l == 0), stop=(l == 31)), r=[dw1, draw], w=[dbank[0]])
                    P.op("act", lambda e, ci=ci: e.activation(out=t0[:, 0:NC_], in_=bank[0][:, 0:NC_], func=AF.Identity, bias=bv[:, ci:ci + 1]),
                         r=[dbank[0], dbv], w=[dt0])
                    P.op("dve", lambda e: e.tensor_tensor(out=t1[:, 0:NC_], in0=t0[:, 0:NC_], in1=t0[:, 0:NC_], op=OP.mult), r=[dt0], w=[dt1])
                    P.op("dve", lambda e: e.tensor_scalar(out=t1[:, 0:NC_], in0=t1[:, 0:NC_], scalar1=0.044715, scalar2=1.0, op0=OP.mult, op1=OP.add),
                         r=[dt1], w=[dt1])
                    P.op("dve", lambda e: e.tensor_tensor(out=t1[:, 0:NC_], in0=t1[:, 0:NC_], in1=t0[:, 0:NC_], op=OP.mult), r=[dt1, dt0], w=[dt1])
                    P.op("act", lambda e: e.activation(out=t1[:, 0:NC_], in_=t1[:, 0:NC_], func=AF.Sigmoid, scale=1.5957691216057308), r=[dt1], w=[dt1])
                    P.op("dve", lambda e, kv=kv, half=half: e.tensor_tensor(out=gT[:, kv, half, 0:NC_], in0=t1[:, 0:NC_], in1=t0[:, 0:NC_], op=OP.mult),
                         r=[dt1, dt0], w=[dgT])
            w2kd = s2.enter_context(SBT(nc, "w2kd", [128, 2, 128], BF16))
            dw2 = Dep()
            for half in range(2):
                for dup in range(2):
                    P.op("dve", lambda e, half=half, dup=dup: e.tensor_copy(out=w2kd[:, half, dup * 64:(dup + 1) * 64], in_=w2[:, 0, half, :]), r=[dw1], w=[dw2])
            for half in range(2):
                P.op("pe", lambda e, half=half: e.matmul(bank[2][:, 0:NC_], lhsT=w2kd[:, half, :], rhs=gT[:, 0, half, 0:NC_],
                                                         start=(half == 0), stop=(half == 1)), r=[dw2, dgT], w=[dbank[2]])
            P.op("act", lambda e: e.activation(out=kcT[:, 0:NC_], in_=bank[2][:, 0:NC_], func=AF.Copy), r=[dbank[2]], w=[dkc])
            for c in range(NCH):
                nv = min(128, NC_ - c * 128)
                for half in range(2):
                    P.op("pe", lambda e, half=half, c=c, nv=nv: e.matmul(bank[3][0:nv, 0:64], lhsT=gT[:, 1, half, c * 128:c * 128 + nv], rhs=w2[:, 1, half, :],
                                                                       start=(half == 0), stop=(half == 1)), r=[dw1, dgT], w=[dbank[3]])
                P.op("dve", lambda e, c=c, nv=nv: e.tensor_copy(out=Rc[0:nv, c, 0:64], in_=bank[3][0:nv, 0:64]), r=[dbank[3]], w=[dRc])
    P.barrier()

    EW = stk.enter_context(SBT(nc, "EW", [128, S], BF16))
    Tc = stk.enter_context(SBT(nc, "Tc", [128, 4, 5, 512], BF16))
    TB = stk.enter_context(SBT(nc, "TB", [128, 2, 2, 128], BF16))
    chv = stk.enter_context(SBT(nc, "chv", [128, 4], F32))
    wsel = stk.enter_context(SBT(nc, "wsel", [128, 255], F32))
    dK = Dep()
    P.dma("sp", chv[:], io["chv_d"][:, :], w=[dK])
    P.dma("sp", wsel[:], io["wsel_d"][:, :], w=[dK])
    P.op("pool", lambda e: e.memset(EW[:], -NEG), w=[dK])
    P.op("pool", lambda e: e.affine_select(out=EW[:], in_=EW[:], pattern=[[1, S]], compare_op=OP.is_ge, fill=0.0, base=0, channel_multiplier=-64),
         r=[dK], w=[dK])
    P.op("pool", lambda e: e.affine_select(out=EW[:], in_=EW[:], pattern=[[-1, S]], compare_op=OP.is_ge, fill=0.0, base=63, channel_multiplier=64),
         r=[dK], w=[dK])
    with ExitStack() as s3:
        tcf = s3.enter_context(SBT(nc, "tcf", [128, 5, 512], F32))
        ncf = s3.enter_context(SBT(nc, "ncf", [128, 5, 512], F32))
        tbf = s3.enter_context(SBT(nc, "tbf", [128, 2, 2, 128], F32))
        ngf = s3.enter_context(SBT(nc, "ngf", [128, 3, 128], F32))
        dtc, dnf, dtb = Dep(), Dep(), Dep()
        P.dma("sp", ncf[:], io["negc_d"][:, :, :], w=[dnf])
        P.dma("sp", ngf[:], io["negs_d"][:, :, :], w=[dnf])
        P.dma("sp", tbf[:], io["tbg_d"][:, :, :, :], w=[dtb])
        for z in range(4):
            P.dma("sp", tcf[:], io["tcg_d"][:, z, :, :], w=[dtc])
            P.op("dve", lambda e, z=z: e.scalar_tensor_tensor(out=Tc[:, z, :, :], in0=tcf[:], scalar=chv[:, z:z + 1], in1=ncf[:],
                                                              op0=OP.subtract, op1=OP.add), r=[dtc, dnf, dK], w=[dK])
        for zz in range(2):
            zc = C.zown[zz]
            P.op("dve", lambda e, zz=zz, zc=zc: e.scalar_tensor_tensor(out=TB[:, zz, 0, :], in0=tbf[:, zz, 0, :], scalar=chv[:, zc:zc + 1], in1=ngf[:, 1, :],
                                                                       op0=OP.subtract, op1=OP.add), r=[dtb, dnf, dK], w=[dK])
            P.op("dve", lambda e, zz=zz, zc=zc: e.tensor_scalar(out=TB[:, zz, 1, :], in0=tbf[:, zz, 1, :], scalar1=chv[:, zc:zc + 1], scalar2=None,
                                                                op0=OP.subtract), r=[dtb, dK], w=[dK])
    P.barrier()

    pb = [stk.enter_context(SBT(nc, "n_p%d" % i, [128, 512], BF16)) for i in range(2)]
    dpb = deps(2)
    imp = stk.enter_context(SBT(nc, "imp", [128, 4, 128], F32))
    onsa = stk.enter_context(SBT(nc, "onsa", [128, 2, 4, 64], F32))
    sm = stk.enter_context(SBT(nc, "n_sm", [128, 16], F32))
    sc = stk.enter_context(SBT(nc, "n_sc", [128, 128], F32))
    sc2 = stk.enter_context(SBT(nc, "n_sc2", [128, 128], F32))
    m8 = stk.enter_context(SBT(nc, "n_m8", [128, 16], F32))
    selb = [stk.enter_context(SBT(nc, "n_sel%d" % i, [128, 128], BF16)) for i in range(4)]
    dselb = deps(4)
    selT = stk.enter_context(SBT(nc, "n_selT", [128, 512], BF16))
    dimp, donsa, dsm, dsc, dsel, dselT = Dep(), Dep(), Dep(), Dep(), Dep(), Dep()
    pit = 0
    zown = C.zown
    otn = C.otn

    def finalize(OBv, zz, br, qc, first_branch):
        P.op("dve", lambda e: e.tensor_scalar(out=sm[:, 0:4], in0=OBv[:, :, 64], scalar1=1e-30, scalar2=None, op0=OP.max), r=[dbank_of[0]], w=[dsm])
        P.op("dve", lambda e: e.reciprocal(out=sm[:, 4:8], in_=sm[:, 0:4]), r=[dsm], w=[dsm])
        P.op("dve", lambda e: e.tensor_tensor(out=sm[:, 8:12], in0=sm[:, 4:8], in1=gat[:, 4 * qc:4 * qc + 4, zz * 3 + br], op=OP.mult), r=[dsm, dgat], w=[dsm])
        for i in range(4):
            if first_branch:
                P.op("dve", lambda e, i=i: e.tensor_scalar(out=onsa[:, zz, i, :], in0=OBv[:, i, 0:64], scalar1=sm[:, 8 + i:9 + i], scalar2=None, op0=OP.mult),
                     r=[dbank_of[0], dsm], w=[donsa])
            else:
                P.op("dve", lambda e, i=i: e.scalar_tensor_tensor(out=onsa[:, zz, i, :], in0=OBv[:, i, 0:64], scalar=sm[:, 8 + i:9 + i], in1=onsa[:, zz, i, :],
                                                                  op0=OP.mult, op1=OP.add), r=[dbank_of[0], dsm], w=[donsa])

    dbank_of = [None]
    pb3 = stk.enter_context(SBT(nc, "n_p2", [128, 512], BF16))
    pb = [pb[0], pb[1], pb3]
    dpb = dpb + [Dep()]
    otc = [otn]
    for qc in range(NQC):
        qsl = slice(qc * 512, (qc + 1) * 512)
        cmax = min(NCH - 1, qc // 4)
        steps = []
        for z in range(4):
            zp = slice((z % 2) * 64, (z % 2) * 64 + 64)
            O0 = 4 + 2 * (z % 2)
            for c in range(cmax + 1):
                b = pit % 3
                A = pit % 4
                pit += 1
                dl = qc - 4 * c
                near = 0 <= dl <= 4

                def stA(z=z, zp=zp, c=c, b=b, A=A, dl=dl, near=near, qsl=qsl):
                    P.op("pe", lambda e: e.matmul(bank[A][:], lhsT=kcT[zp, c * 128:(c + 1) * 128], rhs=qn[zp, z // 2, qsl], start=True, stop=not near),
                         r=[dkc, dqn], w=[dbank[A]])
                    if near:
                        P.op("pe", lambda e: e.matmul(bank[A][:], lhsT=C.ident[:], rhs=Tc[:, z, dl, :], start=False, stop=True), r=[dK, C.dconst], w=[dbank[A]])
                    P.op("act", lambda e: e.activation(out=pb[b][:], in_=bank[A][:], func=AF.Exp, bias=chv[:, z:z + 1]), r=[dbank[A], dK], w=[dpb[b]])

                def stB(z=z, c=c, b=b, O0=O0, cmax=cmax, qc=qc):
                    for i in range(4):
                        ob = O0 + i // 2
                        P.op("pe", lambda e, i=i, ob=ob: e.matmul(bank[ob][:, (i % 2) * 193:(i % 2) * 193 + 193], lhsT=pb[b][:, i * 128:(i + 1) * 128], rhs=Rc[:, c, :],
                                                                  start=(c == 0 and i % 2 == 0), stop=(c == cmax and i % 2 == 1), skip_group_check=True),
                             r=[dpb[b], dRc], w=[dbank[ob]])
                    if c != cmax:
                        return
                    own = z in zown
                    for i in range(4):
                        ob = O0 + i // 2
                        v = bank[ob][:, (i % 2) * 193:(i % 2) * 193 + 193]
                        P.op("dve", lambda e, v=v, i=i: e.tensor_scalar(out=sm[:, i:i + 1], in0=v[:, 64:65], scalar1=1e-30, scalar2=None, op0=OP.max), r=[dbank[ob]], w=[dsm])
                        P.op("dve", lambda e, i=i: e.reciprocal(out=sm[:, 4 + i:5 + i], in_=sm[:, i:i + 1]), r=[dsm], w=[dsm])
                        if z == 0:
                            P.op("dve", lambda e, v=v, i=i: e.tensor_scalar(out=imp[:, i, :], in0=v[:, 65:193], scalar1=sm[:, 4 + i:5 + i], scalar2=None, op0=OP.mult),
                                 r=[dbank[ob], dsm], w=[dimp])
                        else:
                            P.op("dve", lambda e, v=v, i=i: e.scalar_tensor_tensor(out=imp[:, i, :], in0=v[:, 65:193], scalar=sm[:, 4 + i:5 + i], in1=imp[:, i, :],
                                                                                   op0=OP.mult, op1=OP.add), r=[dbank[ob], dsm], w=[dimp])
                        if own:
                            zz = zown.index(z)
                            P.op("dve", lambda e, i=i, zz=zz: e.tensor_tensor(out=sm[:, 8 + i:9 + i], in0=sm[:, 4 + i:5 + i], in1=gat[:, 4 * qc + i, zz * 3:zz * 3 + 1], op=OP.mult),
                                 r=[dsm, dgat], w=[dsm])
                            P.op("dve", lambda e, v=v, i=i, zz=zz: e.tensor_scalar(out=onsa[:, zz, i, :], in0=v[:, 0:64], scalar1=sm[:, 8 + i:9 + i], scalar2=None, op0=OP.mult),
                                 r=[dbank[ob], dsm], w=[donsa])

                steps.append((stA, stB))
        run_pipeline(steps, 2)
        for i in range(4):
            qb = 4 * qc + i
            P.op("dve", lambda e, i=i, qb=qb: e.tensor_tensor(out=sc[:], in0=imp[:, i, :], in1=wsel[:, 127 - 2 * qb:255 - 2 * qb], op=OP.add), r=[dimp, dK], w=[dsc])
            if qb >= 1:
                P.op("dve", lambda e: e.tensor_scalar(out=sc[:, 0:1], in0=sc[:, 0:1], scalar1=1e4, scalar2=None, op0=OP.add), r=[dsc], w=[dsc])
            P.op("dve", lambda e: e.max(out=m8[:, 0:8], in_=sc[:]), r=[dsc], w=[dsm])
            P.op("dve", lambda e: e.match_replace(out=sc2[:], in_to_replace=m8[:, 0:8], in_values=sc[:], imm_value=-1e9), r=[dsc, dsm], w=[dsc])
            P.op("dve", lambda e: e.max(out=m8[:, 8:16], in_=sc2[:]), r=[dsc], w=[dsm])
            P.op("dve", lambda e: e.tensor_scalar(out=m8[:, 15:16], in0=m8[:, 15:16], scalar1=0.0, scalar2=None, op0=OP.max), r=[dsm], w=[dsm])
            P.op("dve", lambda e, i=i: e.tensor_scalar(out=selb[i][:], in0=sc[:], scalar1=m8[:, 15:16], scalar2=None, op0=OP.is_ge), r=[dsc, dsm], w=[dselb[i]])

        def sel_transposes():
            for i in range(4):
                pT = bank[0][:].bitcast(BF16)
                P.op("pe", lambda e, pT=pT, i=i: e.transpose(out=pT[:, 0:128], in_=selb[i][:], identity=C.ident[:]), r=[dselb[i], C.dconst], w=[dbank[0]])
                P.op("dve", lambda e, pT=pT, i=i: e.tensor_scalar(out=selT[:, i * 128:(i + 1) * 128], in0=pT[:, 0:128], scalar1=-1.0, scalar2=None, op0=OP.add),
                     r=[dbank[0]], w=[dselT])

        for br in (2, 1):
            steps = []
            if br == 1:
                sel_transposes()
            klo = 0 if br == 1 else max(0, 4 * qc - 4)
            for kb in range(klo, 4 * qc + 4):
                for zz in range(2):
                    z = zown[zz]
                    zp = slice((z % 2) * 64, (z % 2) * 64 + 64)
                    OB = (6 if br == 1 else 4) + zz
                    i0 = max(0, kb - 4 * qc)
                    i1 = 3 if br == 1 else min(3, kb - 4 * qc + 4)
                    c0, c1 = 128 * i0, 128 * (i1 + 1)
                    cs = slice(c0, c1)
                    b = pit % 3
                    A = pit % 3
                    pit += 1
                    extra = []
                    if br == 1:
                        extra.append((cs, EW[:, kb * 128:(kb + 1) * 128], selT[:, cs], [dK, dselT]))
                    for i in range(i0, i1 + 1):
                        d = kb - (4 * qc + i)
                        isl = slice(i * 128, (i + 1) * 128)
                        if d == 0:
                            extra.append((isl, C.ident[:], TB[:, zz, 0, :], [dK, C.dconst]))
                        elif d == -1:
                            extra.append((isl, C.ident[:], TB[:, zz, 1, :], [dK, C.dconst]))
                        elif d == -4 and br == 2:
                            extra.append((isl, C.ident[:], negs[:, 2, :], [C.dconst]))
                    firstk = kb == klo
                    lastk = kb == 4 * qc + 3

                    def stA(z=z, zp=zp, br=br, kb=kb, cs=cs, c0=c0, c1=c1, A=A, b=b, extra=extra, qc=qc):
                        P.op("pe", lambda e: e.matmul(bank[A][:, cs], lhsT=kk[zp, br - 1, kb * 128:(kb + 1) * 128], rhs=qn[zp, z // 2, qc * 512 + c0:qc * 512 + c1],
                                                      start=True, stop=(len(extra) == 0)), r=[dkk, dqn], w=[dbank[A]])
                        for xi, (sl, lt, rh, dd) in enumerate(extra):
                            P.op("pe", lambda e, sl=sl, lt=lt, rh=rh, lastx=(xi == len(extra) - 1): e.matmul(bank[A][:, sl], lhsT=lt, rhs=rh, start=False, stop=lastx,
                                                                                                           skip_group_check=True), r=dd, w=[dbank[A]])
                        P.op("act", lambda e: e.activation(out=pb[b][:, cs], in_=bank[A][:, cs], func=AF.Exp, bias=chv[:, z:z + 1]), r=[dbank[A], dK], w=[dpb[b]])
                        P.op("pe", lambda e: e.matmul(bank[3][:, 0:512], lhsT=C.ident[:], rhs=EW[:, 0:512], start=True, stop=True), r=[], w=[])
                        P.op("pe", lambda e: e.matmul(bank[3][:, 0:256], lhsT=C.ident[:], rhs=EW[:, 512:768], start=True, stop=True), r=[], w=[])

                    def stB(zz=zz, br=br, kb=kb, i0=i0, i1=i1, b=b, OB=OB, firstk=firstk, lastk=lastk, qc=qc):
                        for i in range(i0, i1 + 1):
                            P.op("pe", lambda e, i=i: e.matmul(bank[OB][:, i * 65:(i + 1) * 65], lhsT=pb[b][:, i * 128:(i + 1) * 128], rhs=vs1[:, kb, br - 1, :],
                                                               start=(firstk and i == i0), stop=(lastk and i == i1), skip_group_check=True),
                                 r=[dpb[b], dvs], w=[dbank[OB]])
                        if not lastk:
                            return
                        dbank_of[0] = dbank[OB]
                        finalize(bank[OB][:, 0:260].rearrange("p (i c) -> p i c", c=65), zz, br, qc, False)
                        if br == 1:
                            ob = otc[0] % 2
                            otc[0] += 1
                            P.op("act", lambda e: e.activation(out=ot[ob][:], in_=onsa[:, zz, :, :], func=AF.Copy), r=[donsa], w=[dot[ob]])
                            P.dma("sp", o[qc * 512:(qc + 1) * 512, 128 + zz * 64:128 + (zz + 1) * 64].rearrange("(i p) c -> p i c", p=128), ot[ob][:],
                                  r=[dot[ob]], w=[C.do])
                            if zz == 1 and qc % 2 == 1 and C.og is not None:
                                c_ = qc // 2
                                P.coll("AllGather", o[c_ * 1024:(c_ + 1) * 1024, :], C.og[c_ * 4096:(c_ + 1) * 4096, :], GROUPS, r=[C.do], w=[C.dog])

                    steps.append((stA, stB))
            run_pipeline(steps, 2)
    stk.close()
    C.top.close()
    P.barrier()


import math

D_MODEL = 1024
W_SB = 512
OFF_SB_Q, OFF_SB_K, OFF_SB_V, OFF_NSA_Q = 0, 512, 1024, 1536
OFF_NSA_KV = 2048
OFF_NSA_GATE = OFF_NSA_KV + 3 * 2 * 2 * 64
IN_COLS = OFF_NSA_GATE + 24


def t5_bucket_np(dist):
    n = np.maximum(dist, 0)
    large = 16 + (np.log(np.maximum(n, 1).astype(np.float32) / np.float32(16)) / np.float32(math.log(128 / 16))
                  * np.float32(16)).astype(np.int32)
    large = np.minimum(large, 31)
    return np.where(n < 16, n, large)


_CONST_CACHE = {}


def consts_A(S):
    if S in _CONST_CACHE:
        return _CONST_CACHE[S]
    NC_ = S // 16 - 1
    k = np.arange(128)[:, None]
    q = np.arange(128)[None, :]
    negs = np.zeros((128, 3, 128), np.float32)
    negs[:, 0, :] = np.where(q > k, 0.0, NEG)
    negs[:, 1, :] = np.where(q >= k, 0.0, NEG)
    negs[:, 2, :] = np.where(q < k, 0.0, NEG)
    idx_tb = np.zeros((128, 2, 128), np.int64)
    idx_tb[:, 0, :] = t5_bucket_np(np.maximum(q - k, 0))
    idx_tb[:, 1, :] = t5_bucket_np(128 + q - k)
    q5 = np.arange(512)[None, None, :]
    dl = np.arange(5)[None, :, None]
    n_ = np.arange(128)[:, None, None]
    dist_c = 512 * dl + q5 - 16 * n_ - 31
    idx_tc = t5_bucket_np(np.maximum(dist_c, 0))
    negc = np.where(dist_c >= 0, 0.0, NEG).astype(np.float32)
    t = np.arange(128)[:, None]
    jp = np.arange(255)[None, :] - 127
    cur = (t >= 64).astype(np.int64)
    wsel = np.zeros((128, 255), np.float32)
    wsel[(jp == cur) | (jp == cur - 1)] = 1e4
    wsel[jp > cur] = -1.0
    rconst = np.zeros((128, 4, 193), np.float32)
    for c in range(4):
        n = c * 128 + np.arange(128)
        valid = n < NC_
        rconst[:, c, 64] = valid
        j = np.arange(128)[None, :]
        ov = (n[:, None] >= 4 * j - 1) & (n[:, None] <= 4 * j + 3) & valid[:, None]
        rconst[:, c, 65:193] = ov
    out = dict(negs=negs, idx_tb=idx_tb, idx_tc=idx_tc, negc=negc, wsel=wsel, rconst=rconst)
    _CONST_CACHE[S] = out
    return out


def prep_A(inp, layer, b, hg, S):
    cs = consts_A(S)
    g = hg // 2
    zo = [2 * (hg % 2), 2 * (hg % 2) + 1]
    L = zo + [z for z in range(4) if z not in zo]
    w_in = inp["w_in"][layer]
    hs = [2 * hg, 2 * hg + 1]

    def cols(off, n=64):
        return list(range(off, off + n))

    c_sb_fm = sum([cols(OFF_SB_Q + h * 64) for h in hs] + [cols(OFF_SB_K + h * 64) for h in hs], [])
    c_sb_tm = sum([cols(OFF_SB_V + h * 64) for h in hs], [])

    def kvcol(br, kvi):
        return cols(OFF_NSA_KV + ((br * 2 + kvi) * 2 + g) * 64)

    c_n_fm = sum([cols(OFF_NSA_Q + (g * 4 + z) * 64) for z in L], []) + kvcol(0, 0) + kvcol(0, 1) + kvcol(1, 0) + kvcol(1, 0) + kvcol(2, 0) + kvcol(2, 0)
    c_n_tm = kvcol(1, 1) + kvcol(2, 1) + sum([cols(OFF_NSA_GATE + (g * 4 + z) * 3, 3) for z in zo], [])
    heads = [g * 4 + z for z in L]
    tab = inp["rel_table"]
    tbg = np.stack([tab[cs["idx_tb"], heads[zz]] for zz in range(2)], axis=1)
    tcg = np.stack([tab[cs["idx_tc"], heads[z]] for z in range(4)], axis=1)
    chv = np.broadcast_to(tab[31, heads][None, :], (128, 4))
    w1 = np.concatenate([inp["cmp_w1_k"][layer].reshape(32, 64, 256).transpose(1, 0, 2),
                         inp["cmp_w1_v"][layer].reshape(32, 64, 256).transpose(1, 0, 2)], axis=0)
    posT = np.concatenate([inp["cmp_pos_k"][layer].T, inp["cmp_pos_v"][layer].T], axis=0)
    w2 = np.stack([inp["cmp_w2_k"][layer].reshape(2, 128, 64).transpose(1, 0, 2),
                   inp["cmp_w2_v"][layer].reshape(2, 128, 64).transpose(1, 0, 2)], axis=1)
    f = lambda a: np.ascontiguousarray(a, dtype=np.float32)
    return {
        "cT": f(inp["c"][b].reshape(8, 128).T), "wada": f(inp["w_ada"][layer][:, 0:2048]),
        "bada": f(inp["b_ada"][layer][None, 0:2048]), "gpre": f(inp["g_pre_mix"][layer][None]),
        "wfm_sb": f(w_in[:, c_sb_fm]), "wtm_sb": f(w_in[:, c_sb_tm]), "wfm_n": f(w_in[:, c_n_fm]), "wtm_n": f(w_in[:, c_n_tm]),
        "w1": f(w1), "posT": f(posT), "w2": f(w2), "rconst": cs["rconst"], "wsel": cs["wsel"],
        "tbg": f(tbg), "tcg": f(tcg), "chv": f(chv), "negs": cs["negs"], "negc": cs["negc"],
    }


def build_B(C, L, NTOK, n_exp, dff, x, dxres, og, dog, idx_d, y, dy, din):
    from contextlib import ExitStack
    NT = NTOK // 128
    NFC = dff // 128
    G = 4
    moe = n_exp > 1
    nc = C.nc
    cT = din("cTb", [128, 8]); wada = din("wadab", [1024, 4096]); bada = din("badab", [1, 4096])
    grow_d = din("grow", [1, 1024]); gpm_d = din("gpm", [1, 1024]); gpf_d = din("gpf", [1, 1024]); gqf_d = din("gqf", [1, 1024])
    wout_d = din("wout", [1024, 1024])
    grouped = (dff % 512 == 0)
    if grouped:
        wg_d = din("wg", [n_exp, dff // 512, 128, 4096]); wu_d = din("wu", [n_exp, dff // 512, 128, 4096]); wd_d = din("wd", [n_exp, dff // 512, 128, 4096])
    else:
        wg_d = din("wg", [n_exp, 1024, dff]); wu_d = din("wu", [n_exp, 1024, dff]); wd_d = din("wd", [n_exp, dff, 1024])
    if moe:
        wr_d = din("wr", [8, 1024]); br_d = din("br", [1, 8])
    x1s = nc.dram_tensor("x1s_%d" % L, [NTOK, 1024], F32).ap()
    P = C.P
    bank, dbank = C.bank, C.dbank
    top = ExitStack()
    epsb = top.enter_context(SBT(nc, "epsb", [128, 1], F32))
    P.op("pool", lambda e: e.memset(epsb[:], 1e-6), w=[C.dconst])
    mod = top.enter_context(SBT(nc, "mod", [128, 4096], F32))
    dmod = Dep()
    with ExitStack() as stk:
        ada_rows(C, stk, cT, wada, bada, 4096, mod, dmod)
    P.barrier()
    h2T = top.enter_context(SBT(nc, "h2T", [128, 8, NTOK], BF16))
    gw = top.enter_context(SBT(nc, "gw", [128, NT, 8], F32))
    dh2T, dgw, dx1s = Dep(), Dep(), Dep()
    gm = mod[:, 0:1024]; shf = mod[:, 1024:2048]; srf = mod[:, 2048:3072]; gf = mod[:, 3072:4096]
    with ExitStack() as stk:
        rows = stk.enter_context(SBT(nc, "rows", [128, 4, 1024], F32))
        drows = Dep()
        for i, d in enumerate((grow_d, gpm_d, gpf_d, gqf_d)):
            P.dma("sp", rows[:, i, :], d[0:1, 0:1024].to_broadcast([128, 1024]), w=[drows])
        P.op("dve", lambda e: e.tensor_tensor(out=gm, in0=gm, in1=rows[:, 1, :], op=OP.mult), r=[drows, dmod], w=[dmod])
        P.op("dve", lambda e: e.scalar_tensor_tensor(out=srf, in0=srf, scalar=1.0, in1=rows[:, 2, :], op0=OP.add, op1=OP.mult), r=[drows, dmod], w=[dmod])
        P.op("dve", lambda e: e.tensor_tensor(out=gf, in0=gf, in1=rows[:, 3, :], op=OP.mult), r=[drows, dmod], w=[dmod])
        wout = stk.enter_context(SBT(nc, "wout", [128, 8, 1024], BF16))
        dwo = Dep()
        P.dma("pool", wout[:], wout_d.rearrange("(kc p) n -> p kc n", p=128), w=[dwo])
        if moe:
            wrr = stk.enter_context(SBT(nc, "wrr", [128, 8, 1024], F32))
            brr = stk.enter_context(SBT(nc, "brr", [128, 8], F32))
            dwr = Dep()
            for e_ in range(8):
                P.dma("sp", wrr[:, e_, :], wr_d[e_:e_ + 1, 0:1024].to_broadcast([128, 1024]), w=[dwr])
            P.dma("sp", brr[:], br_d[0:1, 0:8].to_broadcast([128, 8]), w=[dwr])
        o2 = [stk.enter_context(SBT(nc, "o2_%d" % i, [128, 4, 256], F32)) for i in range(2)]
        idxs = stk.enter_context(SBT(nc, "idxs", [128, NT * 4], mybir.dt.int32))
        didx = Dep()
        P.dma("sp", idxs[:], idx_d[:, :], w=[didx])
        xt = [stk.enter_context(SBT(nc, "xt_%d" % i, [128, 1024], F32)) for i in range(2)]
        x1t = [stk.enter_context(SBT(nc, "x1t_%d" % i, [128, 1024], F32)) for i in range(2)]
        junk = stk.enter_context(SBT(nc, "junk", [128, 1024], BF16))
        junkf = stk.enter_context(SBT(nc, "junkf", [128, 1024], F32))
        tmp = stk.enter_context(SBT(nc, "tmp", [128, 1024], F32))
        h2f = stk.enter_context(SBT(nc, "h2f", [128, 1024], F32))
        mg = stk.enter_context(SBT(nc, "mg", [128, 1024], BF16))
        h2b = stk.enter_context(SBT(nc, "h2b", [128, 1024], BF16))
        mT = stk.enter_context(SBT(nc, "mT", [128, 8, 128], BF16))
        st = stk.enter_context(SBT(nc, "st", [128, 16], F32))
        lg = stk.enter_context(SBT(nc, "lg", [128, 32], F32))
        do2, dxt, dx1t = deps(2), deps(2), deps(2)
        djunk, djf, dtmp, dh2f, dmg, dh2b, dmT, dst_, dlg = (Dep() for _ in range(9))

        def rms(src_aps, n, col):
            nn = len(src_aps)
            for k_, (ap_, dd) in enumerate(src_aps):
                jo = junk[:, 0:n] if len(ap_.shape) == 2 else junk[:, 0:n].rearrange("p (r c) -> p r c", r=ap_.shape[1])
                P.op("act", lambda e, ap_=ap_, k_=k_, jo=jo: e.activation(out=jo, in_=ap_, func=AF.Square, accum_out=st[:, col + k_:col + k_ + 1]),
                     r=[dd], w=[djunk, dst_])
            P.op("act", lambda e: e.activation(out=st[:, col:col + nn], in_=st[:, col:col + nn], func=AF.Sqrt, scale=1.0 / n, bias=epsb[:, 0:1]),
                 r=[dst_, C.dconst], w=[dst_])
            P.op("dve", lambda e: e.reciprocal(out=st[:, col:col + nn], in_=st[:, col:col + nn]), r=[dst_], w=[dst_])

        steps = []
        for tt in range(NT):
            b = tt % 2
            tsl = slice(tt * 128, (tt + 1) * 128)
            wo = 2 * (tt % 2)

            def stA(tt=tt, b=b, tsl=tsl, wo=wo):
                for r_ in range(4):
                    P.dma("pool", o2[b][:, r_, :], None, r=[dog, didx], w=[do2[b]])
                    waits_, fn_, inc_ = P.q["pool"][-1]
                    P.q["pool"][-1] = (waits_, freeze(lambda e, r_=r_, b=b, tt=tt: e.indirect_dma_start(
                        out=o2[b][:, r_, :], out_offset=None, in_=og[:, :], in_offset=bass.IndirectOffsetOnAxis(ap=idxs[:, tt * 4 + r_:tt * 4 + r_ + 1], axis=0))), inc_)
                P.dma("sp", xt[b][:], x[tsl, :], r=[dxres], w=[dxt[b]])
                rms([(o2[b][:, :, 0:128], do2[b]), (o2[b][:, :, 128:256], do2[b])], 512, 0)
                for hf in range(2):
                    hs = slice(hf * 512, (hf + 1) * 512)
                    P.op("dve", lambda e, hf=hf, hs=hs, b=b: e.scalar_tensor_tensor(out=mg[:, hs].rearrange("p (r c) -> p r c", r=4), in0=o2[b][:, :, hf * 128:(hf + 1) * 128], scalar=st[:, hf:hf + 1],
                                                                                   in1=rows[:, 0, hs].rearrange("p (r c) -> p r c", r=4),
                                                                                   op0=OP.mult, op1=OP.mult), r=[do2[b], dst_, drows], w=[dmg])
                pT = bank[6][:].bitcast(BF16)
                for kc in range(8):
                    P.op("pe", lambda e, kc=kc, pT=pT: e.transpose(out=pT[:, kc * 128:(kc + 1) * 128], in_=mg[:, kc * 128:(kc + 1) * 128], identity=C.ident[:]),
                         r=[dmg, C.dconst], w=[dbank[6]])
                P.op("act", lambda e, pT=pT: e.activation(out=mT[:], in_=pT.rearrange("p (k t) -> p k t", k=8), func=AF.Copy), r=[dbank[6]], w=[dmT])
                for hf in range(2):
                    for kc in range(8):
                        P.op("pe", lambda e, kc=kc, hf=hf: e.matmul(bank[hf + wo][:], lhsT=mT[:, kc, :], rhs=wout[:, kc, hf * 512:(hf + 1) * 512], start=(kc == 0), stop=(kc == 7)),
                             r=[dmT, dwo], w=[dbank[hf + wo]])

            def stB(tt=tt, b=b, tsl=tsl, wo=wo):
                for hf in range(2):
                    P.op("act", lambda e, hf=hf: e.activation(out=junk[:, 0:512], in_=bank[hf + wo][:], func=AF.Square, accum_out=st[:, 4 + hf:5 + hf]), r=[dbank[hf + wo]], w=[djunk, dst_])
                P.op("dve", lambda e: e.tensor_tensor(out=st[:, 6:7], in0=st[:, 4:5], in1=st[:, 5:6], op=OP.add), r=[dst_], w=[dst_])
                P.op("act", lambda e: e.activation(out=st[:, 6:7], in_=st[:, 6:7], func=AF.Sqrt, scale=1.0 / 1024, bias=epsb[:, 0:1]), r=[dst_, C.dconst], w=[dst_])
                P.op("dve", lambda e: e.reciprocal(out=st[:, 6:7], in_=st[:, 6:7]), r=[dst_], w=[dst_])
                for hf in range(2):
                    hs = slice(hf * 512, (hf + 1) * 512)
                    P.op("dve", lambda e, hf=hf, hs=hs: e.scalar_tensor_tensor(out=tmp[:, hs], in0=bank[hf + wo][:], scalar=st[:, 6:7], in1=gm[:, hs], op0=OP.mult, op1=OP.mult),
                         r=[dbank[hf + wo], dst_, dmod], w=[dtmp])
                P.op("dve", lambda e, b=b: e.tensor_tensor(out=x1t[b][:], in0=tmp[:], in1=xt[b][:], op=OP.add), r=[dtmp, dxt[b]], w=[dx1t[b]])
                P.dma("sp", x1s[tsl, :], x1t[b][:], r=[dx1t[b]], w=[dx1s])
                rms([(x1t[b][:], dx1t[b])], 1024, 8)
                P.op("dve", lambda e, b=b: e.scalar_tensor_tensor(out=tmp[:], in0=x1t[b][:], scalar=st[:, 8:9], in1=srf, op0=OP.mult, op1=OP.mult),
                     r=[dx1t[b], dst_, dmod], w=[dtmp])
                P.op("dve", lambda e: e.tensor_tensor(out=h2f[:], in0=tmp[:], in1=shf, op=OP.add), r=[dtmp, dmod], w=[dh2f])
                P.op("act", lambda e: e.activation(out=h2b[:], in_=h2f[:], func=AF.Copy), r=[dh2f], w=[dh2b])
                pT2 = bank[7][:].bitcast(BF16)
                for kc in range(8):
                    P.op("pe", lambda e, kc=kc, pT2=pT2: e.transpose(out=pT2[:, kc * 128:(kc + 1) * 128], in_=h2b[:, kc * 128:(kc + 1) * 128], identity=C.ident[:]),
                         r=[dh2b, C.dconst], w=[dbank[7]])
                P.op("act", lambda e, pT2=pT2, tsl=tsl: e.activation(out=h2T[:, :, tsl], in_=pT2.rearrange("p (k t) -> p k t", k=8), func=AF.Copy), r=[dbank[7]], w=[dh2T])
                if moe:
                    for e_ in range(8):
                        P.op("dve", lambda e, e_=e_: e.scalar_tensor_tensor(out=junkf[:], in0=h2f[:], scalar=1.0, in1=wrr[:, e_, :], op0=OP.mult, op1=OP.mult,
                                                                            accum_out=lg[:, e_:e_ + 1]), r=[dh2f, dwr], w=[djf, dlg])
                    P.op("dve", lambda e: e.tensor_tensor(out=lg[:, 0:8], in0=lg[:, 0:8], in1=brr[:], op=OP.add), r=[dlg, dwr], w=[dlg])
                    P.op("dve", lambda e: e.max(out=lg[:, 8:16], in_=lg[:, 0:8]), r=[dlg], w=[dlg])
                    P.op("dve", lambda e: e.tensor_scalar(out=lg[:, 16:24], in0=lg[:, 0:8], scalar1=lg[:, 8:9], scalar2=None, op0=OP.subtract), r=[dlg], w=[dlg])
                    P.op("act", lambda e: e.activation(out=lg[:, 16:24], in_=lg[:, 16:24], func=AF.Exp), r=[dlg], w=[dlg])
                    P.op("dve", lambda e: e.scalar_tensor_tensor(out=lg[:, 16:24], in0=lg[:, 0:8], scalar=lg[:, 9:10], in1=lg[:, 16:24], op0=OP.is_ge, op1=OP.mult,
                                                                 accum_out=lg[:, 24:25]), r=[dlg], w=[dlg])
                    P.op("dve", lambda e: e.reciprocal(out=lg[:, 25:26], in_=lg[:, 24:25]), r=[dlg], w=[dlg])
                    P.op("dve", lambda e, tt=tt: e.tensor_scalar(out=gw[:, tt, :], in0=lg[:, 16:24], scalar1=lg[:, 25:26], scalar2=None, op0=OP.mult), r=[dlg], w=[dgw])

            steps.append((stA, stB))
        run_pipeline(steps, 2)
    P.barrier()

    acc = top.enter_context(SBT(nc, "acc", [128, NT, 1024], F32))
    dacc = Dep()
    with ExitStack() as stk:
        wgb = [stk.enter_context(SBT(nc, "wg%d" % i, [128, 8, G * 128], BF16)) for i in range(2)]
        wub = [stk.enter_context(SBT(nc, "wu%d" % i, [128, 8, G * 128], BF16)) for i in range(2)]
        wdb = [stk.enter_context(SBT(nc, "wd%d" % i, [128, G, 1024], BF16)) for i in range(2)]
        sg = [stk.enter_context(SBT(nc, "sg%d" % i, [128, 256], F32)) for i in range(2)]
        aT = [stk.enter_context(SBT(nc, "aT%d" % i, [128, 256], BF16)) for i in range(2)]
        dwb, dsg, daT = deps(2), deps(2), deps(2)
        groups_ = [(ex, g0, min(G, NFC - g0)) for ex in range(n_exp) for g0 in range(0, NFC, G)]

        def load_group(gi):
            ex, g0, ng = groups_[gi]
            wb_ = gi % 2
            cs = slice(g0 * 128, (g0 + ng) * 128)
            if grouped:
                gq = g0 // G
                P.dma("pool", wgb[wb_][:].rearrange("p k n -> p (k n)"), wg_d[ex, gq, :, :], w=[dwb[wb_]], max_dma_last_dim=8192)
                P.dma("pool", wub[wb_][:].rearrange("p k n -> p (k n)"), wu_d[ex, gq, :, :], w=[dwb[wb_]], max_dma_last_dim=8192)
                P.dma("pool", wdb[wb_][:].rearrange("p f n -> p (f n)"), wd_d[ex, gq, :, :], w=[dwb[wb_]], max_dma_last_dim=8192)
                return
            P.dma("pool", wgb[wb_][:, :, 0:ng * 128], wg_d[ex, :, cs].rearrange("(kc p) n -> p kc n", p=128), w=[dwb[wb_]])
            P.dma("pool", wub[wb_][:, :, 0:ng * 128], wu_d[ex, :, cs].rearrange("(kc p) n -> p kc n", p=128), w=[dwb[wb_]])
            P.dma("pool", wdb[wb_][:, 0:ng, :], wd_d[ex, cs, :].rearrange("(f p) n -> p f n", p=128), w=[dwb[wb_]])

        load_group(0)
        steps = []
        it = 0
        for gi, (ex, g0, ng) in enumerate(groups_):
            wb_ = gi % 2
            firstgrp = gi == 0
            for tc_ in range(NTOK // 256):
                tks = slice(tc_ * 256, (tc_ + 1) * 256)
                for f in range(ng):
                    b = it % 2
                    it += 1
                    GB, UB = b, 2 + b
                    pre = gi + 1 if (tc_ == 0 and f == 0 and gi + 1 < len(groups_)) else None

                    def stA(f=f, b=b, GB=GB, UB=UB, wb_=wb_, tks=tks):
                        for kc in range(8):
                            P.op("pe", lambda e, kc=kc: e.matmul(bank[GB][:, 0:256], lhsT=wgb[wb_][:, kc, f * 128:(f + 1) * 128], rhs=h2T[:, kc, tks],
                                                                 start=(kc == 0), stop=(kc == 7)), r=[dwb[wb_], dh2T], w=[dbank[GB]])
                        for kc in range(8):
                            P.op("pe", lambda e, kc=kc: e.matmul(bank[UB][:, 0:256], lhsT=wub[wb_][:, kc, f * 128:(f + 1) * 128], rhs=h2T[:, kc, tks],
                                                                 start=(kc == 0), stop=(kc == 7)), r=[dwb[wb_], dh2T], w=[dbank[UB]])
                        P.op("act", lambda e: e.activation(out=sg[b][:], in_=bank[GB][:, 0:256], func=AF.Silu), r=[dbank[GB]], w=[dsg[b]])
                        P.op("dve", lambda e: e.tensor_tensor(out=aT[b][:], in0=bank[UB][:, 0:256], in1=sg[b][:], op=OP.mult), r=[dbank[UB], dsg[b]], w=[daT[b]])

                    def stB(f=f, b=b, wb_=wb_, ng=ng, tc_=tc_, ex=ex, firstgrp=firstgrp, pre=pre):
                        if pre is not None:
                            load_group(pre)
                        for sub in range(2):
                            for hf in range(2):
                                ob = 4 + sub * 2 + hf
                                P.op("pe", lambda e, sub=sub, hf=hf, ob=ob: e.matmul(
                                    bank[ob][:], lhsT=aT[b][:, sub * 128:(sub + 1) * 128], rhs=wdb[wb_][:, f, hf * 512:(hf + 1) * 512],
                                    start=(f == 0), stop=(f == ng - 1)), r=[daT[b], dwb[wb_]], w=[dbank[ob]])
                        if f != ng - 1:
                            return
                        for sub in range(2):
                            tt = tc_ * 2 + sub
                            for hf in range(2):
                                ob = 4 + sub * 2 + hf
                                hs = slice(hf * 512, (hf + 1) * 512)
                                if moe:
                                    if firstgrp:
                                        P.op("dve", lambda e, ob=ob, tt=tt, hs=hs: e.tensor_scalar(out=acc[:, tt, hs], in0=bank[ob][:], scalar1=gw[:, tt, ex:ex + 1], scalar2=None, op0=OP.mult),
                                             r=[dbank[ob], dgw], w=[dacc])
                                    else:
                                        P.op("dve", lambda e, ob=ob, tt=tt, hs=hs: e.scalar_tensor_tensor(out=acc[:, tt, hs], in0=bank[ob][:], scalar=gw[:, tt, ex:ex + 1], in1=acc[:, tt, hs],
                                                                                                        op0=OP.mult, op1=OP.add), r=[dbank[ob], dgw], w=[dacc])
                                else:
                                    if firstgrp:
                                        P.op("dve", lambda e, ob=ob, tt=tt, hs=hs: e.tensor_copy(out=acc[:, tt, hs], in_=bank[ob][:]), r=[dbank[ob]], w=[dacc])
                                    else:
                                        P.op("dve", lambda e, ob=ob, tt=tt, hs=hs: e.tensor_tensor(out=acc[:, tt, hs], in0=bank[ob][:], in1=acc[:, tt, hs], op=OP.add), r=[dbank[ob]], w=[dacc])

                    steps.append((stA, stB))
        run_pipeline(steps, 2)
    P.barrier()
    with ExitStack() as stk:
        xt = [stk.enter_context(SBT(nc, "fx%d" % i, [128, 1024], F32)) for i in range(2)]
        yt = [stk.enter_context(SBT(nc, "fy%d" % i, [128, 1024], F32)) for i in range(2)]
        junk = stk.enter_context(SBT(nc, "fjunk", [128, 1024], BF16))
        st = stk.enter_context(SBT(nc, "fst", [128, 4], F32))
        dxt, dyt = deps(2), deps(2)
        djunk, dst_ = Dep(), Dep()
        for tt in range(NT):
            b = tt % 2
            tsl = slice(tt * 128, (tt + 1) * 128)
            P.dma("sp", xt[b][:], x1s[tsl, :], r=[dx1s], w=[dxt[b]])
            P.op("act", lambda e, tt=tt: e.activation(out=junk[:], in_=acc[:, tt, :], func=AF.Square, accum_out=st[:, 0:1]), r=[dacc], w=[djunk, dst_])
            P.op("act", lambda e: e.activation(out=st[:, 1:2], in_=st[:, 0:1], func=AF.Sqrt, scale=1.0 / 1024, bias=epsb[:, 0:1]), r=[dst_, C.dconst], w=[dst_])
            P.op("dve", lambda e: e.reciprocal(out=st[:, 2:3], in_=st[:, 1:2]), r=[dst_], w=[dst_])
            P.op("dve", lambda e, tt=tt, b=b: e.scalar_tensor_tensor(out=yt[b][:], in0=acc[:, tt, :], scalar=st[:, 2:3], in1=gf, op0=OP.mult, op1=OP.mult),
                 r=[dacc, dst_, dmod], w=[dyt[b]])
            P.op("dve", lambda e, b=b: e.tensor_tensor(out=yt[b][:], in0=yt[b][:], in1=xt[b][:], op=OP.add), r=[dxt[b], dyt[b]], w=[dyt[b]])
            P.dma("sp", y[tsl, :], yt[b][:], r=[dyt[b]], w=[dy])
    top.close()
    P.barrier()


def prep_B(inp, layer, b):
    f = lambda a: np.ascontiguousarray(a, dtype=np.float32)
    m = {
        "cTb": f(inp["c"][b].reshape(8, 128).T), "wadab": f(inp["w_ada"][layer][:, 2048:6144]), "badab": f(inp["b_ada"][layer][None, 2048:6144]),
        "grow": f(np.concatenate([inp["g_sb"][layer], inp["g_nsa"][layer]])[None]), "gpm": f(inp["g_post_mix"][layer][None]),
        "gpf": f(inp["g_pre_ffn"][layer][None]), "gqf": f(inp["g_post_ffn"][layer][None]), "wout": f(inp["w_out"][layer]),
    }
    i = layer // 2
    if layer % 2 == 0:
        m["wg"] = f(inp["ffn_w_gate"][i][None]); m["wu"] = f(inp["ffn_w_up"][i][None]); m["wd"] = f(inp["ffn_w_down"][i][None])
    else:
        def grp_in(w):
            E, _, dff = w.shape
            return w.reshape(E, 8, 128, dff // 512, 512).transpose(0, 3, 2, 1, 4).reshape(E, dff // 512, 128, 4096)

        def grp_dn(w):
            E, dff, _ = w.shape
            return w.reshape(E, dff // 512, 4, 128, 1024).transpose(0, 1, 3, 2, 4).reshape(E, dff // 512, 128, 4096)

        m["wg"] = f(grp_in(inp["moe_w_gate"][i])); m["wu"] = f(grp_in(inp["moe_w_up"][i])); m["wd"] = f(grp_dn(inp["moe_w_down"][i]))
        m["wr"] = f(inp["moe_w_router"][i].T); m["br"] = f(inp["moe_b_router"][i][None])
    return m


_PROG_CACHE = {}
GROUPS = [[0, 1, 2, 3], [4, 5, 6, 7]]


def build_fused(S):
    NTOK = 2 * S // 8
    NT = NTOK // 128
    nc = bass.Bass("TRN2", target_bir_lowering=False)
    C = mk_ctx(nc)
    P = C.P

    def mk_din(L):
        def din(name, shape):
            return nc.dram_tensor("%s_%d" % (name, L), shape, F32, kind="ExternalInput").ap()
        return din

    xb = nc.dram_tensor("xb", [S, 1024], F32, kind="ExternalInput").ap()
    xtok = nc.dram_tensor("xtok", [NTOK, 1024], F32, kind="ExternalInput").ap()
    idx_d = nc.dram_tensor("idxg", [128, NT * 4], mybir.dt.int32, kind="ExternalInput").ap()
    y = nc.dram_tensor("y", [NTOK, 1024], F32, kind="ExternalOutput").ap()
    o0 = nc.dram_tensor("o0", [S, 256], F32).ap(); og0 = nc.dram_tensor("og0", [4 * S, 256], F32).ap()
    o1 = nc.dram_tensor("o1", [S, 256], F32).ap(); og1 = nc.dram_tensor("og1", [4 * S, 256], F32).ap()
    xo0 = nc.dram_tensor("xo0", [NTOK, 1024], F32).ap(); xg1 = nc.dram_tensor("xg1", [4 * NTOK, 1024], F32).ap()
    def gather_o(o, og, dsrc, ddst):
        for c in range(S // 1024):
            P.coll("AllGather", o[c * 1024:(c + 1) * 1024, :], og[c * 4096:(c + 1) * 4096, :], GROUPS, r=[dsrc], w=[ddst])

    dx0 = Dep()
    C.dxsrc = lambda tt: dx0
    C.xtile = lambda tt: xb[tt * 128:(tt + 1) * 128, :]
    dog0 = Dep()
    C.og, C.dog = og0, dog0
    build_A(C, 0, S, xb, o0, mk_din(0)); build_A_nsa(C)
    dxo0 = Dep()
    build_B(C, 0, NTOK, 1, 2816, xtok, Dep(), og0, dog0, idx_d, xo0, dxo0, mk_din(0))
    dxg1 = deps(NTOK // 256)
    for c in range(NTOK // 256):
        P.coll("AllGather", xo0[c * 256:(c + 1) * 256, :], xg1[c * 1024:(c + 1) * 1024, :], GROUPS, r=[dxo0], w=[dxg1[c]])
    C.dxsrc = lambda tt: dxg1[((tt * 128) % NTOK) // 256]

    def xtile1(tt):
        t = tt * 128
        rank, row = t // NTOK, t % NTOK
        c, rr = row // 256, row % 256
        r0 = c * 1024 + rank * 256 + rr
        return xg1[r0:r0 + 128, :]

    C.xtile = xtile1
    dog1 = Dep()
    C.og, C.dog = og1, dog1
    build_A(C, 1, S, xg1, o1, mk_din(1)); build_A_nsa(C)
    dy = Dep()
    build_B(C, 1, NTOK, 8, 3584, xo0, dxo0, og1, dog1, idx_d, y, dy, mk_din(1))
    P.finish("sp")
    P.emit()
    return nc


def kernel(**inp):
    inp = {k: np.asarray(v) for k, v in inp.items()}
    x = np.ascontiguousarray(inp["x"], dtype=np.float32)
    B, S, _ = x.shape
    NTOK = B * S // 8
    NT = NTOK // 128
    if S not in _PROG_CACHE:
        _PROG_CACHE[S] = build_fused(S)
    nc = _PROG_CACHE[S]
    maps = []
    for cid in range(8):
        b, part = cid // 4, cid % 4
        m = {"xb": x[b], "xtok": np.ascontiguousarray(x[b, part * NTOK:(part + 1) * NTOK])}
        p = np.arange(128)[:, None, None]
        tt = np.arange(NT)[None, :, None]
        r = np.arange(4)[None, None, :]
        t0 = part * NTOK + tt * 128
        m["idxg"] = ((t0 // 1024) * 4096 + r * 1024 + (t0 % 1024) + p).reshape(128, NT * 4).astype(np.int32)
        for L in range(2):
            for k, v in prep_A(inp, L, b, part, S).items():
                m["%s_%d" % (k, L)] = v
            for k, v in prep_B(inp, L, b).items():
                m["%s_%d" % (k, L)] = v
        maps.append(m)
    res = run_bass_kernel_spmd(nc, maps, core_ids=list(range(8)))
    out = np.zeros((B, S, 1024), np.float32)
    for cid in range(8):
        b, part = cid // 4, cid % 4
        out[b, part * NTOK:(part + 1) * NTOK] = res.results[cid]["y"]
    return out
```
